# Optimizing a Trainium2 kernel written in Bass

```python
import math
import jax, jax.numpy as jnp
from jax import lax
import numpy as np

D_MODEL = 1024
BATCH = 4
SEQ = 8192
DEPTH = 1

ATT_HEADS = 8
HEAD_DIM = 64
ATT_WIDTH = ATT_HEADS * HEAD_DIM
MOBA_BLOCK = 256
MOBA_TOPK = 3
Q_CHUNK = 64
ATT_SCALE = 1.0 / math.sqrt(HEAD_DIM)
SSM_WIDTH = D_MODEL // 2
SSM_GROUP = 16
SSM_GROUPS = SSM_WIDTH // SSM_GROUP
SSM_STATE = 64
DT_MIN = 1e-3
DT_MAX = 1e-1
D_FF = 2816
N_ADA = 9
EPS = 1e-6
NEG = -1e30
IN_SPLITS = [ATT_WIDTH, 2 * ATT_WIDTH, 3 * ATT_WIDTH, 3 * ATT_WIDTH + SSM_WIDTH,
             3 * ATT_WIDTH + SSM_WIDTH + D_MODEL]
IN_COLS = 3 * ATT_WIDTH + SSM_WIDTH + 2 * D_MODEL

kernel_name = "hybrid_moba_s5_macaron_adaln"


def rmsnorm(x, g):
    xf = x.astype(jnp.float32)
    y = xf * lax.rsqrt(jnp.mean(xf * xf, axis=-1, keepdims=True) + EPS)
    return (y * g.astype(jnp.float32)).astype(x.dtype)


def modulate(h, shift, scale):
    return h * (1.0 + scale[:, None, :]) + shift[:, None, :]


def swiglu(h, w_gu, w_down):
    g, u = jnp.split(h @ w_gu, 2, axis=-1)
    return (jax.nn.silu(g) * u) @ w_down


def alibi_slopes():
    return jnp.asarray([2.0 ** (-8.0 * (h + 1) / ATT_HEADS) for h in range(ATT_HEADS)],
                       dtype=jnp.float32)


def moba_attention(q, k, v):
    bsz, s = q.shape[0], q.shape[1]
    nb = -(-s // MOBA_BLOCK)
    s_pad = nb * MOBA_BLOCK
    n_sel = min(MOBA_TOPK, nb)
    pad = ((0, 0), (0, s_pad - s), (0, 0), (0, 0))
    q, k, v = [jnp.pad(t, pad).transpose(0, 2, 1, 3) for t in (q, k, v)]
    kb = k.reshape(bsz, ATT_HEADS, nb, MOBA_BLOCK, HEAD_DIM)
    vb = v.reshape(bsz, ATT_HEADS, nb, MOBA_BLOCK, HEAD_DIM)
    kmean = jnp.mean(kb.astype(jnp.float32), axis=3)
    slopes = alibi_slopes()[None, :, None, None]
    bi = jnp.arange(bsz)[:, None, None]
    hi = jnp.arange(ATT_HEADS)[None, :, None]
    blk_ids = jnp.arange(nb)
    key_off = jnp.arange(MOBA_BLOCK)

    def chunk(ci):
        q0 = ci * Q_CHUNK
        own = q0 // MOBA_BLOCK
        qc = lax.dynamic_slice_in_dim(q, q0, Q_CHUNK, axis=2)
        tq = (q0 + jnp.arange(Q_CHUNK)).astype(jnp.float32)
        gate = jnp.einsum('bhqd,bhnd->bhqn', qc.astype(jnp.float32), kmean)
        gate = jnp.where(blk_ids < own, gate, NEG)
        _, sel = lax.top_k(gate, n_sel)
        scores, vals = [], []
        for j in range(n_sel):
            idx = sel[..., j]
            kj = kb[bi, hi, idx]
            vj = vb[bi, hi, idx]
            sj = jnp.einsum('bhqd,bhqkd->bhqk', qc, kj).astype(jnp.float32) * ATT_SCALE
            kpos = (idx[..., None] * MOBA_BLOCK + key_off).astype(jnp.float32)
            sj = sj - slopes * (tq[:, None] - kpos)
            sj = jnp.where(j < own, sj, NEG)
            scores.append(sj)
            vals.append(vj)
        ko = lax.dynamic_slice_in_dim(k, own * MOBA_BLOCK, MOBA_BLOCK, axis=2)
        vo = lax.dynamic_slice_in_dim(v, own * MOBA_BLOCK, MOBA_BLOCK, axis=2)
        so = jnp.einsum('bhqd,bhkd->bhqk', qc, ko).astype(jnp.float32) * ATT_SCALE
        dist = tq[:, None] - (own * MOBA_BLOCK + key_off).astype(jnp.float32)[None, :]
        so = jnp.where(dist >= 0, so - slopes * dist, NEG)
        scores.append(so)
        p = jax.nn.softmax(jnp.concatenate(scores, axis=-1), axis=-1).astype(v.dtype)
        p = p.reshape(bsz, ATT_HEADS, Q_CHUNK, n_sel + 1, MOBA_BLOCK)
        out = jnp.einsum('bhqk,bhkd->bhqd', p[..., n_sel, :], vo)
        for j in range(n_sel):
            out = out + jnp.einsum('bhqk,bhqkd->bhqd', p[..., j, :], vals[j])
        return out

    out = lax.map(chunk, jnp.arange(s_pad // Q_CHUNK))
    out = out.transpose(1, 0, 3, 2, 4).reshape(bsz, s_pad, ATT_WIDTH)
    return out[:, :s]


def s5_mixer(u, lam_re, lam_im, log_dt, b_re, b_im, c_re, c_im, d_skip, w_glu, b_glu):
    bsz, s, _ = u.shape
    f32 = jnp.float32
    uf = u.astype(f32)
    ug = uf.reshape(bsz, s, SSM_GROUPS, SSM_GROUP)
    lr, li = lam_re.astype(f32), lam_im.astype(f32)
    dt = jnp.exp(log_dt.astype(f32))[:, None]
    mag = jnp.exp(lr * dt)
    ang = li * dt
    ab_re, ab_im = mag * jnp.cos(ang), mag * jnp.sin(ang)
    nr, ni = ab_re - 1.0, ab_im
    den = lr * lr + li * li
    f_re = (nr * lr + ni * li) / den
    f_im = (ni * lr - nr * li) / den
    br, bim = b_re.astype(f32), b_im.astype(f32)
    bb_re = f_re[..., None] * br - f_im[..., None] * bim
    bb_im = f_re[..., None] * bim + f_im[..., None] * br
    bu_re = jnp.einsum('bsgc,gpc->sbgp', ug, bb_re)
    bu_im = jnp.einsum('bsgc,gpc->sbgp', ug, bb_im)
    a_re = jnp.broadcast_to(ab_re, bu_re.shape)
    a_im = jnp.broadcast_to(ab_im, bu_im.shape)

    def combine(e1, e2):
        a1r, a1i, b1r, b1i = e1
        a2r, a2i, b2r, b2i = e2
        return (a2r * a1r - a2i * a1i,
                a2r * a1i + a2i * a1r,
                a2r * b1r - a2i * b1i + b2r,
                a2r * b1i + a2i * b1r + b2i)

    _, _, xr, xi = lax.associative_scan(combine, (a_re, a_im, bu_re, bu_im), axis=0)
    y = (jnp.einsum('sbgp,gcp->bsgc', xr, c_re.astype(f32))
         - jnp.einsum('sbgp,gcp->bsgc', xi, c_im.astype(f32)))
    y = y.reshape(bsz, s, SSM_WIDTH) + d_skip.astype(f32) * uf
    y = jax.nn.gelu(y)
    y = y * jax.nn.sigmoid(y @ w_glu.astype(f32) + b_glu.astype(f32))
    return y.astype(u.dtype)


def setup_inputs(seed: int = 0) -> dict:
    key = jax.random.key(seed)
    ks = jax.random.split(key, 32)
    n = lambda k, shape, s: jax.random.normal(k, shape, jnp.float32) * s
    L, D, G, P, C = DEPTH, D_MODEL, SSM_GROUPS, SSM_STATE, SSM_GROUP
    lam_im0 = jnp.pi * jnp.arange(P, dtype=jnp.float32)
    return {
        "x": n(ks[0], (BATCH, SEQ, D), 1.0),
        "c": n(ks[1], (BATCH, D), 1.0),
        "w_ada": n(ks[2], (L, D, N_ADA * D), D ** -0.5),
        "b_ada": n(ks[3], (L, N_ADA * D), 0.01),
        "norm_ffn1": 1.0 + n(ks[4], (L, D), 0.02),
        "w_ffn1_in": n(ks[5], (L, D, 2 * D_FF), D ** -0.5),
        "w_ffn1_out": n(ks[6], (L, D_FF, D), D_FF ** -0.5),
        "norm_mix": 1.0 + n(ks[7], (L, D), 0.02),
        "w_in": n(ks[8], (L, D, IN_COLS), D ** -0.5),
        "lam_re": -0.5 + n(ks[9], (L, G, P), 0.01),
        "lam_im": lam_im0 + n(ks[10], (L, G, P), 0.01),
        "log_dt": jax.random.uniform(ks[11], (L, G), jnp.float32,
                                     math.log(DT_MIN), math.log(DT_MAX)),
        "ssm_b_re": n(ks[12], (L, G, P, C), (2 * C) ** -0.5),
        "ssm_b_im": n(ks[13], (L, G, P, C), (2 * C) ** -0.5),
        "ssm_c_re": n(ks[14], (L, G, C, P), (2 * P) ** -0.5 * 4.0),
        "ssm_c_im": n(ks[15], (L, G, C, P), (2 * P) ** -0.5 * 4.0),
        "ssm_d": n(ks[16], (L, SSM_WIDTH), 1.0),
        "w_glu": n(ks[17], (L, SSM_WIDTH, SSM_WIDTH), SSM_WIDTH ** -0.5),
        "b_glu": n(ks[18], (L, SSM_WIDTH), 0.01),
        "w_br_att": n(ks[19], (L, ATT_WIDTH, D), ATT_WIDTH ** -0.5),
        "w_br_ssm": n(ks[20], (L, SSM_WIDTH, D), SSM_WIDTH ** -0.5),
        "w_out": n(ks[21], (L, D, D), D ** -0.5),
        "norm_ffn2": 1.0 + n(ks[22], (L, D), 0.02),
        "w_ffn2_in": n(ks[23], (L, D, 2 * D_FF), D ** -0.5),
        "w_ffn2_out": n(ks[24], (L, D_FF, D), D_FF ** -0.5),
        "norm_final": 1.0 + n(ks[25], (D,), 0.02),
    }


def reference(x, c, w_ada, b_ada, norm_ffn1, w_ffn1_in, w_ffn1_out, norm_mix, w_in,
              lam_re, lam_im, log_dt, ssm_b_re, ssm_b_im, ssm_c_re, ssm_c_im, ssm_d,
              w_glu, b_glu, w_br_att, w_br_ssm, w_out, norm_ffn2, w_ffn2_in, w_ffn2_out,
              norm_final):
    bsz, s, _ = x.shape
    for l in range(DEPTH):
        ada = jax.nn.silu(c) @ w_ada[l] + b_ada[l]
        sh1, sc1, g1, sh2, sc2, g2, sh3, sc3, g3 = jnp.split(ada, N_ADA, axis=-1)
        h = modulate(rmsnorm(x, norm_ffn1[l]), sh1, sc1)
        x = x + 0.5 * g1[:, None, :] * swiglu(h, w_ffn1_in[l], w_ffn1_out[l])
        h = modulate(rmsnorm(x, norm_mix[l]), sh2, sc2)
        q, k, v, u, ga, gs = jnp.split(h @ w_in[l], IN_SPLITS, axis=-1)
        hd = (bsz, s, ATT_HEADS, HEAD_DIM)
        y_att = moba_attention(q.reshape(hd), k.reshape(hd), v.reshape(hd))
        y_ssm = s5_mixer(u, lam_re[l], lam_im[l], log_dt[l], ssm_b_re[l], ssm_b_im[l],
                         ssm_c_re[l], ssm_c_im[l], ssm_d[l], w_glu[l], b_glu[l])
        merged = (jax.nn.sigmoid(ga) * (y_att @ w_br_att[l])
                  + jax.nn.sigmoid(gs) * (y_ssm @ w_br_ssm[l]))
        x = x + g2[:, None, :] * (merged @ w_out[l])
        h = modulate(rmsnorm(x, norm_ffn2[l]), sh3, sc3)
        x = x + 0.5 * g3[:, None, :] * swiglu(h, w_ffn2_in[l], w_ffn2_out[l])
    return rmsnorm(x, norm_final)
```

```python
from contextlib import ExitStack
import numpy as np
import concourse.bass as bass
import concourse.mybir as mybir
from concourse.bass_utils import run_bass_kernel_spmd

F32 = mybir.dt.float32
BF16 = mybir.dt.bfloat16
AF = mybir.ActivationFunctionType
ALU = mybir.AluOpType
AX = mybir.AxisListType


class Prog:
    ENGINES = ["sync", "scalar", "vector", "gpsimd", "tensor"]

    def __init__(self, nc, n_dma_sems=40):
        self.nc = nc
        self.stack = ExitStack()
        self.streams = {e: [] for e in self.ENGINES}
        self.sem = {}
        for e in self.ENGINES:
            self.sem[("e", e)] = self.stack.enter_context(nc.semaphore(f"se_{e}"))
        for i in range(n_dma_sems):
            self.sem[("d", i)] = self.stack.enter_context(nc.semaphore(f"sd_{i}"))
        self.n_dma = n_dma_sems
        self.cnt = {k: 0 for k in self.sem}
        self.known = {e: {} for e in self.ENGINES}
        self.snap = {}
        self.res = {}
        self.dnext = 0
        self.finals = []
        self.nops = 0

    def sb(self, name, shape, dtype):
        return self.stack.enter_context(self.nc.sbuf_tensor(name, list(shape), dtype))

    def ps(self, name, shape, dtype):
        return self.stack.enter_context(self.nc.psum_tensor(name, list(shape), dtype))

    def _deps(self, engine, reads, writes):
        deps = {}
        def add(tok):
            if tok is None:
                return
            k, v = tok
            if engine == "tensor" and k == ("e", "tensor"):
                return
            if deps.get(k, 0) < v:
                deps[k] = v
        for r in reads:
            st = self.res.get(r)
            if st:
                add(st["w"])
        for w in writes:
            st = self.res.get(w)
            if st:
                add(st["w"])
                for t in st["r"]:
                    add(t)
        return deps

    def _emit_waits(self, engine, deps):
        kn = self.known[engine]
        for k, v in deps.items():
            if kn.get(k, 0) >= v:
                continue
            self.streams[engine].append(("w", self.sem[k], v))
            sn = self.snap.get((k, v))
            if sn:
                for kk, vv in sn.items():
                    if kn.get(kk, 0) < vv:
                        kn[kk] = vv
            kn[k] = v

    def _commit(self, engine, tok, reads, writes):
        sn = dict(self.known[engine])
        sn[tok[0]] = tok[1]
        self.snap[tok] = sn
        for r in reads:
            st = self.res.setdefault(r, {"w": None, "r": []})
            st["r"].append(tok)
        for w in writes:
            self.res[w] = {"w": tok, "r": []}
        self.nops += 1

    def op(self, engine, fn, reads=(), writes=()):
        deps = self._deps(engine, reads, writes)
        self._emit_waits(engine, deps)
        k = ("e", engine)
        self.cnt[k] += 1
        self.streams[engine].append(("o", fn, self.sem[k], 1))
        tok = (k, self.cnt[k])
        if engine != "tensor":
            pass
        self._commit(engine, tok, reads, writes)
        return tok

    def dma(self, out, in_, reads=(), writes=(), queue="sync", final=False, **kw):
        deps = self._deps(queue, reads, writes)
        idx = self.dnext
        self.dnext = (self.dnext + 1) % self.n_dma
        k = ("d", idx)
        if self.cnt[k] > 0:
            if deps.get(k, 0) < self.cnt[k]:
                deps[k] = self.cnt[k]
        self._emit_waits(queue, deps)
        self.cnt[k] += 16
        self.streams[queue].append(("o", lambda e: e.dma_start(out=out, in_=in_, **kw), self.sem[k], 16))
        tok = (k, self.cnt[k])
        self._commit(queue, tok, reads, writes)
        if final:
            self.finals.append(tok)
        return tok

    def barrier(self):
        for e in self.ENGINES:
            deps = {k: v for k, v in self.cnt.items() if v > 0}
            self._emit_waits(e, deps)
        self.res = {}

    def finish(self):
        deps = {}
        for k, v in self.finals:
            if deps.get(k, 0) < v:
                deps[k] = v
        self._emit_waits("sync", deps)
        with self.nc.Block() as block:
            for e in self.ENGINES:
                stream = self.streams[e]

                def body(eng, stream=stream):
                    for it in stream:
                        if it[0] == "w":
                            eng.wait_ge(it[1], it[2])
                        else:
                            it[1](eng).then_inc(it[2], it[3])

                getattr(block, e)(body)
        self.stack.close()


D = 1024
T = 8192
NT = 512
NTL = T // NT
DFF = 2816
FC = DFF // 128
NBLK = T // 256
TO = T // 2
OT0 = NTL // 2
NEGM = -30000.0
DBG = set()
DBG_NTL = NTL
DBG_NH = 8
DBG_NQT = 64
DBG_FLIP = 0
DBG_NPAIR = 16


class Arena:
    def __init__(self, P, name, n):
        self.t = P.sb(name, [128, n], F32)
        self.n = n
        self.off = 0

    def reset(self):
        self.off = 0

    def f32(self, n):
        ap = self.t[:, self.off:self.off + n]
        self.off += n
        assert self.off <= self.n, (self.off, self.n)
        return ap

    def bf16(self, n):
        m = (n + 1) // 2
        ap = self.t[:, self.off:self.off + m].bitcast(BF16)
        self.off += m
        assert self.off <= self.n, (self.off, self.n)
        return ap


def build_program(stages=("cast", "A", "ssm", "att", "C")):
    nc = bass.Bass("TRN2", target_bir_lowering=False)
    P = Prog(nc)

    def din(name, shape, dt=F32):
        return nc.dram_tensor(name, list(shape), dt, kind="ExternalInput").ap()

    def dscr(name, shape, dt):
        kind = "ExternalOutput" if name in DBG else "Internal"
        return nc.dram_tensor(name, list(shape), dt, kind=kind).ap()

    xT = din("xT", [D, T])
    cT = din("cT", [128, 8])
    w_ada = din("w_ada", [D, 9 * D])
    b_adaT = din("b_adaT", [128, 72])
    nvec = {n: din(n, [128, 8]) for n in ("nf1", "nmix", "nf2", "nfin")}
    w1i = din("w_ffn1_in", [D, 2 * DFF])
    w1o = din("w_ffn1_out", [DFF, D])
    w2i = din("w_ffn2_in", [D, 2 * DFF])
    w2o = din("w_ffn2_out", [DFF, D])
    w_in = din("w_in", [D, 4096])
    w_glu = din("w_glu", [512, 512])
    w_ba = din("w_br_att", [512, D])
    w_bs = din("w_br_ssm", [512, D])
    w_out = din("w_out", [D, D])
    lamr_d = din("lamr", [128, 16])
    lami_d = din("lami", [128, 16])
    logdt_d = din("logdt", [128, 16])
    bsrc_re_d = din("bsrc_re", [128, 16, 32])
    bsrc_im_d = din("bsrc_im", [128, 16, 32])
    csrc_re_d = din("csrc_re", [32, 16, 128])
    csrc_im_d = din("csrc_im", [32, 16, 128])
    dsk_d = din("dsk", [128, 4])
    bglu_d = din("bglu", [128, 4])
    kaug_d = din("kaug", [8, 4, T], BF16)
    qaug_d = din("qaug", [8, 4, T], BF16)
    eind_d = din("eind", [32, T], BF16)
    tri_d = din("tri", [128, 128], BF16)
    elig_d = din("elig", [128, NBLK, NBLK])
    uflag_d = din("uflag", [128, 1])
    outT = nc.dram_tensor("outT", [D, TO], F32, kind="ExternalOutput").ap()

    wgu1_s = dscr("wgu1_s", [2 * FC, 128, 8, 128], BF16)
    wd1_s = dscr("wd1_s", [8, 128, FC, 128], BF16)
    wgu2_s = dscr("wgu2_s", [2 * FC, 128, 8, 128], BF16)
    wd2_s = dscr("wd2_s", [8, 128, FC, 128], BF16)
    win_s = dscr("win_s", [32, 128, 8, 128], BF16)
    wba_s = dscr("wba_s", [8, 128, 4, 128], BF16)
    wbs_s = dscr("wbs_s", [8, 128, 4, 128], BF16)
    wout_s = dscr("wout_s", [8, 128, 8, 128], BF16)
    wglu_s = dscr("wglu_s", [4, 128, 4, 128], BF16)
    x1_s = dscr("x1_s", [D, T], F32)
    qT_s = dscr("qT_s", [512, T], BF16)
    kT_s = dscr("kT_s", [512, T], BF16)
    uT_s = dscr("uT_s", [512, T], BF16)
    v_s = dscr("v_s", [T, 512], BF16)
    sga_s = dscr("sga_s", [D, T], BF16)
    sgs_s = dscr("sgs_s", [D, T], BF16)
    kmean_s = dscr("kmean_s", [512, NBLK], F32)
    yattT_s = dscr("yattT_s", [512, T], BF16)
    ypre_s = dscr("ypre_s", [512, T], F32)

    ones_bf = P.sb("ones_bf", [128, 128], BF16)
    ident_bf = P.sb("ident_bf", [128, 128], BF16)
    ident_f = P.sb("ident_f", [128, 128], F32)
    eps_col = P.sb("eps_col", [128, 1], F32)
    uflag = P.sb("uflag_sb", [128, 1], F32)
    vecs = P.sb("vecs", [128, 32 + 72 + 9 * 8 + 8 + 8], F32)
    nf1 = vecs[:, 0:8]; nmix = vecs[:, 8:16]; nf2 = vecs[:, 16:24]; nfin = vecs[:, 24:32]
    adaT = vecs[:, 32:104]
    der = vecs[:, 104:176]
    a1, b1, g1h, a2, b2, g2, a3, b3, g3h = [der[:, i * 8:(i + 1) * 8] for i in range(9)]
    dsk = vecs[:, 176:180]; bglu = vecs[:, 180:184]
    kmacc = P.sb("kmacc", [128, 4, NBLK], F32)
    ar = Arena(P, "arena", 44600)
    psb = [P.ps(f"psb{i}", [128, 512], F32) for i in range(8)]
    PSN = [f"ps{i}" for i in range(8)]

    def mm(out, lhsT, rhs, start, stop, reads, writes):
        P.op("tensor", lambda e: e.matmul(out, lhsT=lhsT, rhs=rhs, start=start, stop=stop), reads, writes)

    def tr(out, in_, ident, reads, writes):
        P.op("tensor", lambda e: e.transpose(out, in_, ident), reads, writes)

    def act(out, in_, func, reads, writes, **kw):
        P.op("scalar", lambda e: e.activation(out=out, in_=in_, func=func, **kw), reads, writes)

    def ts(eng, out, in0, s1, s2, op0, op1, reads, writes):
        if op1 is None:
            P.op(eng, lambda e: e.tensor_scalar(out=out, in0=in0, scalar1=s1, scalar2=None, op0=op0), reads, writes)
        else:
            P.op(eng, lambda e: e.tensor_scalar(out=out, in0=in0, scalar1=s1, scalar2=s2, op0=op0, op1=op1), reads, writes)

    def stt(out, in0, scalar, in1, op0, op1, reads, writes):
        P.op("vector", lambda e: e.scalar_tensor_tensor(out=out, in0=in0, scalar=scalar, in1=in1, op0=op0, op1=op1), reads, writes)

    def tt(eng, out, in0, in1, op, reads, writes):
        P.op(eng, lambda e: e.tensor_tensor(out=out, in0=in0, in1=in1, op=op), reads, writes)

    def cp(eng, out, in_, reads, writes):
        if eng == "scalar":
            P.op(eng, lambda e: e.copy(out, in_), reads, writes)
        else:
            P.op(eng, lambda e: e.tensor_copy(out=out, in_=in_), reads, writes)

    def pipeline(steps, look=2):
        n = len(steps)
        for i in range(min(look, n)):
            steps[i][0]()
        for i in range(n):
            steps[i][1]()
            if i + look < n:
                steps[i + look][0]()

    P.op("gpsimd", lambda e: e.memset(ones_bf[:], 1.0), [], ["ones_bf"])
    P.op("gpsimd", lambda e: e.memset(ident_bf[:], 1.0), [], ["ident_bf"])
    P.op("gpsimd", lambda e: e.affine_select(out=ident_bf[:], in_=ident_bf[:], pattern=[[-1, 128]],
                                             compare_op=ALU.is_equal, fill=0.0, base=0, channel_multiplier=1),
         ["ident_bf"], ["ident_bf"])
    P.op("gpsimd", lambda e: e.memset(ident_f[:], 1.0), [], ["ident_f"])
    P.op("gpsimd", lambda e: e.affine_select(out=ident_f[:], in_=ident_f[:], pattern=[[-1, 128]],
                                             compare_op=ALU.is_equal, fill=0.0, base=0, channel_multiplier=1),
         ["ident_f"], ["ident_f"])
    P.op("gpsimd", lambda e: e.memset(eps_col[:], 1e-6), [], ["eps_col"])
    P.op("gpsimd", lambda e: e.memset(kmacc[:], 0.0), [], ["kmacc"])
    P.dma(nf1, nvec["nf1"], [], ["vecs"])
    P.dma(nmix, nvec["nmix"], [], ["vecs"])
    P.dma(nf2, nvec["nf2"], [], ["vecs"])
    P.dma(nfin, nvec["nfin"], [], ["vecs"])
    P.dma(dsk, dsk_d, [], ["vecs"])
    P.dma(bglu, bglu_d, [], ["vecs"])
    P.dma(uflag[:], uflag_d, [], ["uflag"])

    ar.reset()
    ct = ar.f32(8)
    sc2 = ar.f32(16).rearrange("p (k t) -> p k t", t=2)
    badaT = ar.f32(72)
    wst = [ar.f32(8 * 512).rearrange("p (k n) -> p k n", k=8) for _ in range(2)]
    P.dma(ct, cT, [], ["ct"])
    P.dma(badaT, b_adaT, [], ["badaT"])
    sgc = ar.f32(8)
    act(sgc, ct, AF.Sigmoid, ["ct"], ["sgc"])
    tt("vector", sc2[:, :, 0], ct, sgc, ALU.mult, ["ct", "sgc"], ["sc2"])
    tt("vector", sc2[:, :, 1], ct, sgc, ALU.mult, ["ct", "sgc"], ["sc2"])
    ps_ada = psb[0][:, 0:144].rearrange("p (j t) -> p j t", t=2)
    w_ada_v = w_ada.rearrange("(k p) n -> p k n", p=128)
    for sl in range(18):
        slot = sl % 2
        P.dma(wst[slot], w_ada_v[:, :, sl * 512:(sl + 1) * 512], [], [f"wst{slot}"])
        for jj in range(4):
            j = sl * 4 + jj
            for k in range(8):
                mm(ps_ada[:, j, :], wst[slot][:, k, jj * 128:(jj + 1) * 128], sc2[:, k, :], k == 0, k == 7,
                   [f"wst{slot}", "sc2"], [PSN[0]])
    tt("vector", adaT, ps_ada[:, :, 0], badaT, ALU.add, [PSN[0], "badaT", "vecs"], ["vecs"])

    def av(i):
        return adaT[:, i * 8:(i + 1) * 8]
    for (aa, bb, gg, nrm, base, gs) in ((a1, b1, g1h, nf1, 0, 0.5), (a2, b2, g2, nmix, 3, 1.0), (a3, b3, g3h, nf2, 6, 0.5)):
        stt(aa, av(base + 1), 1.0, nrm, ALU.add, ALU.mult, ["vecs"], ["vecs"])
        cp("vector", bb, av(base), ["vecs"], ["vecs"])
        ts("vector", gg, av(base + 2), gs, None, ALU.mult, None, ["vecs"], ["vecs"])
    P.barrier()

    CAST_WORDS = 2 * 2816 + 2 * 1408
    CAST_BASE = ar.n - CAST_WORDS
    stg = [ar.t[:, CAST_BASE + i * 2816:CAST_BASE + (i + 1) * 2816] for i in range(2)]
    cbf = [ar.t[:, CAST_BASE + 5632 + i * 1408:CAST_BASE + 5632 + (i + 1) * 1408].bitcast(BF16) for i in range(2)]
    cast_i = [0]

    def cast_slabs(src, dst, K, SW, ldq):
        kc = K // 128
        ncols = src.shape[1]
        srcv = src.rearrange("(k p) n -> p k n", p=128)
        assert kc * SW <= 2816
        for c0 in range(0, ncols, SW):
            def slab(c0=c0):
                i = cast_i[0]
                cast_i[0] += 1
                sl = i % 2
                s_ = stg[sl][:, 0:kc * SW].rearrange("p (k n) -> p k n", k=kc)
                b_ = cbf[sl][:, 0:kc * SW].rearrange("p (k n) -> p k n", k=kc)
                P.dma(s_, srcv[:, :, c0:c0 + SW], [], [f"stg{sl}"], queue=ldq)
                cp(("scalar", "vector")[i % 2], b_, s_, [f"stg{sl}"], [f"cbf{sl}"])
                for cc in range(SW // 128):
                    P.dma(dst[c0 // 128 + cc], b_[:, :, cc * 128:(cc + 1) * 128], [f"cbf{sl}"], [], queue="gpsimd")
            yield slab

    deferred = []
    if "cast" in stages:
        for sl_ in cast_slabs(w1i, wgu1_s, D, 256, "sync"):
            sl_()
        for sl_ in cast_slabs(w1o, wd1_s, DFF, 128, "sync"):
            sl_()
        for sl_ in cast_slabs(w_in, win_s, D, 256, "sync"):
            sl_()
        for (src_, dst_, K_, SW_) in ((w_glu, wglu_s, 512, 512), (w_ba, wba_s, 512, 512), (w_bs, wbs_s, 512, 512),
                                      (w_out, wout_s, D, 256), (w2i, wgu2_s, D, 256), (w2o, wd2_s, DFF, 128)):
            deferred.extend(cast_slabs(src_, dst_, K_, SW_, "gpsimd"))
        P.barrier()

    def alloc_common(nwd=3):
        B = {}
        B["x"] = [ar.f32(8 * NT).rearrange("p (k t) -> p k t", k=8) for _ in range(2)]
        B["h"] = ar.bf16(8 * NT).rearrange("p (k t) -> p k t", k=8)
        B["act"] = ar.bf16(FC * NT).rearrange("p (k t) -> p k t", k=FC)
        B["sq"] = ar.bf16(8 * NT).rearrange("p (k t) -> p k t", k=8)
        B["tmp"] = [ar.f32(NT) for _ in range(2)]
        B["rstd"] = ar.f32(NT)
        B["sd"] = ar.f32(NT)
        B["sg"] = [ar.f32(NT) for _ in range(2)]
        B["wg"] = [ar.bf16(1024).rearrange("p (k j) -> p k j", k=8) for _ in range(3)]
        B["wu"] = [ar.bf16(1024).rearrange("p (k j) -> p k j", k=8) for _ in range(3)]
        B["wd"] = [ar.bf16(FC * 128).rearrange("p (k j) -> p k j", k=FC) for _ in range(nwd)]
        return B

    cnt = {}
    slot = {}

    def nslot(cls, key, n):
        s = cnt.get(cls, 0) % n
        cnt[cls] = cnt.get(cls, 0) + 1
        slot[key] = s
        return s

    def norm_stats(B, xs):
        x = B["x"][xs]
        for k in range(8):
            act(B["sq"][:, k, :], x[:, k, :], AF.Square, [f"x{xs}_{k}"], [f"sq{k}"])
        for k in range(8):
            mm(psb[0][:, :], ones_bf[:], B["sq"][:, k, :], k == 0, k == 7, [f"sq{k}"], [PSN[0]])
        act(B["sd"], psb[0][:, :], AF.Sqrt, [PSN[0]], ["sd"], bias=eps_col[:, 0:1], scale=1.0 / D)
        P.op("vector", lambda e: e.reciprocal(out=B["rstd"], in_=B["sd"]), ["sd"], ["rstd"])

    def norm_mod(B, xs, a, b):
        x = B["x"][xs]
        norm_stats(B, xs)
        for k in range(8):
            t = B["tmp"][k % 2]
            tt("vector", t, x[:, k, :], B["rstd"], ALU.mult, [f"x{xs}_{k}", "rstd"], [f"tmp{k % 2}"])
            act(B["h"][:, k, :], t, AF.Identity, [f"tmp{k % 2}"], [f"h{k}"], bias=b[:, k:k + 1], scale=a[:, k:k + 1])

    def ffn_steps(B, xs, wgu_s, wd_s, gcol, tag):
        steps = []
        x = B["x"][xs]
        for c in range(FC):
            def load(c=c):
                s = nslot("gu", (tag, "gu", c), 3)
                P.dma(B["wg"][s], wgu_s[c], [], [f"wg{s}"])
                P.dma(B["wu"][s], wgu_s[FC + c], [], [f"wu{s}"])

            def comp(c=c):
                s = slot[(tag, "gu", c)]
                pg = 1 + (c % 2)
                pu = 3 + (c % 2)
                for k in range(8):
                    mm(psb[pg][:, :], B["wg"][s][:, k, :], B["h"][:, k, :], k == 0, k == 7, [f"wg{s}", f"h{k}"], [PSN[pg]])
                for k in range(8):
                    mm(psb[pu][:, :], B["wu"][s][:, k, :], B["h"][:, k, :], k == 0, k == 7, [f"wu{s}", f"h{k}"], [PSN[pu]])
                sg = B["sg"][c % 2]
                act(sg, psb[pg][:, :], AF.Silu, [PSN[pg]], [f"sg{c % 2}"])
                tt("vector", B["act"][:, c, :], sg, psb[pu][:, :], ALU.mult, [f"sg{c % 2}", PSN[pu]], [f"act{c}"])
            steps.append((load, comp))
        for m in range(8):
            def load(m=m):
                s = nslot("wd", (tag, "wd", m), len(B["wd"]))
                P.dma(B["wd"][s], wd_s[m], [], [f"wd{s}"])

            def comp(m=m):
                s = slot[(tag, "wd", m)]
                po = 5 + (m % 2)
                for k in range(FC):
                    mm(psb[po][:, :], B["wd"][s][:, k, :], B["act"][:, k, :], k == 0, k == FC - 1, [f"wd{s}", f"act{k}"], [PSN[po]])
                stt(x[:, m, :], psb[po][:, :], gcol[:, m:m + 1], x[:, m, :], ALU.mult, ALU.add,
                    [PSN[po], f"x{xs}_{m}"], [f"x{xs}_{m}"])
            steps.append((load, comp))
        return steps

    xTv = xT.rearrange("(k p) t -> p k t", p=128)
    x1v = x1_s.rearrange("(k p) t -> p k t", p=128)
    XR = lambda xs: [f"x{xs}_{k}" for k in range(8)]

    if "A" in stages:
        ar.reset()
        B = alloc_common()
        win = [ar.bf16(1024).rearrange("p (k j) -> p k j", k=8) for _ in range(3)]
        wv = ar.bf16(4096).rearrange("p (c k j) -> p c k j", c=4, k=8)
        ost = [ar.bf16(NT) for _ in range(4)]
        vst = [ar.bf16(512) for _ in range(2)]
        assert ar.off <= CAST_BASE, (ar.off, CAST_BASE)
        P.dma(B["x"][0], xTv[:, :, 0:NT], [], XR(0))
        for ti in range(DBG_NTL):
            xs = ti % 2
            t0 = ti * NT
            if ti + 1 < NTL:
                P.dma(B["x"][1 - xs], xTv[:, :, t0 + NT:t0 + 2 * NT], [], XR(1 - xs))
            norm_mod(B, xs, a1, b1)
            pipeline(ffn_steps(B, xs, wgu1_s, wd1_s, g1h, ("A1", ti)), 2)
            prefix = ti < OT0
            if not prefix:
                P.dma(x1v[:, :, t0:t0 + NT], B["x"][xs], XR(xs), [], queue="gpsimd")
            norm_mod(B, xs, a2, b2)
            steps = []
            if prefix:
                order = list(range(4, 8)) + ["v"] + list(range(12, 16))
            else:
                order = list(range(0, 8)) + ["v"] + list(range(12, 32))
            for ci, c in enumerate(order):
                if c == "v":
                    def load():
                        for cc in range(4):
                            P.dma(wv[:, cc], win_s[8 + cc], [], ["wv"] if cc == 0 else [f"wv_{cc}"])

                    def comp(ti=ti, t0=t0):
                        for q4 in range(4):
                            for k in range(8):
                                mm(psb[7][:, :].rearrange("p (c j) -> p c j", c=4), B["h"][:, k, q4 * 128:(q4 + 1) * 128],
                                   wv[:, :, k, :], k == 0, k == 7, ["wv", "wv_1", "wv_2", "wv_3", f"h{k}"], [PSN[7]])
                            s = nslot("vst", None, 2)
                            cp("vector", vst[s], psb[7][:, :], [PSN[7]], [f"vst{s}"])
                            P.dma(v_s[t0 + q4 * 128:t0 + (q4 + 1) * 128, :], vst[s], [f"vst{s}"], [], queue="gpsimd")
                    steps.append((load, comp))
                    continue

                def load(c=c, ti=ti):
                    s = nslot("win", ("A", ti, c), 3)
                    P.dma(win[s], win_s[c], [], [f"win{s}"])

                def comp(c=c, ci=ci, ti=ti, t0=t0, prefix=prefix):
                    s = slot[("A", ti, c)]
                    po = 5 + (ci % 2)
                    for k in range(8):
                        mm(psb[po][:, :], win[s][:, k, :], B["h"][:, k, :], k == 0, k == 7, [f"win{s}", f"h{k}"], [PSN[po]])
                    o = nslot("ost", None, 4)
                    if c < 4:
                        act(ost[o], psb[po][:, :], AF.Identity, [PSN[po]], [f"ost{o}"], scale=0.125)
                        dst = qT_s[c * 128:(c + 1) * 128, t0:t0 + NT]
                    elif c < 8:
                        cp("vector", ost[o], psb[po][:, :], [PSN[po]], [f"ost{o}"])
                        P.op("vector", lambda e: e.tensor_reduce(out=kmacc[:, c - 4, 2 * ti:2 * ti + 2],
                                                                 in_=psb[po][:, :].rearrange("p (b t) -> p b t", b=2),
                                                                 axis=AX.X, op=ALU.add), [PSN[po]], ["kmacc"])
                        dst = kT_s[(c - 4) * 128:(c - 3) * 128, t0:t0 + NT]
                    elif c < 16:
                        if prefix:
                            act(ost[o], psb[po][:, :], AF.Identity, [PSN[po]], [f"ost{o}"], scale=uflag[:, 0:1])
                        else:
                            act(ost[o], psb[po][:, :], AF.Identity, [PSN[po]], [f"ost{o}"])
                        dst = uT_s[(c - 12) * 128:(c - 11) * 128, t0:t0 + NT]
                    elif c < 24:
                        act(ost[o], psb[po][:, :], AF.Sigmoid, [PSN[po]], [f"ost{o}"])
                        dst = sga_s[(c - 16) * 128:(c - 15) * 128, t0:t0 + NT]
                    else:
                        act(ost[o], psb[po][:, :], AF.Sigmoid, [PSN[po]], [f"ost{o}"])
                        dst = sgs_s[(c - 24) * 128:(c - 23) * 128, t0:t0 + NT]
                    P.dma(dst, ost[o], [f"ost{o}"], [], queue="gpsimd")
                steps.append((load, comp))
            pipeline(steps, 2)
            for _ in range(3):
                if deferred:
                    deferred.pop(0)()
        while deferred:
            deferred.pop(0)()
        ts("vector", kmacc[:], kmacc[:], 1.0 / 256, None, ALU.mult, None, ["kmacc"], ["kmacc"])
        P.dma(kmean_s.rearrange("(c p) n -> p c n", p=128), kmacc[:], ["kmacc"], [])
        P.barrier()


    if "ssm" in stages:
        ar.reset()
        hpi = ar.f32(1)
        P.op("gpsimd", lambda e: e.memset(hpi, float(np.pi / 2)), [], ["sm"])
        SM = {}
        def smt(name, n=16):
            SM[name] = ar.f32(n)
            return SM[name]
        for nm in ("lr", "li", "ld", "dt", "mag", "ang", "c", "s", "t1", "t2", "t3", "abr", "abi", "nr", "den", "fre", "fim", "nfim"):
            smt(nm)
        AR = ar.f32(13 * 16).rearrange("p (k g) -> p k g", k=13)
        AI = ar.f32(13 * 16).rearrange("p (k g) -> p k g", k=13)
        NAI = ar.f32(13 * 16).rearrange("p (k g) -> p k g", k=13)
        UR = ar.f32(9 * 16).rearrange("p (k g) -> p k g", k=9)
        UI = ar.f32(9 * 16).rearrange("p (k g) -> p k g", k=9)
        NUI = ar.f32(9 * 16).rearrange("p (k g) -> p k g", k=9)
        R = ["sm"]
        P.dma(SM["lr"], lamr_d, [], R)
        P.dma(SM["li"], lami_d, [], R)
        P.dma(SM["ld"], logdt_d, [], R)
        act(SM["dt"], SM["ld"], AF.Exp, R, R)
        tt("vector", SM["t1"], SM["lr"], SM["dt"], ALU.mult, R, R)
        act(SM["mag"], SM["t1"], AF.Exp, R, R)
        tt("vector", SM["ang"], SM["li"], SM["dt"], ALU.mult, R, R)
        act(SM["s"], SM["ang"], AF.Sin, R, R, scale=1.0 / 16)
        act(SM["c"], SM["ang"], AF.Sin, R, R, scale=1.0 / 16, bias=hpi)
        for _ in range(4):
            tt("vector", SM["t1"], SM["c"], SM["c"], ALU.mult, R, R)
            tt("vector", SM["t2"], SM["s"], SM["s"], ALU.mult, R, R)
            tt("vector", SM["t3"], SM["c"], SM["s"], ALU.mult, R, R)
            tt("vector", SM["c"], SM["t1"], SM["t2"], ALU.subtract, R, R)
            ts("vector", SM["s"], SM["t3"], 2.0, None, ALU.mult, None, R, R)
        cp("vector", UR[:, 0, :], SM["c"], R, R)
        cp("vector", UI[:, 0, :], SM["s"], R, R)
        for k in range(8):
            tt("vector", SM["t1"], UR[:, k, :], UR[:, k, :], ALU.mult, R, R)
            tt("vector", SM["t2"], UI[:, k, :], UI[:, k, :], ALU.mult, R, R)
            tt("vector", SM["t3"], UR[:, k, :], UI[:, k, :], ALU.mult, R, R)
            tt("vector", UR[:, k + 1, :], SM["t1"], SM["t2"], ALU.subtract, R, R)
            ts("vector", UI[:, k + 1, :], SM["t3"], 2.0, None, ALU.mult, None, R, R)
        ts("vector", NUI.rearrange("p k g -> p (k g)"), UI.rearrange("p k g -> p (k g)"), -1.0, None, ALU.mult, None, R, R)
        tt("vector", AR[:, 0, :], SM["mag"], SM["c"], ALU.mult, R, R)
        tt("vector", AI[:, 0, :], SM["mag"], SM["s"], ALU.mult, R, R)
        for k in range(12):
            tt("vector", SM["t1"], AR[:, k, :], AR[:, k, :], ALU.mult, R, R)
            tt("vector", SM["t2"], AI[:, k, :], AI[:, k, :], ALU.mult, R, R)
            tt("vector", SM["t3"], AR[:, k, :], AI[:, k, :], ALU.mult, R, R)
            tt("vector", AR[:, k + 1, :], SM["t1"], SM["t2"], ALU.subtract, R, R)
            ts("vector", AI[:, k + 1, :], SM["t3"], 2.0, None, ALU.mult, None, R, R)
        ts("vector", NAI.rearrange("p k g -> p (k g)"), AI.rearrange("p k g -> p (k g)"), -1.0, None, ALU.mult, None, R, R)
        ts("vector", SM["nr"], AR[:, 0, :], -1.0, None, ALU.add, None, R, R)
        tt("vector", SM["t1"], SM["lr"], SM["lr"], ALU.mult, R, R)
        tt("vector", SM["t2"], SM["li"], SM["li"], ALU.mult, R, R)
        tt("vector", SM["den"], SM["t1"], SM["t2"], ALU.add, R, R)
        P.op("vector", lambda e: e.reciprocal(out=SM["den"], in_=SM["den"]), R, R)
        tt("vector", SM["t1"], SM["nr"], SM["lr"], ALU.mult, R, R)
        tt("vector", SM["t2"], AI[:, 0, :], SM["li"], ALU.mult, R, R)
        tt("vector", SM["t1"], SM["t1"], SM["t2"], ALU.add, R, R)
        tt("vector", SM["fre"], SM["t1"], SM["den"], ALU.mult, R, R)
        tt("vector", SM["t1"], AI[:, 0, :], SM["lr"], ALU.mult, R, R)
        tt("vector", SM["t2"], SM["nr"], SM["li"], ALU.mult, R, R)
        tt("vector", SM["t1"], SM["t1"], SM["t2"], ALU.subtract, R, R)
        tt("vector", SM["fim"], SM["t1"], SM["den"], ALU.mult, R, R)
        BTre = ar.bf16(16 * 128).rearrange("p (q j) -> p q j", q=16)
        BTim = ar.bf16(16 * 128).rearrange("p (q j) -> p q j", q=16)
        CTre = ar.f32(16 * 32).rearrange("p (g c) -> p g c", g=16)
        CTimn = ar.f32(16 * 32).rearrange("p (g c) -> p g c", g=16)
        CTren = ar.f32(16 * 32).rearrange("p (g c) -> p g c", g=16)
        ssm_mark = ar.off
        bre = ar.f32(16 * 32).rearrange("p (g c) -> p g c", g=16)
        bim = ar.f32(16 * 32).rearrange("p (g c) -> p g c", g=16)
        btmp = ar.f32(32)
        bbre = ar.bf16(16 * 32).rearrange("p (g c) -> p g c", g=16)
        bbim = ar.bf16(16 * 32).rearrange("p (g c) -> p g c", g=16)
        csr = ar.f32(16 * 128).rearrange("p (g j) -> p g j", g=16)
        csi = ar.f32(16 * 128).rearrange("p (g j) -> p g j", g=16)
        P.dma(bre, bsrc_re_d, [], ["bre"])
        P.dma(bim, bsrc_im_d, [], ["bim"])
        P.dma(csr[0:32], csrc_re_d, [], ["csr"])
        P.dma(csi[0:32], csrc_im_d, [], ["csi"])
        for g in range(16):
            ts("vector", btmp, bim[:, g, :], SM["fim"][:, g:g + 1], None, ALU.mult, None, ["bim"] + R, ["btmp"])
            stt(bbre[:, g, :], bre[:, g, :], SM["fre"][:, g:g + 1], btmp, ALU.mult, ALU.subtract, ["bre", "btmp"] + R, ["bbre"])
            ts("vector", btmp, bre[:, g, :], SM["fim"][:, g:g + 1], None, ALU.mult, None, ["bre"] + R, ["btmp"])
            stt(bbim[:, g, :], bim[:, g, :], SM["fre"][:, g:g + 1], btmp, ALU.mult, ALU.add, ["bim", "btmp"] + R, ["bbim"])
        psbf = [psb[i][:, :].bitcast(BF16) for i in range(8)]
        for g in range(16):
            tr(psbf[0][0:32, 0:128], bbre[:, g, :], ident_bf[:], ["bbre"], [PSN[0]])
            tr(psbf[0][0:32, 128:256], bbim[:, g, :], ident_bf[:], ["bbim"], [PSN[0]])
            cp("vector", BTre[0:32, g, :], psbf[0][0:32, 0:128], [PSN[0]], ["BT"])
            cp("vector", BTim[0:32, g, :], psbf[0][0:32, 128:256], [PSN[0]], ["BT"])
        for g in range(16):
            tr(psb[1][:, 0:32], csr[0:32, g, :], ident_f[0:32, 0:32], ["csr"], [PSN[1]])
            tr(psb[1][:, 32:64], csi[0:32, g, :], ident_f[0:32, 0:32], ["csi"], [PSN[1]])
            cp("vector", CTre[:, g, :], psb[1][:, 0:32], [PSN[1]], ["CT"])
            ts("vector", CTimn[:, g, :], psb[1][:, 32:64], -1.0, None, ALU.mult, None, [PSN[1]], ["CT"])
            ts("vector", CTren[:, g, :], psb[1][:, 0:32], -1.0, None, ALU.mult, None, [PSN[1]], ["CT"])
        P.barrier()
        ar.off = ssm_mark
        H = TO
        LC = 256
        NCH = H // LC
        ZTr, ZTi, XTr, XTi = ar.f32(H), ar.f32(H), ar.f32(H), ar.f32(H)
        ZP = [ar.f32(H), ar.f32(H)]
        TQ = [ar.f32(H // 2), ar.f32(H // 2)]
        up1 = ar.bf16(T)
        yst = [ar.f32(512) for _ in range(2)]
        Dc = [ar.f32(512) for _ in range(2)]
        Ds = [ar.f32(512) for _ in range(2)]
        dtmp = ar.f32(128)
        ptm = [ar.f32(512) for _ in range(4)]
        ones_l = ar.f32(LC)
        rtile = ar.f32(LC)
        ini = ar.f32(4)
        P.op("gpsimd", lambda e: e.memset(ones_l, 1.0), [], ["ones_l"])
        tbufs = [(ZP[0], ZP[1], "zpr", "zpi"), (TQ[0], TQ[1], "tqr", "tqi")]

        def build_D(g):
            d = g % 2
            dc, ds_ = Dc[d], Ds[d]
            rn = [f"D{d}"]
            P.op("vector", lambda e: e.memset(dc[:, 0:1], 1.0), rn, rn)
            P.op("vector", lambda e: e.memset(ds_[:, 0:1], 0.0), rn, rn)
            for k in range(8):
                n = 1 << k
                ur, ui, nui = UR[:, k, g:g + 1], UI[:, k, g:g + 1], NUI[:, k, g:g + 1]
                ts("vector", dtmp[:, 0:n], ds_[:, 0:n], nui, None, ALU.mult, None, rn + R, ["dtmp"])
                ts("vector", dc[:, n:2 * n], dc[:, 0:n], ur, None, ALU.mult, None, rn + R, rn)
                tt("vector", dc[:, n:2 * n], dc[:, n:2 * n], dtmp[:, 0:n], ALU.add, rn + ["dtmp"], rn)
                ts("vector", dtmp[:, 0:n], ds_[:, 0:n], ur, None, ALU.mult, None, rn + R, ["dtmp"])
                ts("vector", ds_[:, n:2 * n], dc[:, 0:n], ui, None, ALU.mult, None, rn + R, rn)
                tt("vector", ds_[:, n:2 * n], ds_[:, n:2 * n], dtmp[:, 0:n], ALU.add, rn + ["dtmp"], rn)
            cp("vector", dc[:, 256:512], dc[:, 0:256], rn, rn)
            cp("vector", ds_[:, 256:512], ds_[:, 0:256], rn, rn)

        build_D(0)
        for g in range(DBG_NPAIR):
            d = g % 2
            dc, ds_ = Dc[d], Ds[d]
            DN = [f"D{d}"]
            P.dma(up1[0:32, :], uT_s[g * 32:(g + 1) * 32, :], [], ["up0"])
            if g + 1 < 16:
                build_D(g + 1)
            ts("vector", rtile, ones_l, SM["mag"][:, g:g + 1], None, ALU.mult, None, ["ones_l"] + R, ["rtile"])
            for blk in range(16):
                cs = slice(blk * 512, (blk + 1) * 512)
                pr = 2 + (blk % 2)
                pi_ = 4 + (blk % 2)
                mm(psb[pr][:, :], BTre[0:32, g, :], up1[0:32, cs], True, True, ["up0", "BT"], [PSN[pr]])
                mm(psb[pi_][:, :], BTim[0:32, g, :], up1[0:32, cs], True, True, ["up0", "BT"], [PSN[pi_]])
                if blk < 8:
                    cp("scalar", ZP[0][:, cs], psb[pr][:, :], [PSN[pr]], ["zpr"])
                    cp("scalar", ZP[1][:, cs], psb[pi_][:, :], [PSN[pi_]], ["zpi"])
                else:
                    ob = blk - 8
                    co = slice(ob * 512, (ob + 1) * 512)
                    t1, t2 = ptm[0], ptm[1]
                    tt("vector", ZTr[:, co], psb[pr][:, :], dc, ALU.mult, [PSN[pr]] + DN, [f"ztr{ob}"])
                    tt("vector", t1, psb[pi_][:, :], ds_, ALU.mult, [PSN[pi_]] + DN, ["ptm0"])
                    tt("vector", ZTr[:, co], ZTr[:, co], t1, ALU.add, [f"ztr{ob}", "ptm0"], [f"ztr{ob}"])
                    tt("vector", ZTi[:, co], psb[pi_][:, :], dc, ALU.mult, [PSN[pi_]] + DN, [f"zti{ob}"])
                    tt("vector", t2, psb[pr][:, :], ds_, ALU.mult, [PSN[pr]] + DN, ["ptm1"])
                    tt("vector", ZTi[:, co], ZTi[:, co], t2, ALU.subtract, [f"zti{ob}", "ptm1"], [f"zti{ob}"])
            n = H
            for k in range(12):
                sr, si, snr, sni = tbufs[k % 2]
                dr, di, dnr, dni = tbufs[(k + 1) % 2]
                m = n // 2
                svr = sr[:, 0:n].rearrange("p (j two) -> p j two", two=2)
                svi = si[:, 0:n].rearrange("p (j two) -> p j two", two=2)
                stt(dr[:, 0:m], svi[:, :, 0], NAI[:, k, g:g + 1], svr[:, :, 1], ALU.mult, ALU.add, [snr, sni] + R, [dnr])
                stt(dr[:, 0:m], svr[:, :, 0], AR[:, k, g:g + 1], dr[:, 0:m], ALU.mult, ALU.add, [snr, sni, dnr] + R, [dnr])
                stt(di[:, 0:m], svr[:, :, 0], AI[:, k, g:g + 1], svi[:, :, 1], ALU.mult, ALU.add, [snr, sni] + R, [dni])
                stt(di[:, 0:m], svi[:, :, 0], AR[:, k, g:g + 1], di[:, 0:m], ALU.mult, ALU.add, [snr, sni, dni] + R, [dni])
                n = m
            Gr, Gi = ZP[0][:, 0:1], ZP[1][:, 0:1]
            stt(ZTr[:, 0:1], Gi, NAI[:, 0, g:g + 1], ZTr[:, 0:1], ALU.mult, ALU.add, ["zpr", "zpi", "ztr0"] + R, ["ztr0"])
            stt(ZTr[:, 0:1], Gr, AR[:, 0, g:g + 1], ZTr[:, 0:1], ALU.mult, ALU.add, ["zpr", "zpi", "ztr0"] + R, ["ztr0"])
            stt(ZTi[:, 0:1], Gr, AI[:, 0, g:g + 1], ZTi[:, 0:1], ALU.mult, ALU.add, ["zpr", "zpi", "zti0"] + R, ["zti0"])
            stt(ZTi[:, 0:1], Gi, AR[:, 0, g:g + 1], ZTi[:, 0:1], ALU.mult, ALU.add, ["zpr", "zpi", "zti0"] + R, ["zti0"])
            for c in range(NCH):
                cc = slice(c * LC, (c + 1) * LC)
                ob = c // 2
                if c == 0:
                    i_r, i_i = 0.0, 0.0
                else:
                    er, ei = XTr[:, c * LC - 1:c * LC], XTi[:, c * LC - 1:c * LC]
                    ul_r, ul_i, nul_i = UR[:, 8, g:g + 1], UI[:, 8, g:g + 1], NUI[:, 8, g:g + 1]
                    ts("vector", ini[:, 2:3], er, ul_r, None, ALU.mult, None, [f"xtr{(c - 1) // 2}"] + R, ["ini_t"])
                    stt(ini[:, 0:1], ei, nul_i, ini[:, 2:3], ALU.mult, ALU.add, [f"xti{(c - 1) // 2}", "ini_t"] + R, ["ini_r"])
                    ts("vector", ini[:, 3:4], ei, ul_r, None, ALU.mult, None, [f"xti{(c - 1) // 2}"] + R, ["ini_u"])
                    stt(ini[:, 1:2], er, ul_i, ini[:, 3:4], ALU.mult, ALU.add, [f"xtr{(c - 1) // 2}", "ini_u"] + R, ["ini_i"])
                    i_r, i_i = ini[:, 0:1], ini[:, 1:2]
                P.op("vector", lambda e, cc=cc, i_r=i_r: e.tensor_tensor_scan(out=XTr[:, cc], data0=rtile, data1=ZTr[:, cc], initial=i_r,
                                                                          op0=ALU.mult, op1=ALU.add),
                     [f"ztr{ob}", "rtile", "ini_r"], [f"xtr{ob}"])
                P.op("vector", lambda e, cc=cc, i_i=i_i: e.tensor_tensor_scan(out=XTi[:, cc], data0=rtile, data1=ZTi[:, cc], initial=i_i,
                                                                          op0=ALU.mult, op1=ALU.add),
                     [f"zti{ob}", "rtile", "ini_i"], [f"xti{ob}"])
            for ob in range(8):
                co = slice(ob * 512, (ob + 1) * 512)
                t1, t2 = ptm[2 + ob % 2], ptm[ob % 2]
                n1, n2 = f"ptm{2 + ob % 2}", f"ptm{ob % 2}"
                tt("vector", ZTr[:, co], XTr[:, co], dc, ALU.mult, [f"xtr{ob}"] + DN, [f"ztr{ob}"])
                tt("vector", t1, XTi[:, co], ds_, ALU.mult, [f"xti{ob}"] + DN, [n1])
                tt("vector", t2, XTr[:, co], ds_, ALU.mult, [f"xtr{ob}"] + DN, [n2])
                tt("vector", ZTi[:, co], XTi[:, co], dc, ALU.mult, [f"xti{ob}"] + DN, [f"zti{ob}"])
                py = 6 + (ob % 2)
                mm(psb[py][0:32, :], CTre[:, g, :], ZTr[:, co], True, False, [f"ztr{ob}", "CT"], [PSN[py]])
                mm(psb[py][0:32, :], CTren[:, g, :], t1, False, False, [n1, "CT"], [PSN[py]])
                mm(psb[py][0:32, :], CTimn[:, g, :], t2, False, False, [n2, "CT"], [PSN[py]])
                mm(psb[py][0:32, :], CTimn[:, g, :], ZTi[:, co], False, True, [f"zti{ob}", "CT"], [PSN[py]])
                ys = nslot("yst", None, 2)
                cp("scalar", yst[ys][0:32, :], psb[py][0:32, :], [PSN[py]], [f"yst{ys}"])
                P.dma(ypre_s[g * 32:(g + 1) * 32, TO + ob * 512:TO + (ob + 1) * 512], yst[ys][0:32, :], [f"yst{ys}"], [], queue="gpsimd")
        P.barrier()


    if "att" in stages:
        ar.reset()
        Kaug = ar.bf16(T)
        Qaug = ar.bf16(T)
        Vh = ar.bf16(64 * 66).rearrange("p (t d) -> p t d", t=64)
        kmf = ar.f32(32)
        kmb = ar.bf16(32)
        elig = ar.f32(32 * 32).rearrange("p (o n) -> p o n", o=32)
        tri = ar.bf16(128)
        gsc = [ar.f32(32) for _ in range(2)]
        top8 = [ar.f32(8) for _ in range(2)]
        thr = [ar.f32(2) for _ in range(2)]
        mb = [ar.bf16(32) for _ in range(2)]
        pT = [ar.bf16(512) for _ in range(3)]
        rs = [ar.f32(2) for _ in range(2)]
        yh = [ar.bf16(64) for _ in range(2)]
        yattT = ar.bf16(T)
        psbf = [psb[i][:, :].bitcast(BF16) for i in range(8)]
        P.dma(Kaug[64:96, :], eind_d, [], ["Kind"])
        P.dma(tri, tri_d, [], ["tri"])
        P.dma(elig, elig_d, [], ["elig"])
        P.op("gpsimd", lambda e: e.memset(Vh[:, :, 64:66], 1.0), [], ["Vones"])
        SB = [2, 3, 4]

        for h in range(DBG_NH):
            P.dma(Kaug[0:64, :], kT_s[h * 64:(h + 1) * 64, :], [], ["Kaug"])
            P.dma(Kaug[96:100, :], kaug_d[h], [], ["Kaug2"])
            P.dma(Qaug[0:64, TO:], qT_s[h * 64:(h + 1) * 64, TO:], [], ["Qaug"])
            P.dma(Qaug[96:100, TO:], qaug_d[h][:, TO:], [], ["Qaug2"])
            vsrc = v_s[:, h * 64:(h + 1) * 64].rearrange("(t p) d -> p t d", p=128)
            for v4 in range(4):
                P.dma(Vh[:, v4 * 16:(v4 + 1) * 16, 0:64], vsrc[:, v4 * 16:(v4 + 1) * 16, :], [], ["Vh"] if v4 == 0 else [f"Vh{v4}"])
            P.dma(kmf[0:64, :], kmean_s[h * 64:(h + 1) * 64, :], [], ["kmf"])
            cp("vector", kmb[0:64, :], kmf[0:64, :], ["kmf"], ["kmb"])
            KR = ["Kaug", "Kaug2", "Kind"]
            QR = ["Qaug", "Qaug2"]
            VR = ["Vh", "Vh1", "Vh2", "Vh3", "Vones"]

            def gate1(qt):
                own = qt // 2
                qc = slice(qt * 128, (qt + 1) * 128)
                p = qt % 2
                mm(psb[0][:, p * 32:(p + 1) * 32], Qaug[0:64, qc], kmb[0:64, :], True, True, ["Qaug", "kmb"], ["psGb"])
                tt("vector", gsc[p], psb[0][:, p * 32:(p + 1) * 32], elig[:, own, :], ALU.add, ["psGb", "elig"], [f"g2{p}"])
                P.op("vector", lambda e: e.max(out=top8[p], in_=gsc[p]), [f"g2{p}"], [f"top8{p}"])
                ts("vector", thr[p][:, 0:1], top8[p][:, 2:3], -1e29, None, ALU.max, None, [f"top8{p}"], [f"thr{p}"])
                ts("vector", mb[p], gsc[p], thr[p][:, 0:1], NEGM, ALU.is_lt, ALU.mult, [f"g2{p}", f"thr{p}"], [f"mb{p}"])
                P.op("vector", lambda e: e.memset(mb[p][:, own:own + 1], 0.0), [f"mb{p}"], [f"mb{p}"])

            def gate2(qt):
                qc = slice(qt * 128, (qt + 1) * 128)
                p = qt % 2
                tr(psbf[1][64:96, p * 128:(p + 1) * 128], mb[p], ident_bf[:], [f"mb{p}"], ["psTb"])
                cp("scalar", Qaug[64:96, qc], psbf[1][64:96, p * 128:(p + 1) * 128], ["psTb"], [f"qm{qt}"])

            gate1(32)
            gate2(32)
            sctr = 0
            for qt in range(32, 64):
                own = qt // 2
                i = qt % 2
                qc = slice(qt * 128, (qt + 1) * 128)
                diag = 2 * own + i
                if qt + 1 < 64:
                    gate1(qt + 1)
                kts = list(range(diag + 1))
                groups = [kts[a:a + 4] for a in range(0, len(kts), 4)]
                po = 5 + ((qt + DBG_FLIP) % 2)
                psO = psb[po][:, 0:65]

                def emit_S(gi):
                    bank = SB[(sctr + gi) % 3]
                    psS = psb[bank][:, :].rearrange("p (s q) -> p s q", s=4)
                    for sl, kt in enumerate(groups[gi]):
                        kc = slice(kt * 128, (kt + 1) * 128)
                        mm(psS[:, sl, :], Kaug[0:100, kc], Qaug[0:100, qc], True, kt != diag, KR + QR + [f"qm{qt}"], [PSN[bank]])
                        if kt == diag:
                            mm(psS[:, sl, :], ident_bf[:], tri, False, True, ["tri"], [PSN[bank]])

                def emit_EP(gi):
                    bank = SB[(sctr + gi) % 3]
                    n = len(groups[gi])
                    pt = pT[(sctr + gi) % 3]
                    act(pt[:, 0:n * 128], psb[bank][:, 0:n * 128], AF.Exp, [PSN[bank]], [f"pT{(sctr + gi) % 3}"])
                    for sl, kt in enumerate(groups[gi]):
                        mm(psO, pt[:, sl * 128:(sl + 1) * 128], Vh[:, kt, 0:65], kt == 0, kt == diag,
                           [f"pT{(sctr + gi) % 3}"] + VR, [PSN[po]])

                emit_S(0)
                for gi in range(len(groups)):
                    if gi + 1 < len(groups):
                        emit_S(gi + 1)
                    emit_EP(gi)
                sctr += len(groups)
                p = qt % 2
                P.op("vector", lambda e, p=p, po=po: e.reciprocal(out=rs[p][:, 0:1], in_=psb[po][:, 64:65]), [PSN[po]], [f"rs{p}"])
                ts("vector", yh[p], psb[po][:, 0:64], rs[p][:, 0:1], None, ALU.mult, None, [PSN[po], f"rs{p}"], [f"yh{p}"])
                tr(psbf[7][0:64, p * 128:(p + 1) * 128], yh[p], ident_bf[:], [f"yh{p}"], ["psYb"])
                cp("scalar", yattT[0:64, qc], psbf[7][0:64, p * 128:(p + 1) * 128], ["psYb"], ["yattT"])
                if qt + 1 < 64:
                    gate2(qt + 1)
            P.dma(yattT_s[h * 64:(h + 1) * 64, TO:], yattT[0:64, TO:], ["yattT"], [], queue="gpsimd")
        P.barrier()

    def phase_C(mix):
        ar.reset()
        B = alloc_common(2 if mix else 3)
        outv = outT.rearrange("(k p) t -> p k t", p=128)
        if mix:
            yatt = ar.bf16(4 * NT).rearrange("p (k t) -> p k t", k=4)
            ypre = ar.f32(4 * NT).rearrange("p (k t) -> p k t", k=4)
            uTt = ar.bf16(4 * NT).rearrange("p (k t) -> p k t", k=4)
            sga = ar.bf16(8 * NT).rearrange("p (k t) -> p k t", k=8)
            sgs = ar.bf16(8 * NT).rearrange("p (k t) -> p k t", k=8)
            merged = ar.bf16(8 * NT).rearrange("p (k t) -> p k t", k=8)
            gT = ar.bf16(4 * NT).rearrange("p (k t) -> p k t", k=4)
            yssm = ar.bf16(4 * NT).rearrange("p (k t) -> p k t", k=4)
            yt = [ar.f32(NT) for _ in range(4)]
            wb = [ar.bf16(512).rearrange("p (k j) -> p k j", k=4) for _ in range(6)]
            wo = [ar.bf16(1024).rearrange("p (k j) -> p k j", k=8) for _ in range(3)]
            v4 = lambda d: d.rearrange("(k p) t -> p k t", p=128)
        P.dma(B["x"][0], x1v[:, :, OT0 * NT:(OT0 + 1) * NT], [], XR(0))
        for ti in range(OT0, NTL):
            xs = ti % 2
            t0 = ti * NT
            x = B["x"][xs]
            if ti + 1 < NTL:
                P.dma(B["x"][1 - xs], x1v[:, :, t0 + NT:t0 + 2 * NT], [], XR(1 - xs))
            if mix:
                P.dma(yatt, v4(yattT_s)[:, :, t0:t0 + NT], [], ["yatt"])
                P.dma(ypre, v4(ypre_s)[:, :, t0:t0 + NT], [], ["ypre"])
                P.dma(uTt, v4(uT_s)[:, :, t0:t0 + NT], [], ["uTt"])
                P.dma(sga, v4(sga_s)[:, :, t0:t0 + NT], [], ["sga"])
                P.dma(sgs, v4(sgs_s)[:, :, t0:t0 + NT], [], ["sgs"])
                for k in range(4):
                    ya = yt[k % 2]
                    yb = yt[2 + k % 2]
                    stt(ya, uTt[:, k, :], dsk[:, k:k + 1], ypre[:, k, :], ALU.mult, ALU.add, ["uTt", "ypre"], [f"yt{k % 2}"])
                    act(yb, ya, AF.Square, [f"yt{k % 2}"], [f"yt{2 + k % 2}"])
                    ts("vector", yb, yb, 0.044715, 1.0, ALU.mult, ALU.add, [f"yt{2 + k % 2}"], [f"yt{2 + k % 2}"])
                    tt("vector", yb, yb, ya, ALU.mult, [f"yt{2 + k % 2}", f"yt{k % 2}"], [f"yt{2 + k % 2}"])
                    act(yb, yb, AF.Sigmoid, [f"yt{2 + k % 2}"], [f"yt{2 + k % 2}"], scale=1.5957691216057308)
                    tt("vector", gT[:, k, :], ya, yb, ALU.mult, [f"yt{k % 2}", f"yt{2 + k % 2}"], [f"gT{k}"])
                steps = []
                for m in range(4):
                    def load(m=m, ti=ti):
                        s = nslot("wb", ("glu", ti, m), 6)
                        P.dma(wb[s], wglu_s[m], [], [f"wb{s}"])

                    def comp(m=m, ti=ti):
                        s = slot[("glu", ti, m)]
                        po = 5 + (m % 2)
                        for k in range(4):
                            mm(psb[po][:, :], wb[s][:, k, :], gT[:, k, :], k == 0, k == 3, [f"wb{s}", f"gT{k}"], [PSN[po]])
                        sgt = B["sg"][m % 2]
                        act(sgt, psb[po][:, :], AF.Sigmoid, [PSN[po]], [f"sg{m % 2}"], bias=bglu[:, m:m + 1])
                        tt("vector", yssm[:, m, :], gT[:, m, :], sgt, ALU.mult, [f"gT{m}", f"sg{m % 2}"], [f"yssm{m}"])
                    steps.append((load, comp))
                for m in range(8):
                    def load(m=m, ti=ti):
                        s = nslot("wb", ("ba", ti, m), 6)
                        P.dma(wb[s], wba_s[m], [], [f"wb{s}"])
                        s = nslot("wb", ("bs", ti, m), 6)
                        P.dma(wb[s], wbs_s[m], [], [f"wb{s}"])

                    def comp(m=m, ti=ti):
                        sa = slot[("ba", ti, m)]
                        ss = slot[("bs", ti, m)]
                        pa = 1 + (m % 2)
                        pss = 3 + (m % 2)
                        for k in range(4):
                            mm(psb[pa][:, :], wb[sa][:, k, :], yatt[:, k, :], k == 0, k == 3, [f"wb{sa}", "yatt"], [PSN[pa]])
                        for k in range(4):
                            mm(psb[pss][:, :], wb[ss][:, k, :], yssm[:, k, :], k == 0, k == 3, [f"wb{ss}", f"yssm{k}"], [PSN[pss]])
                        t1 = yt[m % 2]
                        t2 = yt[2 + m % 2]
                        tt("vector", t1, psb[pa][:, :], sga[:, m, :], ALU.mult, [PSN[pa], "sga"], [f"yt{m % 2}"])
                        tt("vector", t2, psb[pss][:, :], sgs[:, m, :], ALU.mult, [PSN[pss], "sgs"], [f"yt{2 + m % 2}"])
                        tt("gpsimd", merged[:, m, :], t1, t2, ALU.add, [f"yt{m % 2}", f"yt{2 + m % 2}"], [f"mg{m}"])
                    steps.append((load, comp))
                for m in range(8):
                    def load(m=m, ti=ti):
                        s = nslot("wo", ("wo", ti, m), 3)
                        P.dma(wo[s], wout_s[m], [], [f"wo{s}"])

                    def comp(m=m, ti=ti, x=x, xs=xs):
                        s = slot[("wo", ti, m)]
                        po = 5 + (m % 2)
                        for k in range(8):
                            mm(psb[po][:, :], wo[s][:, k, :], merged[:, k, :], k == 0, k == 7, [f"wo{s}", f"mg{k}"], [PSN[po]])
                        stt(x[:, m, :], psb[po][:, :], g2[:, m:m + 1], x[:, m, :], ALU.mult, ALU.add,
                            [PSN[po], f"x{xs}_{m}"], [f"x{xs}_{m}"])
                    steps.append((load, comp))
                pipeline(steps, 2)
            norm_mod(B, xs, a3, b3)
            pipeline(ffn_steps(B, xs, wgu2_s, wd2_s, g3h, ("C2", ti)), 2)
            norm_stats(B, xs)
            for k in range(8):
                stt(x[:, k, :], x[:, k, :], nfin[:, k:k + 1], B["rstd"], ALU.mult, ALU.mult,
                    [f"x{xs}_{k}", "rstd"], [f"x{xs}_{k}"])
            P.dma(outv[:, :, t0 - TO:t0 - TO + NT], x, XR(xs), [], queue="gpsimd", final=True)

    if "C" in stages:
        phase_C(("att" in stages) and ("ssm" in stages))

    P.finish()
    return nc


_CACHE = {}


def _vec8(v):
    return np.ascontiguousarray(np.asarray(v, np.float32).reshape(-1, 128).T)


def _host_inputs(inp, core):
    import ml_dtypes
    f = lambda a: np.ascontiguousarray(np.asarray(a, np.float32))
    m = {}
    b, half = core // 2, core % 2
    xb = np.asarray(inp["x"][b], np.float32)
    if half == 1:
        win = xb
    else:
        win = np.concatenate([np.zeros((TO, D), np.float32), xb[:TO]], axis=0)
    m["xT"] = np.ascontiguousarray(win.T)
    m["uflag"] = np.full((128, 1), float(half), np.float32)
    m["cT"] = _vec8(inp["c"][b])
    m["w_ada"] = f(inp["w_ada"][0])
    m["b_adaT"] = _vec8(inp["b_ada"][0])
    m["nf1"] = _vec8(inp["norm_ffn1"][0]); m["nmix"] = _vec8(inp["norm_mix"][0])
    m["nf2"] = _vec8(inp["norm_ffn2"][0]); m["nfin"] = _vec8(inp["norm_final"])
    for k in ("w_ffn1_in", "w_ffn1_out", "w_ffn2_in", "w_ffn2_out", "w_in", "w_glu", "w_br_att", "w_br_ssm", "w_out"):
        m[k] = f(inp[k][0])
    def gp(a):
        a = np.asarray(a, np.float32).reshape(16, 2, 64)
        return np.ascontiguousarray(a.transpose(1, 2, 0).reshape(128, 16))
    m["lamr"] = gp(inp["lam_re"][0]); m["lami"] = gp(inp["lam_im"][0])
    m["logdt"] = gp(np.repeat(np.asarray(inp["log_dt"][0], np.float32)[:, None], 64, axis=1))
    def bsrc(a):
        a = np.asarray(a, np.float32).reshape(16, 2, 64, 16)
        o = np.zeros((2, 64, 16, 2, 16), np.float32)
        for two in range(2):
            o[two, :, :, two, :] = a[:, two].transpose(1, 0, 2)
        return np.ascontiguousarray(o.reshape(128, 16, 32))
    m["bsrc_re"] = bsrc(inp["ssm_b_re"][0]); m["bsrc_im"] = bsrc(inp["ssm_b_im"][0])
    def csrc(a):
        a = np.asarray(a, np.float32).reshape(16, 2, 16, 64)
        o = np.zeros((2, 16, 16, 2, 64), np.float32)
        for two in range(2):
            o[two, :, :, two, :] = a[:, two].transpose(1, 0, 2)
        return np.ascontiguousarray(o.reshape(32, 16, 128))
    m["csrc_re"] = csrc(inp["ssm_c_re"][0]); m["csrc_im"] = csrc(inp["ssm_c_im"][0])
    m["dsk"] = _vec8(inp["ssm_d"][0]); m["bglu"] = _vec8(inp["b_glu"][0])
    t = np.arange(T)
    kaug = np.zeros((8, 4, T), np.float32); qaug = np.zeros((8, 4, T), np.float32)
    for h in range(8):
        sl = 2.0 ** (-(h + 1))
        kaug[h, 0] = 1.0; kaug[h, 1] = 1.0; kaug[h, 2] = sl * 256.0 * (t // 256); kaug[h, 3] = sl * (t % 256)
        qaug[h, 0] = -sl * 256.0 * (t // 256); qaug[h, 1] = -sl * (t % 256); qaug[h, 2] = 1.0; qaug[h, 3] = 1.0
    m["kaug"] = kaug.astype(ml_dtypes.bfloat16); m["qaug"] = qaug.astype(ml_dtypes.bfloat16)
    m["eind"] = (np.arange(32)[:, None] == (t // 256)[None, :]).astype(np.float32).astype(ml_dtypes.bfloat16)
    kk = np.arange(128)
    m["tri"] = np.where(kk[:, None] <= kk[None, :], 0.0, NEGM).astype(np.float32).astype(ml_dtypes.bfloat16)
    okn = np.arange(32)[None, :] < np.arange(32)[:, None]
    if half == 0:
        okn = okn & (np.arange(32)[None, :] >= 16)
    el = np.where(okn, 0.0, -1e30).astype(np.float32)
    m["elig"] = np.ascontiguousarray(np.broadcast_to(el[None], (128, 32, 32)))
    return m


STAGES = ("cast", "A", "ssm", "att", "C")


def kernel(**inputs):
    key = STAGES
    if key not in _CACHE:
        _CACHE[key] = build_program(STAGES)
    nc = _CACHE[key]
    in_maps = [_host_inputs(inputs, c) for c in range(8)]
    res = run_bass_kernel_spmd(nc, in_maps, core_ids=list(range(8)))
    out = np.empty((4, T, D), np.float32)
    for c in range(8):
        out[c // 2, (c % 2) * TO:(c % 2 + 1) * TO] = res.results[c]["outT"].T
    return out
```

```python
from contextlib import ExitStack
import numpy as np
import concourse.bass as bass
import concourse.mybir as mybir
from concourse.bass_utils import run_bass_kernel_spmd

F32 = mybir.dt.float32
BF16 = mybir.dt.bfloat16
AF = mybir.ActivationFunctionType
ALU = mybir.AluOpType
AX = mybir.AxisListType


class Prog:
    ENGINES = ["sync", "scalar", "vector", "gpsimd", "tensor"]

    def __init__(self, nc, n_dma_sems=40):
        self.nc = nc
        self.stack = ExitStack()
        self.streams = {e: [] for e in self.ENGINES}
        self.sem = {}
        for e in self.ENGINES:
            self.sem[("e", e)] = self.stack.enter_context(nc.semaphore(f"se_{e}"))
        for i in range(n_dma_sems):
            self.sem[("d", i)] = self.stack.enter_context(nc.semaphore(f"sd_{i}"))
        self.n_dma = n_dma_sems
        self.cnt = {k: 0 for k in self.sem}
        self.known = {e: {} for e in self.ENGINES}
        self.snap = {}
        self.res = {}
        self.dnext = 0
        self.finals = []
        self.nops = 0

    def sb(self, name, shape, dtype):
        return self.stack.enter_context(self.nc.sbuf_tensor(name, list(shape), dtype))

    def ps(self, name, shape, dtype):
        return self.stack.enter_context(self.nc.psum_tensor(name, list(shape), dtype))

    def _deps(self, engine, reads, writes):
        deps = {}
        def add(tok):
            if tok is None:
                return
            k, v = tok
            if engine == "tensor" and k == ("e", "tensor"):
                return
            if deps.get(k, 0) < v:
                deps[k] = v
        for r in reads:
            st = self.res.get(r)
            if st:
                add(st["w"])
        for w in writes:
            st = self.res.get(w)
            if st:
                add(st["w"])
                for t in st["r"]:
                    add(t)
        return deps

    def _emit_waits(self, engine, deps):
        kn = self.known[engine]
        for k, v in deps.items():
            if kn.get(k, 0) >= v:
                continue
            self.streams[engine].append(("w", self.sem[k], v))
            sn = self.snap.get((k, v))
            if sn:
                for kk, vv in sn.items():
                    if kn.get(kk, 0) < vv:
                        kn[kk] = vv
            kn[k] = v

    def _commit(self, engine, tok, reads, writes):
        sn = dict(self.known[engine])
        sn[tok[0]] = tok[1]
        self.snap[tok] = sn
        for r in reads:
            st = self.res.setdefault(r, {"w": None, "r": []})
            st["r"].append(tok)
        for w in writes:
            self.res[w] = {"w": tok, "r": []}
        self.nops += 1

    def op(self, engine, fn, reads=(), writes=()):
        deps = self._deps(engine, reads, writes)
        self._emit_waits(engine, deps)
        k = ("e", engine)
        self.cnt[k] += 1
        self.streams[engine].append(("o", fn, self.sem[k], 1))
        tok = (k, self.cnt[k])
        if engine != "tensor":
            pass
        self._commit(engine, tok, reads, writes)
        return tok

    def dma(self, out, in_, reads=(), writes=(), queue="sync", final=False, **kw):
        deps = self._deps(queue, reads, writes)
        idx = self.dnext
        self.dnext = (self.dnext + 1) % self.n_dma
        k = ("d", idx)
        if self.cnt[k] > 0:
            if deps.get(k, 0) < self.cnt[k]:
                deps[k] = self.cnt[k]
        self._emit_waits(queue, deps)
        self.cnt[k] += 16
        self.streams[queue].append(("o", lambda e: e.dma_start(out=out, in_=in_, **kw), self.sem[k], 16))
        tok = (k, self.cnt[k])
        self._commit(queue, tok, reads, writes)
        if final:
            self.finals.append(tok)
        return tok

    def barrier(self):
        for e in self.ENGINES:
            deps = {k: v for k, v in self.cnt.items() if v > 0}
            self._emit_waits(e, deps)
        self.res = {}

    def finish(self):
        deps = {}
        for k, v in self.finals:
            if deps.get(k, 0) < v:
                deps[k] = v
        self._emit_waits("sync", deps)
        with self.nc.Block() as block:
            for e in self.ENGINES:
                stream = self.streams[e]

                def body(eng, stream=stream):
                    for it in stream:
                        if it[0] == "w":
                            eng.wait_ge(it[1], it[2])
                        else:
                            it[1](eng).then_inc(it[2], it[3])

                getattr(block, e)(body)
        self.stack.close()


D = 1024
T = 8192
NT = 512
NTL = T // NT
DFF = 2816
FC = DFF // 128
NBLK = T // 256
TO = T // 2
OT0 = NTL // 2
NEGM = -30000.0
DBG = set()
DBG_NTL = NTL
DBG_NH = 8
DBG_NQT = 64
DBG_FLIP = 0
DBG_NPAIR = 16


class Arena:
    def __init__(self, P, name, n):
        self.t = P.sb(name, [128, n], F32)
        self.n = n
        self.off = 0

    def reset(self):
        self.off = 0

    def f32(self, n):
        ap = self.t[:, self.off:self.off + n]
        self.off += n
        assert self.off <= self.n, (self.off, self.n)
        return ap

    def bf16(self, n):
        m = (n + 1) // 2
        ap = self.t[:, self.off:self.off + m].bitcast(BF16)
        self.off += m
        assert self.off <= self.n, (self.off, self.n)
        return ap


def build_program(stages=("cast", "A", "ssm", "att", "C")):
    nc = bass.Bass("TRN2", target_bir_lowering=False)
    P = Prog(nc)

    def din(name, shape, dt=F32):
        return nc.dram_tensor(name, list(shape), dt, kind="ExternalInput").ap()

    def dscr(name, shape, dt):
        kind = "ExternalOutput" if name in DBG else "Internal"
        return nc.dram_tensor(name, list(shape), dt, kind=kind).ap()

    xT = din("xT", [D, T])
    cT = din("cT", [128, 8])
    w_ada = din("w_ada", [D, 9 * D])
    b_adaT = din("b_adaT", [128, 72])
    nvec = {n: din(n, [128, 8]) for n in ("nf1", "nmix", "nf2", "nfin")}
    w1i = din("w_ffn1_in", [D, 2 * DFF])
    w1o = din("w_ffn1_out", [DFF, D])
    w2i = din("w_ffn2_in", [D, 2 * DFF])
    w2o = din("w_ffn2_out", [DFF, D])
    w_in = din("w_in", [D, 4096])
    w_glu = din("w_glu", [512, 512])
    w_ba = din("w_br_att", [512, D])
    w_bs = din("w_br_ssm", [512, D])
    w_out = din("w_out", [D, D])
    lamr_d = din("lamr", [128, 16])
    lami_d = din("lami", [128, 16])
    logdt_d = din("logdt", [128, 16])
    bsrc_re_d = din("bsrc_re", [128, 16, 32])
    bsrc_im_d = din("bsrc_im", [128, 16, 32])
    csrc_re_d = din("csrc_re", [32, 16, 128])
    csrc_im_d = din("csrc_im", [32, 16, 128])
    dsk_d = din("dsk", [128, 4])
    bglu_d = din("bglu", [128, 4])
    kaug_d = din("kaug", [8, 4, T], BF16)
    qaug_d = din("qaug", [8, 4, T], BF16)
    eind_d = din("eind", [32, T], BF16)
    tri_d = din("tri", [128, 128], BF16)
    elig_d = din("elig", [128, NBLK, NBLK])
    uflag_d = din("uflag", [128, 1])
    outT = nc.dram_tensor("outT", [D, TO], F32, kind="ExternalOutput").ap()

    wgu1_s = dscr("wgu1_s", [2 * FC, 128, 8, 128], BF16)
    wd1_s = dscr("wd1_s", [8, 128, FC, 128], BF16)
    wgu2_s = dscr("wgu2_s", [2 * FC, 128, 8, 128], BF16)
    wd2_s = dscr("wd2_s", [8, 128, FC, 128], BF16)
    win_s = dscr("win_s", [32, 128, 8, 128], BF16)
    wba_s = dscr("wba_s", [8, 128, 4, 128], BF16)
    wbs_s = dscr("wbs_s", [8, 128, 4, 128], BF16)
    wout_s = dscr("wout_s", [8, 128, 8, 128], BF16)
    wglu_s = dscr("wglu_s", [4, 128, 4, 128], BF16)
    x1_s = dscr("x1_s", [D, T], F32)
    qT_s = dscr("qT_s", [512, T], BF16)
    kT_s = dscr("kT_s", [512, T], BF16)
    uT_s = dscr("uT_s", [512, T], BF16)
    v_s = dscr("v_s", [T, 512], BF16)
    sga_s = dscr("sga_s", [D, T], BF16)
    sgs_s = dscr("sgs_s", [D, T], BF16)
    kmean_s = dscr("kmean_s", [512, NBLK], F32)
    yattT_s = dscr("yattT_s", [512, T], BF16)
    ypre_s = dscr("ypre_s", [512, T], F32)

    ones_bf = P.sb("ones_bf", [128, 128], BF16)
    ident_bf = P.sb("ident_bf", [128, 128], BF16)
    ident_f = P.sb("ident_f", [128, 128], F32)
    eps_col = P.sb("eps_col", [128, 1], F32)
    uflag = P.sb("uflag_sb", [128, 1], F32)
    vecs = P.sb("vecs", [128, 32 + 72 + 9 * 8 + 8 + 8], F32)
    nf1 = vecs[:, 0:8]; nmix = vecs[:, 8:16]; nf2 = vecs[:, 16:24]; nfin = vecs[:, 24:32]
    adaT = vecs[:, 32:104]
    der = vecs[:, 104:176]
    a1, b1, g1h, a2, b2, g2, a3, b3, g3h = [der[:, i * 8:(i + 1) * 8] for i in range(9)]
    dsk = vecs[:, 176:180]; bglu = vecs[:, 180:184]
    kmacc = P.sb("kmacc", [128, 4, NBLK], F32)
    ar = Arena(P, "arena", 44600)
    psb = [P.ps(f"psb{i}", [128, 512], F32) for i in range(8)]
    PSN = [f"ps{i}" for i in range(8)]

    def mm(out, lhsT, rhs, start, stop, reads, writes):
        P.op("tensor", lambda e: e.matmul(out, lhsT=lhsT, rhs=rhs, start=start, stop=stop), reads, writes)

    def tr(out, in_, ident, reads, writes):
        P.op("tensor", lambda e: e.transpose(out, in_, ident), reads, writes)

    def act(out, in_, func, reads, writes, **kw):
        P.op("scalar", lambda e: e.activation(out=out, in_=in_, func=func, **kw), reads, writes)

    def ts(eng, out, in0, s1, s2, op0, op1, reads, writes):
        if op1 is None:
            P.op(eng, lambda e: e.tensor_scalar(out=out, in0=in0, scalar1=s1, scalar2=None, op0=op0), reads, writes)
        else:
            P.op(eng, lambda e: e.tensor_scalar(out=out, in0=in0, scalar1=s1, scalar2=s2, op0=op0, op1=op1), reads, writes)

    def stt(out, in0, scalar, in1, op0, op1, reads, writes):
        P.op("vector", lambda e: e.scalar_tensor_tensor(out=out, in0=in0, scalar=scalar, in1=in1, op0=op0, op1=op1), reads, writes)

    def tt(eng, out, in0, in1, op, reads, writes):
        P.op(eng, lambda e: e.tensor_tensor(out=out, in0=in0, in1=in1, op=op), reads, writes)

    def cp(eng, out, in_, reads, writes):
        if eng == "scalar":
            P.op(eng, lambda e: e.copy(out, in_), reads, writes)
        else:
            P.op(eng, lambda e: e.tensor_copy(out=out, in_=in_), reads, writes)

    def pipeline(steps, look=2):
        n = len(steps)
        for i in range(min(look, n)):
            steps[i][0]()
        for i in range(n):
            steps[i][1]()
            if i + look < n:
                steps[i + look][0]()

    P.op("gpsimd", lambda e: e.memset(ones_bf[:], 1.0), [], ["ones_bf"])
    P.op("gpsimd", lambda e: e.memset(ident_bf[:], 1.0), [], ["ident_bf"])
    P.op("gpsimd", lambda e: e.affine_select(out=ident_bf[:], in_=ident_bf[:], pattern=[[-1, 128]],
                                             compare_op=ALU.is_equal, fill=0.0, base=0, channel_multiplier=1),
         ["ident_bf"], ["ident_bf"])
    P.op("gpsimd", lambda e: e.memset(ident_f[:], 1.0), [], ["ident_f"])
    P.op("gpsimd", lambda e: e.affine_select(out=ident_f[:], in_=ident_f[:], pattern=[[-1, 128]],
                                             compare_op=ALU.is_equal, fill=0.0, base=0, channel_multiplier=1),
         ["ident_f"], ["ident_f"])
    P.op("gpsimd", lambda e: e.memset(eps_col[:], 1e-6), [], ["eps_col"])
    P.op("gpsimd", lambda e: e.memset(kmacc[:], 0.0), [], ["kmacc"])
    P.dma(nf1, nvec["nf1"], [], ["vecs"])
    P.dma(nmix, nvec["nmix"], [], ["vecs"])
    P.dma(nf2, nvec["nf2"], [], ["vecs"])
    P.dma(nfin, nvec["nfin"], [], ["vecs"])
    P.dma(dsk, dsk_d, [], ["vecs"])
    P.dma(bglu, bglu_d, [], ["vecs"])
    P.dma(uflag[:], uflag_d, [], ["uflag"])

    ar.reset()
    ct = ar.f32(8)
    sc2 = ar.f32(16).rearrange("p (k t) -> p k t", t=2)
    badaT = ar.f32(72)
    wst = [ar.f32(8 * 512).rearrange("p (k n) -> p k n", k=8) for _ in range(2)]
    P.dma(ct, cT, [], ["ct"])
    P.dma(badaT, b_adaT, [], ["badaT"])
    sgc = ar.f32(8)
    act(sgc, ct, AF.Sigmoid, ["ct"], ["sgc"])
    tt("vector", sc2[:, :, 0], ct, sgc, ALU.mult, ["ct", "sgc"], ["sc2"])
    tt("vector", sc2[:, :, 1], ct, sgc, ALU.mult, ["ct", "sgc"], ["sc2"])
    ps_ada = psb[0][:, 0:144].rearrange("p (j t) -> p j t", t=2)
    w_ada_v = w_ada.rearrange("(k p) n -> p k n", p=128)
    for sl in range(18):
        slot = sl % 2
        P.dma(wst[slot], w_ada_v[:, :, sl * 512:(sl + 1) * 512], [], [f"wst{slot}"])
        for jj in range(4):
            j = sl * 4 + jj
            for k in range(8):
                mm(ps_ada[:, j, :], wst[slot][:, k, jj * 128:(jj + 1) * 128], sc2[:, k, :], k == 0, k == 7,
                   [f"wst{slot}", "sc2"], [PSN[0]])
    tt("vector", adaT, ps_ada[:, :, 0], badaT, ALU.add, [PSN[0], "badaT", "vecs"], ["vecs"])

    def av(i):
        return adaT[:, i * 8:(i + 1) * 8]
    for (aa, bb, gg, nrm, base, gs) in ((a1, b1, g1h, nf1, 0, 0.5), (a2, b2, g2, nmix, 3, 1.0), (a3, b3, g3h, nf2, 6, 0.5)):
        stt(aa, av(base + 1), 1.0, nrm, ALU.add, ALU.mult, ["vecs"], ["vecs"])
        cp("vector", bb, av(base), ["vecs"], ["vecs"])
        ts("vector", gg, av(base + 2), gs, None, ALU.mult, None, ["vecs"], ["vecs"])
    P.barrier()

    CAST_WORDS = 2 * 2816 + 2 * 1408
    CAST_BASE = ar.n - CAST_WORDS
    stg = [ar.t[:, CAST_BASE + i * 2816:CAST_BASE + (i + 1) * 2816] for i in range(2)]
    cbf = [ar.t[:, CAST_BASE + 5632 + i * 1408:CAST_BASE + 5632 + (i + 1) * 1408].bitcast(BF16) for i in range(2)]
    cast_i = [0]

    def cast_slabs(src, dst, K, SW, ldq):
        kc = K // 128
        ncols = src.shape[1]
        srcv = src.rearrange("(k p) n -> p k n", p=128)
        assert kc * SW <= 2816
        for c0 in range(0, ncols, SW):
            def slab(c0=c0):
                i = cast_i[0]
                cast_i[0] += 1
                sl = i % 2
                s_ = stg[sl][:, 0:kc * SW].rearrange("p (k n) -> p k n", k=kc)
                b_ = cbf[sl][:, 0:kc * SW].rearrange("p (k n) -> p k n", k=kc)
                P.dma(s_, srcv[:, :, c0:c0 + SW], [], [f"stg{sl}"], queue=ldq)
                cp(("scalar", "vector")[i % 2], b_, s_, [f"stg{sl}"], [f"cbf{sl}"])
                for cc in range(SW // 128):
                    P.dma(dst[c0 // 128 + cc], b_[:, :, cc * 128:(cc + 1) * 128], [f"cbf{sl}"], [], queue="gpsimd")
            yield slab

    deferred = []
    if "cast" in stages:
        for sl_ in cast_slabs(w1i, wgu1_s, D, 256, "sync"):
            sl_()
        for sl_ in cast_slabs(w1o, wd1_s, DFF, 128, "sync"):
            sl_()
        for sl_ in cast_slabs(w_in, win_s, D, 256, "sync"):
            sl_()
        for (src_, dst_, K_, SW_) in ((w_glu, wglu_s, 512, 512), (w_ba, wba_s, 512, 512), (w_bs, wbs_s, 512, 512),
                                      (w_out, wout_s, D, 256), (w2i, wgu2_s, D, 256), (w2o, wd2_s, DFF, 128)):
            for sl_ in cast_slabs(src_, dst_, K_, SW_, "sync"):
                sl_()
        P.barrier()

    def alloc_common(nwd=3):
        B = {}
        B["x"] = [ar.f32(8 * NT).rearrange("p (k t) -> p k t", k=8) for _ in range(2)]
        B["h"] = ar.bf16(8 * NT).rearrange("p (k t) -> p k t", k=8)
        B["act"] = ar.bf16(FC * NT).rearrange("p (k t) -> p k t", k=FC)
        B["sq"] = ar.bf16(8 * NT).rearrange("p (k t) -> p k t", k=8)
        B["tmp"] = [ar.f32(NT) for _ in range(2)]
        B["rstd"] = ar.f32(NT)
        B["sd"] = ar.f32(NT)
        B["sg"] = [ar.f32(NT) for _ in range(2)]
        B["wg"] = [ar.bf16(1024).rearrange("p (k j) -> p k j", k=8) for _ in range(3)]
        B["wu"] = [ar.bf16(1024).rearrange("p (k j) -> p k j", k=8) for _ in range(3)]
        B["wd"] = [ar.bf16(FC * 128).rearrange("p (k j) -> p k j", k=FC) for _ in range(nwd)]
        return B

    cnt = {}
    slot = {}

    def nslot(cls, key, n):
        s = cnt.get(cls, 0) % n
        cnt[cls] = cnt.get(cls, 0) + 1
        slot[key] = s
        return s

    def norm_stats(B, xs):
        x = B["x"][xs]
        for k in range(8):
            act(B["sq"][:, k, :], x[:, k, :], AF.Square, [f"x{xs}_{k}"], [f"sq{k}"])
        for k in range(8):
            mm(psb[0][:, :], ones_bf[:], B["sq"][:, k, :], k == 0, k == 7, [f"sq{k}"], [PSN[0]])
        act(B["sd"], psb[0][:, :], AF.Sqrt, [PSN[0]], ["sd"], bias=eps_col[:, 0:1], scale=1.0 / D)
        P.op("vector", lambda e: e.reciprocal(out=B["rstd"], in_=B["sd"]), ["sd"], ["rstd"])

    def norm_mod(B, xs, a, b):
        x = B["x"][xs]
        norm_stats(B, xs)
        for k in range(8):
            t = B["tmp"][k % 2]
            tt("vector", t, x[:, k, :], B["rstd"], ALU.mult, [f"x{xs}_{k}", "rstd"], [f"tmp{k % 2}"])
            act(B["h"][:, k, :], t, AF.Identity, [f"tmp{k % 2}"], [f"h{k}"], bias=b[:, k:k + 1], scale=a[:, k:k + 1])

    def ffn_steps(B, xs, wgu_s, wd_s, gcol, tag):
        steps = []
        x = B["x"][xs]
        for c in range(FC):
            def load(c=c):
                s = nslot("gu", (tag, "gu", c), 3)
                P.dma(B["wg"][s], wgu_s[c], [], [f"wg{s}"])
                P.dma(B["wu"][s], wgu_s[FC + c], [], [f"wu{s}"])

            def comp(c=c):
                s = slot[(tag, "gu", c)]
                pg = 1 + (c % 2)
                pu = 3 + (c % 2)
                for k in range(8):
                    mm(psb[pg][:, :], B["wg"][s][:, k, :], B["h"][:, k, :], k == 0, k == 7, [f"wg{s}", f"h{k}"], [PSN[pg]])
                for k in range(8):
                    mm(psb[pu][:, :], B["wu"][s][:, k, :], B["h"][:, k, :], k == 0, k == 7, [f"wu{s}", f"h{k}"], [PSN[pu]])
                sg = B["sg"][c % 2]
                act(sg, psb[pg][:, :], AF.Silu, [PSN[pg]], [f"sg{c % 2}"])
                tt("vector", B["act"][:, c, :], sg, psb[pu][:, :], ALU.mult, [f"sg{c % 2}", PSN[pu]], [f"act{c}"])
            steps.append((load, comp))
        for m in range(8):
            def load(m=m):
                s = nslot("wd", (tag, "wd", m), len(B["wd"]))
                P.dma(B["wd"][s], wd_s[m], [], [f"wd{s}"])

            def comp(m=m):
                s = slot[(tag, "wd", m)]
                po = 5 + (m % 2)
                for k in range(FC):
                    mm(psb[po][:, :], B["wd"][s][:, k, :], B["act"][:, k, :], k == 0, k == FC - 1, [f"wd{s}", f"act{k}"], [PSN[po]])
                stt(x[:, m, :], psb[po][:, :], gcol[:, m:m + 1], x[:, m, :], ALU.mult, ALU.add,
                    [PSN[po], f"x{xs}_{m}"], [f"x{xs}_{m}"])
            steps.append((load, comp))
        return steps

    xTv = xT.rearrange("(k p) t -> p k t", p=128)
    x1v = x1_s.rearrange("(k p) t -> p k t", p=128)
    XR = lambda xs: [f"x{xs}_{k}" for k in range(8)]

    if "A" in stages:
        ar.reset()
        B = alloc_common()
        win = [ar.bf16(1024).rearrange("p (k j) -> p k j", k=8) for _ in range(3)]
        wv = ar.bf16(4096).rearrange("p (c k j) -> p c k j", c=4, k=8)
        ost = [ar.bf16(NT) for _ in range(4)]
        vst = [ar.bf16(512) for _ in range(2)]
        assert ar.off <= CAST_BASE, (ar.off, CAST_BASE)
        P.dma(B["x"][0], xTv[:, :, 0:NT], [], XR(0))
        for ti in range(DBG_NTL):
            xs = ti % 2
            t0 = ti * NT
            if ti + 1 < NTL:
                P.dma(B["x"][1 - xs], xTv[:, :, t0 + NT:t0 + 2 * NT], [], XR(1 - xs))
            norm_mod(B, xs, a1, b1)
            pipeline(ffn_steps(B, xs, wgu1_s, wd1_s, g1h, ("A1", ti)), 2)
            prefix = ti < OT0
            if not prefix:
                P.dma(x1v[:, :, t0:t0 + NT], B["x"][xs], XR(xs), [], queue="gpsimd")
            norm_mod(B, xs, a2, b2)
            steps = []
            if prefix:
                order = list(range(4, 8)) + ["v"] + list(range(12, 16))
            else:
                order = list(range(0, 8)) + ["v"] + list(range(12, 32))
            for ci, c in enumerate(order):
                if c == "v":
                    def load():
                        for cc in range(4):
                            P.dma(wv[:, cc], win_s[8 + cc], [], ["wv"] if cc == 0 else [f"wv_{cc}"])

                    def comp(ti=ti, t0=t0):
                        for q4 in range(4):
                            for k in range(8):
                                mm(psb[7][:, :].rearrange("p (c j) -> p c j", c=4), B["h"][:, k, q4 * 128:(q4 + 1) * 128],
                                   wv[:, :, k, :], k == 0, k == 7, ["wv", "wv_1", "wv_2", "wv_3", f"h{k}"], [PSN[7]])
                            s = nslot("vst", None, 2)
                            cp("vector", vst[s], psb[7][:, :], [PSN[7]], [f"vst{s}"])
                            P.dma(v_s[t0 + q4 * 128:t0 + (q4 + 1) * 128, :], vst[s], [f"vst{s}"], [], queue="gpsimd")
                    steps.append((load, comp))
                    continue

                def load(c=c, ti=ti):
                    s = nslot("win", ("A", ti, c), 3)
                    P.dma(win[s], win_s[c], [], [f"win{s}"])

                def comp(c=c, ci=ci, ti=ti, t0=t0, prefix=prefix):
                    s = slot[("A", ti, c)]
                    po = 5 + (ci % 2)
                    for k in range(8):
                        mm(psb[po][:, :], win[s][:, k, :], B["h"][:, k, :], k == 0, k == 7, [f"win{s}", f"h{k}"], [PSN[po]])
                    o = nslot("ost", None, 4)
                    if c < 4:
                        act(ost[o], psb[po][:, :], AF.Identity, [PSN[po]], [f"ost{o}"], scale=0.125)
                        dst = qT_s[c * 128:(c + 1) * 128, t0:t0 + NT]
                    elif c < 8:
                        cp("vector", ost[o], psb[po][:, :], [PSN[po]], [f"ost{o}"])
                        P.op("vector", lambda e: e.tensor_reduce(out=kmacc[:, c - 4, 2 * ti:2 * ti + 2],
                                                                 in_=psb[po][:, :].rearrange("p (b t) -> p b t", b=2),
                                                                 axis=AX.X, op=ALU.add), [PSN[po]], ["kmacc"])
                        dst = kT_s[(c - 4) * 128:(c - 3) * 128, t0:t0 + NT]
                    elif c < 16:
                        if prefix:
                            act(ost[o], psb[po][:, :], AF.Identity, [PSN[po]], [f"ost{o}"], scale=uflag[:, 0:1])
                        else:
                            act(ost[o], psb[po][:, :], AF.Identity, [PSN[po]], [f"ost{o}"])
                        dst = uT_s[(c - 12) * 128:(c - 11) * 128, t0:t0 + NT]
                    elif c < 24:
                        act(ost[o], psb[po][:, :], AF.Sigmoid, [PSN[po]], [f"ost{o}"])
                        dst = sga_s[(c - 16) * 128:(c - 15) * 128, t0:t0 + NT]
                    else:
                        act(ost[o], psb[po][:, :], AF.Sigmoid, [PSN[po]], [f"ost{o}"])
                        dst = sgs_s[(c - 24) * 128:(c - 23) * 128, t0:t0 + NT]
                    P.dma(dst, ost[o], [f"ost{o}"], [], queue="gpsimd")
                steps.append((load, comp))
            pipeline(steps, 2)
            for _ in range(3):
                if deferred:
                    deferred.pop(0)()
        while deferred:
            deferred.pop(0)()
        ts("vector", kmacc[:], kmacc[:], 1.0 / 256, None, ALU.mult, None, ["kmacc"], ["kmacc"])
        P.dma(kmean_s.rearrange("(c p) n -> p c n", p=128), kmacc[:], ["kmacc"], [])
        P.barrier()


    ssm_gen = None
    if "ssm" in stages:
        ar.reset()
        hpi = ar.f32(1)
        P.op("gpsimd", lambda e: e.memset(hpi, float(np.pi / 2)), [], ["sm"])
        SM = {}
        def smt(name, n=16):
            SM[name] = ar.f32(n)
            return SM[name]
        for nm in ("lr", "li", "ld", "dt", "mag", "ang", "c", "s", "t1", "t2", "t3", "abr", "abi", "nr", "den", "fre", "fim", "nfim"):
            smt(nm)
        AR = ar.f32(13 * 16).rearrange("p (k g) -> p k g", k=13)
        AI = ar.f32(13 * 16).rearrange("p (k g) -> p k g", k=13)
        NAI = ar.f32(13 * 16).rearrange("p (k g) -> p k g", k=13)
        UR = ar.f32(9 * 16).rearrange("p (k g) -> p k g", k=9)
        UI = ar.f32(9 * 16).rearrange("p (k g) -> p k g", k=9)
        NUI = ar.f32(9 * 16).rearrange("p (k g) -> p k g", k=9)
        R = ["sm"]
        P.dma(SM["lr"], lamr_d, [], R)
        P.dma(SM["li"], lami_d, [], R)
        P.dma(SM["ld"], logdt_d, [], R)
        act(SM["dt"], SM["ld"], AF.Exp, R, R)
        tt("vector", SM["t1"], SM["lr"], SM["dt"], ALU.mult, R, R)
        act(SM["mag"], SM["t1"], AF.Exp, R, R)
        tt("vector", SM["ang"], SM["li"], SM["dt"], ALU.mult, R, R)
        act(SM["s"], SM["ang"], AF.Sin, R, R, scale=1.0 / 16)
        act(SM["c"], SM["ang"], AF.Sin, R, R, scale=1.0 / 16, bias=hpi)
        for _ in range(4):
            tt("vector", SM["t1"], SM["c"], SM["c"], ALU.mult, R, R)
            tt("vector", SM["t2"], SM["s"], SM["s"], ALU.mult, R, R)
            tt("vector", SM["t3"], SM["c"], SM["s"], ALU.mult, R, R)
            tt("vector", SM["c"], SM["t1"], SM["t2"], ALU.subtract, R, R)
            ts("vector", SM["s"], SM["t3"], 2.0, None, ALU.mult, None, R, R)
        cp("vector", UR[:, 0, :], SM["c"], R, R)
        cp("vector", UI[:, 0, :], SM["s"], R, R)
        for k in range(8):
            tt("vector", SM["t1"], UR[:, k, :], UR[:, k, :], ALU.mult, R, R)
            tt("vector", SM["t2"], UI[:, k, :], UI[:, k, :], ALU.mult, R, R)
            tt("vector", SM["t3"], UR[:, k, :], UI[:, k, :], ALU.mult, R, R)
            tt("vector", UR[:, k + 1, :], SM["t1"], SM["t2"], ALU.subtract, R, R)
            ts("vector", UI[:, k + 1, :], SM["t3"], 2.0, None, ALU.mult, None, R, R)
        ts("vector", NUI.rearrange("p k g -> p (k g)"), UI.rearrange("p k g -> p (k g)"), -1.0, None, ALU.mult, None, R, R)
        tt("vector", AR[:, 0, :], SM["mag"], SM["c"], ALU.mult, R, R)
        tt("vector", AI[:, 0, :], SM["mag"], SM["s"], ALU.mult, R, R)
        for k in range(12):
            tt("vector", SM["t1"], AR[:, k, :], AR[:, k, :], ALU.mult, R, R)
            tt("vector", SM["t2"], AI[:, k, :], AI[:, k, :], ALU.mult, R, R)
            tt("vector", SM["t3"], AR[:, k, :], AI[:, k, :], ALU.mult, R, R)
            tt("vector", AR[:, k + 1, :], SM["t1"], SM["t2"], ALU.subtract, R, R)
            ts("vector", AI[:, k + 1, :], SM["t3"], 2.0, None, ALU.mult, None, R, R)
        ts("vector", NAI.rearrange("p k g -> p (k g)"), AI.rearrange("p k g -> p (k g)"), -1.0, None, ALU.mult, None, R, R)
        ts("vector", SM["nr"], AR[:, 0, :], -1.0, None, ALU.add, None, R, R)
        tt("vector", SM["t1"], SM["lr"], SM["lr"], ALU.mult, R, R)
        tt("vector", SM["t2"], SM["li"], SM["li"], ALU.mult, R, R)
        tt("vector", SM["den"], SM["t1"], SM["t2"], ALU.add, R, R)
        P.op("vector", lambda e: e.reciprocal(out=SM["den"], in_=SM["den"]), R, R)
        tt("vector", SM["t1"], SM["nr"], SM["lr"], ALU.mult, R, R)
        tt("vector", SM["t2"], AI[:, 0, :], SM["li"], ALU.mult, R, R)
        tt("vector", SM["t1"], SM["t1"], SM["t2"], ALU.add, R, R)
        tt("vector", SM["fre"], SM["t1"], SM["den"], ALU.mult, R, R)
        tt("vector", SM["t1"], AI[:, 0, :], SM["lr"], ALU.mult, R, R)
        tt("vector", SM["t2"], SM["nr"], SM["li"], ALU.mult, R, R)
        tt("vector", SM["t1"], SM["t1"], SM["t2"], ALU.subtract, R, R)
        tt("vector", SM["fim"], SM["t1"], SM["den"], ALU.mult, R, R)
        BTre = ar.bf16(16 * 128).rearrange("p (q j) -> p q j", q=16)
        BTim = ar.bf16(16 * 128).rearrange("p (q j) -> p q j", q=16)
        CTre = ar.f32(16 * 32).rearrange("p (g c) -> p g c", g=16)
        CTimn = ar.f32(16 * 32).rearrange("p (g c) -> p g c", g=16)
        CTren = ar.f32(16 * 32).rearrange("p (g c) -> p g c", g=16)
        ssm_mark = ar.off
        bre = ar.f32(16 * 32).rearrange("p (g c) -> p g c", g=16)
        bim = ar.f32(16 * 32).rearrange("p (g c) -> p g c", g=16)
        btmp = ar.f32(32)
        bbre = ar.bf16(16 * 32).rearrange("p (g c) -> p g c", g=16)
        bbim = ar.bf16(16 * 32).rearrange("p (g c) -> p g c", g=16)
        csr = ar.f32(16 * 128).rearrange("p (g j) -> p g j", g=16)
        csi = ar.f32(16 * 128).rearrange("p (g j) -> p g j", g=16)
        P.dma(bre, bsrc_re_d, [], ["bre"])
        P.dma(bim, bsrc_im_d, [], ["bim"])
        P.dma(csr[0:32], csrc_re_d, [], ["csr"])
        P.dma(csi[0:32], csrc_im_d, [], ["csi"])
        for g in range(16):
            ts("vector", btmp, bim[:, g, :], SM["fim"][:, g:g + 1], None, ALU.mult, None, ["bim"] + R, ["btmp"])
            stt(bbre[:, g, :], bre[:, g, :], SM["fre"][:, g:g + 1], btmp, ALU.mult, ALU.subtract, ["bre", "btmp"] + R, ["bbre"])
            ts("vector", btmp, bre[:, g, :], SM["fim"][:, g:g + 1], None, ALU.mult, None, ["bre"] + R, ["btmp"])
            stt(bbim[:, g, :], bim[:, g, :], SM["fre"][:, g:g + 1], btmp, ALU.mult, ALU.add, ["bim", "btmp"] + R, ["bbim"])
        psbf = [psb[i][:, :].bitcast(BF16) for i in range(8)]
        for g in range(16):
            tr(psbf[0][0:32, 0:128], bbre[:, g, :], ident_bf[:], ["bbre"], [PSN[0]])
            tr(psbf[0][0:32, 128:256], bbim[:, g, :], ident_bf[:], ["bbim"], [PSN[0]])
            cp("vector", BTre[0:32, g, :], psbf[0][0:32, 0:128], [PSN[0]], ["BT"])
            cp("vector", BTim[0:32, g, :], psbf[0][0:32, 128:256], [PSN[0]], ["BT"])
        for g in range(16):
            tr(psb[1][:, 0:32], csr[0:32, g, :], ident_f[0:32, 0:32], ["csr"], [PSN[1]])
            tr(psb[1][:, 32:64], csi[0:32, g, :], ident_f[0:32, 0:32], ["csi"], [PSN[1]])
            cp("vector", CTre[:, g, :], psb[1][:, 0:32], [PSN[1]], ["CT"])
            ts("vector", CTimn[:, g, :], psb[1][:, 32:64], -1.0, None, ALU.mult, None, [PSN[1]], ["CT"])
            ts("vector", CTren[:, g, :], psb[1][:, 0:32], -1.0, None, ALU.mult, None, [PSN[1]], ["CT"])
        P.barrier()
        ar.off = ssm_mark
        H = TO
        LC = 256
        NCH = H // LC
        ZTr, ZTi, XTr, XTi = ar.f32(H), ar.f32(H), ar.f32(H), ar.f32(H)
        up1 = ar.bf16(T)
        yst = [ar.f32(512) for _ in range(2)]
        Dc = [ar.f32(512) for _ in range(2)]
        Ds = [ar.f32(512) for _ in range(2)]
        dtmp = ar.f32(128)
        ptm = [ar.f32(512) for _ in range(4)]
        ones_l = ar.f32(LC)
        rtile = ar.f32(LC)
        ini = ar.f32(4)
        Gs = ar.f32(2)
        P.op("gpsimd", lambda e: e.memset(ones_l, 1.0), [], ["ones_l"])
        ZB_R, ZB_I, YB = 5, 6, 7

        def bn(pref, lo, hi):
            return [f"{pref}{b}" for b in range(lo // 512, (hi + 511) // 512)]

        def build_D(g):
            d = g % 2
            dc, ds_ = Dc[d], Ds[d]
            rn = [f"D{d}"]
            P.op("vector", lambda e: e.memset(dc[:, 0:1], 1.0), rn, rn)
            P.op("vector", lambda e: e.memset(ds_[:, 0:1], 0.0), rn, rn)
            for k in range(8):
                n = 1 << k
                ur, ui, nui = UR[:, k, g:g + 1], UI[:, k, g:g + 1], NUI[:, k, g:g + 1]
                ts("vector", dtmp[:, 0:n], ds_[:, 0:n], nui, None, ALU.mult, None, rn + R, ["dtmp"])
                ts("vector", dc[:, n:2 * n], dc[:, 0:n], ur, None, ALU.mult, None, rn + R, rn)
                tt("vector", dc[:, n:2 * n], dc[:, n:2 * n], dtmp[:, 0:n], ALU.add, rn + ["dtmp"], rn)
                ts("vector", dtmp[:, 0:n], ds_[:, 0:n], ur, None, ALU.mult, None, rn + R, ["dtmp"])
                ts("vector", ds_[:, n:2 * n], dc[:, 0:n], ui, None, ALU.mult, None, rn + R, rn)
                tt("vector", ds_[:, n:2 * n], ds_[:, n:2 * n], dtmp[:, 0:n], ALU.add, rn + ["dtmp"], rn)
            cp("vector", dc[:, 256:512], dc[:, 0:256], rn, rn)
            cp("vector", ds_[:, 256:512], ds_[:, 0:256], rn, rn)

        def ssm_main():
            build_D(0)
            yield
            for g in range(DBG_NPAIR):
                d = g % 2
                dc, ds_ = Dc[d], Ds[d]
                DN = [f"D{d}"]
                P.dma(up1[0:32, :], uT_s[g * 32:(g + 1) * 32, :], [], ["up0"])
                if g + 1 < 16:
                    build_D(g + 1)
                ts("vector", rtile, ones_l, SM["mag"][:, g:g + 1], None, ALU.mult, None, ["ones_l"] + R, ["rtile"])
                yield
                for blk in range(8):
                    cs = slice(blk * 512, (blk + 1) * 512)
                    mm(psb[ZB_R][:, :], BTre[0:32, g, :], up1[0:32, cs], True, True, ["up0", "BT"], [PSN[ZB_R]])
                    mm(psb[ZB_I][:, :], BTim[0:32, g, :], up1[0:32, cs], True, True, ["up0", "BT"], [PSN[ZB_I]])
                    cp("scalar", XTr[:, cs], psb[ZB_R][:, :], [PSN[ZB_R]], [f"xtr{blk}"])
                    cp("scalar", XTi[:, cs], psb[ZB_I][:, :], [PSN[ZB_I]], [f"xti{blk}"])
                    yield
                tb = [(XTr, XTi, "xtr", "xti"), (ZTr, ZTi, "ztr", "zti")]
                n = H
                for k in range(12):
                    sr, si, pr_, pi2 = tb[k % 2]
                    dr, di, qr_, qi2 = tb[(k + 1) % 2]
                    m = n // 2
                    svr = sr[:, 0:n].rearrange("p (j two) -> p j two", two=2)
                    svi = si[:, 0:n].rearrange("p (j two) -> p j two", two=2)
                    srcn = bn(pr_, 0, n) + bn(pi2, 0, n)
                    stt(dr[:, 0:m], svi[:, :, 0], NAI[:, k, g:g + 1], svr[:, :, 1], ALU.mult, ALU.add, srcn + R, bn(qr_, 0, m))
                    stt(dr[:, 0:m], svr[:, :, 0], AR[:, k, g:g + 1], dr[:, 0:m], ALU.mult, ALU.add, srcn + bn(qr_, 0, m) + R, bn(qr_, 0, m))
                    stt(di[:, 0:m], svr[:, :, 0], AI[:, k, g:g + 1], svi[:, :, 1], ALU.mult, ALU.add, srcn + R, bn(qi2, 0, m))
                    stt(di[:, 0:m], svi[:, :, 0], AR[:, k, g:g + 1], di[:, 0:m], ALU.mult, ALU.add, srcn + bn(qi2, 0, m) + R, bn(qi2, 0, m))
                    n = m
                    if k < 4:
                        yield
                cp("vector", Gs[:, 0:1], XTr[:, 0:1], ["xtr0"], ["Gs"])
                cp("vector", Gs[:, 1:2], XTi[:, 0:1], ["xti0"], ["Gs"])
                Gr, Gi = Gs[:, 0:1], Gs[:, 1:2]
                yield
                for ob in range(8):
                    blk = 8 + ob
                    cs = slice(blk * 512, (blk + 1) * 512)
                    co = slice(ob * 512, (ob + 1) * 512)
                    mm(psb[ZB_R][:, :], BTre[0:32, g, :], up1[0:32, cs], True, True, ["up0", "BT"], [PSN[ZB_R]])
                    mm(psb[ZB_I][:, :], BTim[0:32, g, :], up1[0:32, cs], True, True, ["up0", "BT"], [PSN[ZB_I]])
                    t1, t2 = ptm[0], ptm[1]
                    tt("vector", ZTr[:, co], psb[ZB_R][:, :], dc, ALU.mult, [PSN[ZB_R]] + DN, [f"ztr{ob}"])
                    tt("vector", t1, psb[ZB_I][:, :], ds_, ALU.mult, [PSN[ZB_I]] + DN, ["ptm0"])
                    tt("vector", ZTi[:, co], psb[ZB_I][:, :], dc, ALU.mult, [PSN[ZB_I]] + DN, [f"zti{ob}"])
                    tt("vector", t2, psb[ZB_R][:, :], ds_, ALU.mult, [PSN[ZB_R]] + DN, ["ptm1"])
                    tt("vector", ZTr[:, co], ZTr[:, co], t1, ALU.add, [f"ztr{ob}", "ptm0"], [f"ztr{ob}"])
                    tt("vector", ZTi[:, co], ZTi[:, co], t2, ALU.subtract, [f"zti{ob}", "ptm1"], [f"zti{ob}"])
                    yield
                stt(ZTr[:, 0:1], Gi, NAI[:, 0, g:g + 1], ZTr[:, 0:1], ALU.mult, ALU.add, ["Gs", "ztr0"] + R, ["ztr0"])
                stt(ZTr[:, 0:1], Gr, AR[:, 0, g:g + 1], ZTr[:, 0:1], ALU.mult, ALU.add, ["Gs", "ztr0"] + R, ["ztr0"])
                stt(ZTi[:, 0:1], Gr, AI[:, 0, g:g + 1], ZTi[:, 0:1], ALU.mult, ALU.add, ["Gs", "zti0"] + R, ["zti0"])
                stt(ZTi[:, 0:1], Gi, AR[:, 0, g:g + 1], ZTi[:, 0:1], ALU.mult, ALU.add, ["Gs", "zti0"] + R, ["zti0"])
                for c in range(NCH):
                    cc = slice(c * LC, (c + 1) * LC)
                    ob = c // 2
                    if c == 0:
                        i_r, i_i = 0.0, 0.0
                    else:
                        er, ei = XTr[:, c * LC - 1:c * LC], XTi[:, c * LC - 1:c * LC]
                        ul_r, ul_i, nul_i = UR[:, 8, g:g + 1], UI[:, 8, g:g + 1], NUI[:, 8, g:g + 1]
                        ts("vector", ini[:, 2:3], er, ul_r, None, ALU.mult, None, [f"xtr{(c - 1) // 2}"] + R, ["ini_t"])
                        stt(ini[:, 0:1], ei, nul_i, ini[:, 2:3], ALU.mult, ALU.add, [f"xti{(c - 1) // 2}", "ini_t"] + R, ["ini_r"])
                        ts("vector", ini[:, 3:4], ei, ul_r, None, ALU.mult, None, [f"xti{(c - 1) // 2}"] + R, ["ini_u"])
                        stt(ini[:, 1:2], er, ul_i, ini[:, 3:4], ALU.mult, ALU.add, [f"xtr{(c - 1) // 2}", "ini_u"] + R, ["ini_i"])
                        i_r, i_i = ini[:, 0:1], ini[:, 1:2]
                    P.op("vector", lambda e, cc=cc, i_r=i_r: e.tensor_tensor_scan(out=XTr[:, cc], data0=rtile, data1=ZTr[:, cc], initial=i_r,
                                                                              op0=ALU.mult, op1=ALU.add),
                         [f"ztr{ob}", "rtile", "ini_r"], [f"xtr{ob}"])
                    P.op("vector", lambda e, cc=cc, i_i=i_i: e.tensor_tensor_scan(out=XTi[:, cc], data0=rtile, data1=ZTi[:, cc], initial=i_i,
                                                                              op0=ALU.mult, op1=ALU.add),
                         [f"zti{ob}", "rtile", "ini_i"], [f"xti{ob}"])
                    if c % 2 == 1:
                        yield
                pend = None
                for ob in range(8):
                    co = slice(ob * 512, (ob + 1) * 512)
                    t1, t2 = ptm[2 + ob % 2], ptm[ob % 2]
                    n1, n2 = f"ptm{2 + ob % 2}", f"ptm{ob % 2}"
                    tt("vector", ZTr[:, co], XTr[:, co], dc, ALU.mult, [f"xtr{ob}"] + DN, [f"ztr{ob}"])
                    tt("vector", t1, XTi[:, co], ds_, ALU.mult, [f"xti{ob}"] + DN, [n1])
                    tt("vector", t2, XTr[:, co], ds_, ALU.mult, [f"xtr{ob}"] + DN, [n2])
                    tt("vector", ZTi[:, co], XTi[:, co], dc, ALU.mult, [f"xti{ob}"] + DN, [f"zti{ob}"])

                    def ymm(ob=ob, co=co, t1=t1, t2=t2, n1=n1, n2=n2, g=g):
                        mm(psb[YB][0:32, :], CTre[:, g, :], ZTr[:, co], True, False, [f"ztr{ob}", "CT"], [PSN[YB]])
                        mm(psb[YB][0:32, :], CTren[:, g, :], t1, False, False, [n1, "CT"], [PSN[YB]])
                        mm(psb[YB][0:32, :], CTimn[:, g, :], t2, False, False, [n2, "CT"], [PSN[YB]])
                        mm(psb[YB][0:32, :], CTimn[:, g, :], ZTi[:, co], False, True, [f"zti{ob}", "CT"], [PSN[YB]])
                        ys = nslot("yst", None, 2)
                        cp("scalar", yst[ys][0:32, :], psb[YB][0:32, :], [PSN[YB]], [f"yst{ys}"])
                        P.dma(ypre_s[g * 32:(g + 1) * 32, TO + ob * 512:TO + (ob + 1) * 512], yst[ys][0:32, :], [f"yst{ys}"], [], queue="gpsimd")
                    if pend is not None:
                        pend()
                    pend = ymm
                    yield
                pend()
                yield

        ssm_gen = ssm_main()
        if "att" not in stages:
            for _ in ssm_gen:
                pass
            P.barrier()

    if "att" in stages:
        if ssm_gen is None:
            ar.reset()
        Kaug = ar.bf16(T)
        Qaug = ar.bf16(TO)
        Vh = ar.bf16(64 * 66).rearrange("p (t d) -> p t d", t=64)
        kmf = ar.f32(32)
        kmb = ar.bf16(32)
        elig = ar.f32(32 * 32).rearrange("p (o n) -> p o n", o=32)
        tri = ar.bf16(128)
        gsc = [ar.f32(32) for _ in range(2)]
        top8 = [ar.f32(8) for _ in range(2)]
        thr = [ar.f32(2) for _ in range(2)]
        mb = [ar.bf16(32) for _ in range(2)]
        pT = [ar.bf16(512) for _ in range(3)]
        rs = [ar.f32(2) for _ in range(2)]
        yh = [ar.bf16(64) for _ in range(2)]
        yattT = ar.bf16(TO)
        psbf = [psb[i][:, :].bitcast(BF16) for i in range(8)]
        P.dma(Kaug[64:96, :], eind_d, [], ["Kind"])
        P.dma(tri, tri_d, [], ["tri"])
        P.dma(elig, elig_d, [], ["elig"])
        P.op("gpsimd", lambda e: e.memset(Vh[:, :, 64:66], 1.0), [], ["Vones"])
        SB = [1, 2]
        B0 = "psB0"
        tick_n = [0]

        def tick():
            tick_n[0] += 1
            if ssm_gen is not None and tick_n[0] % 4 == 0:
                next(ssm_gen, None)

        for h in range(DBG_NH):
            P.dma(Kaug[0:64, :], kT_s[h * 64:(h + 1) * 64, :], [], ["Kaug"])
            P.dma(Kaug[96:100, :], kaug_d[h], [], ["Kaug2"])
            P.dma(Qaug[0:64, :], qT_s[h * 64:(h + 1) * 64, TO:], [], ["Qaug"])
            P.dma(Qaug[96:100, :], qaug_d[h][:, TO:], [], ["Qaug2"])
            vsrc = v_s[:, h * 64:(h + 1) * 64].rearrange("(t p) d -> p t d", p=128)
            for v4 in range(4):
                P.dma(Vh[:, v4 * 16:(v4 + 1) * 16, 0:64], vsrc[:, v4 * 16:(v4 + 1) * 16, :], [], ["Vh"] if v4 == 0 else [f"Vh{v4}"])
            P.dma(kmf[0:64, :], kmean_s[h * 64:(h + 1) * 64, :], [], ["kmf"])
            cp("vector", kmb[0:64, :], kmf[0:64, :], ["kmf"], ["kmb"])
            KR = ["Kaug", "Kaug2", "Kind"]
            QR = ["Qaug", "Qaug2"]
            VR = ["Vh", "Vh1", "Vh2", "Vh3", "Vones"]

            def gate1(qt):
                own = qt // 2
                qc = slice((qt - 32) * 128, (qt - 31) * 128)
                p = qt % 2
                mm(psb[0][:, p * 32:(p + 1) * 32], Qaug[0:64, qc], kmb[0:64, :], True, True, ["Qaug", "kmb"], [B0])
                tt("vector", gsc[p], psb[0][:, p * 32:(p + 1) * 32], elig[:, own, :], ALU.add, [B0, "elig"], [f"g2{p}"])
                P.op("vector", lambda e: e.max(out=top8[p], in_=gsc[p]), [f"g2{p}"], [f"top8{p}"])
                ts("vector", thr[p][:, 0:1], top8[p][:, 2:3], -1e29, None, ALU.max, None, [f"top8{p}"], [f"thr{p}"])
                ts("vector", mb[p], gsc[p], thr[p][:, 0:1], NEGM, ALU.is_lt, ALU.mult, [f"g2{p}", f"thr{p}"], [f"mb{p}"])
                P.op("vector", lambda e: e.memset(mb[p][:, own:own + 1], 0.0), [f"mb{p}"], [f"mb{p}"])

            def gate2(qt):
                qc = slice((qt - 32) * 128, (qt - 31) * 128)
                p = qt % 2
                tr(psbf[0][64:96, 256 + p * 128:256 + (p + 1) * 128], mb[p], ident_bf[:], [f"mb{p}"], [B0])
                cp("scalar", Qaug[64:96, qc], psbf[0][64:96, 256 + p * 128:256 + (p + 1) * 128], [B0], [f"qm{qt}"])

            gate1(32)
            gate2(32)
            sctr = 0
            for qt in range(32, 64):
                own = qt // 2
                i = qt % 2
                qc = slice((qt - 32) * 128, (qt - 31) * 128)
                diag = 2 * own + i
                if qt + 1 < 64:
                    gate1(qt + 1)
                kts = list(range(diag + 1))
                groups = [kts[a:a + 4] for a in range(0, len(kts), 4)]
                po = 3 + ((qt + DBG_FLIP) % 2)
                psO = psb[po][:, 0:65]

                def emit_S(gi):
                    bank = SB[(sctr + gi) % 2]
                    psS = psb[bank][:, :].rearrange("p (s q) -> p s q", s=4)
                    for sl, kt in enumerate(groups[gi]):
                        kc = slice(kt * 128, (kt + 1) * 128)
                        mm(psS[:, sl, :], Kaug[0:100, kc], Qaug[0:100, qc], True, kt != diag, KR + QR + [f"qm{qt}"], [PSN[bank]])
                        if kt == diag:
                            mm(psS[:, sl, :], ident_bf[:], tri, False, True, ["tri"], [PSN[bank]])

                def emit_EP(gi):
                    bank = SB[(sctr + gi) % 2]
                    n = len(groups[gi])
                    pt = pT[(sctr + gi) % 3]
                    act(pt[:, 0:n * 128], psb[bank][:, 0:n * 128], AF.Exp, [PSN[bank]], [f"pT{(sctr + gi) % 3}"])
                    for sl, kt in enumerate(groups[gi]):
                        mm(psO, pt[:, sl * 128:(sl + 1) * 128], Vh[:, kt, 0:65], kt == 0, kt == diag,
                           [f"pT{(sctr + gi) % 3}"] + VR, [PSN[po]])

                emit_S(0)
                for gi in range(len(groups)):
                    if gi + 1 < len(groups):
                        emit_S(gi + 1)
                    emit_EP(gi)
                    tick()
                sctr += len(groups)
                p = qt % 2
                P.op("vector", lambda e, p=p, po=po: e.reciprocal(out=rs[p][:, 0:1], in_=psb[po][:, 64:65]), [PSN[po]], [f"rs{p}"])
                ts("vector", yh[p], psb[po][:, 0:64], rs[p][:, 0:1], None, ALU.mult, None, [PSN[po], f"rs{p}"], [f"yh{p}"])
                tr(psbf[0][0:64, 512 + p * 128:512 + (p + 1) * 128], yh[p], ident_bf[:], [f"yh{p}"], [B0])
                cp("scalar", yattT[0:64, qc], psbf[0][0:64, 512 + p * 128:512 + (p + 1) * 128], [B0], ["yattT"])
                if qt + 1 < 64:
                    gate2(qt + 1)
            P.dma(yattT_s[h * 64:(h + 1) * 64, TO:], yattT[0:64, :], ["yattT"], [], queue="gpsimd")
        if ssm_gen is not None:
            for _ in ssm_gen:
                pass
        P.barrier()

    def phase_C(mix):
        ar.reset()
        B = alloc_common(2 if mix else 3)
        outv = outT.rearrange("(k p) t -> p k t", p=128)
        if mix:
            yatt = ar.bf16(4 * NT).rearrange("p (k t) -> p k t", k=4)
            ypre = ar.f32(4 * NT).rearrange("p (k t) -> p k t", k=4)
            uTt = ar.bf16(4 * NT).rearrange("p (k t) -> p k t", k=4)
            sga = ar.bf16(8 * NT).rearrange("p (k t) -> p k t", k=8)
            sgs = ar.bf16(8 * NT).rearrange("p (k t) -> p k t", k=8)
            merged = ar.bf16(8 * NT).rearrange("p (k t) -> p k t", k=8)
            gT = ar.bf16(4 * NT).rearrange("p (k t) -> p k t", k=4)
            yssm = ar.bf16(4 * NT).rearrange("p (k t) -> p k t", k=4)
            yt = [ar.f32(NT) for _ in range(4)]
            wb = [ar.bf16(512).rearrange("p (k j) -> p k j", k=4) for _ in range(6)]
            wo = [ar.bf16(1024).rearrange("p (k j) -> p k j", k=8) for _ in range(3)]
            v4 = lambda d: d.rearrange("(k p) t -> p k t", p=128)
        P.dma(B["x"][0], x1v[:, :, OT0 * NT:(OT0 + 1) * NT], [], XR(0))
        for ti in range(OT0, NTL):
            xs = ti % 2
            t0 = ti * NT
            x = B["x"][xs]
            if ti + 1 < NTL:
                P.dma(B["x"][1 - xs], x1v[:, :, t0 + NT:t0 + 2 * NT], [], XR(1 - xs))
            if mix:
                P.dma(yatt, v4(yattT_s)[:, :, t0:t0 + NT], [], ["yatt"])
                P.dma(ypre, v4(ypre_s)[:, :, t0:t0 + NT], [], ["ypre"])
                P.dma(uTt, v4(uT_s)[:, :, t0:t0 + NT], [], ["uTt"])
                P.dma(sga, v4(sga_s)[:, :, t0:t0 + NT], [], ["sga"])
                P.dma(sgs, v4(sgs_s)[:, :, t0:t0 + NT], [], ["sgs"])
                for k in range(4):
                    ya = yt[k % 2]
                    yb = yt[2 + k % 2]
                    stt(ya, uTt[:, k, :], dsk[:, k:k + 1], ypre[:, k, :], ALU.mult, ALU.add, ["uTt", "ypre"], [f"yt{k % 2}"])
                    act(yb, ya, AF.Square, [f"yt{k % 2}"], [f"yt{2 + k % 2}"])
                    ts("vector", yb, yb, 0.044715, 1.0, ALU.mult, ALU.add, [f"yt{2 + k % 2}"], [f"yt{2 + k % 2}"])
                    tt("vector", yb, yb, ya, ALU.mult, [f"yt{2 + k % 2}", f"yt{k % 2}"], [f"yt{2 + k % 2}"])
                    act(yb, yb, AF.Sigmoid, [f"yt{2 + k % 2}"], [f"yt{2 + k % 2}"], scale=1.5957691216057308)
                    tt("vector", gT[:, k, :], ya, yb, ALU.mult, [f"yt{k % 2}", f"yt{2 + k % 2}"], [f"gT{k}"])
                steps = []
                for m in range(4):
                    def load(m=m, ti=ti):
                        s = nslot("wb", ("glu", ti, m), 6)
                        P.dma(wb[s], wglu_s[m], [], [f"wb{s}"])

                    def comp(m=m, ti=ti):
                        s = slot[("glu", ti, m)]
                        po = 5 + (m % 2)
                        for k in range(4):
                            mm(psb[po][:, :], wb[s][:, k, :], gT[:, k, :], k == 0, k == 3, [f"wb{s}", f"gT{k}"], [PSN[po]])
                        sgt = B["sg"][m % 2]
                        act(sgt, psb[po][:, :], AF.Sigmoid, [PSN[po]], [f"sg{m % 2}"], bias=bglu[:, m:m + 1])
                        tt("vector", yssm[:, m, :], gT[:, m, :], sgt, ALU.mult, [f"gT{m}", f"sg{m % 2}"], [f"yssm{m}"])
                    steps.append((load, comp))
                for m in range(8):
                    def load(m=m, ti=ti):
                        s = nslot("wb", ("ba", ti, m), 6)
                        P.dma(wb[s], wba_s[m], [], [f"wb{s}"])
                        s = nslot("wb", ("bs", ti, m), 6)
                        P.dma(wb[s], wbs_s[m], [], [f"wb{s}"])

                    def comp(m=m, ti=ti):
                        sa = slot[("ba", ti, m)]
                        ss = slot[("bs", ti, m)]
                        pa = 1 + (m % 2)
                        pss = 3 + (m % 2)
                        for k in range(4):
                            mm(psb[pa][:, :], wb[sa][:, k, :], yatt[:, k, :], k == 0, k == 3, [f"wb{sa}", "yatt"], [PSN[pa]])
                        for k in range(4):
                            mm(psb[pss][:, :], wb[ss][:, k, :], yssm[:, k, :], k == 0, k == 3, [f"wb{ss}", f"yssm{k}"], [PSN[pss]])
                        t1 = yt[m % 2]
                        t2 = yt[2 + m % 2]
                        tt("vector", t1, psb[pa][:, :], sga[:, m, :], ALU.mult, [PSN[pa], "sga"], [f"yt{m % 2}"])
                        tt("vector", t2, psb[pss][:, :], sgs[:, m, :], ALU.mult, [PSN[pss], "sgs"], [f"yt{2 + m % 2}"])
                        tt("gpsimd", merged[:, m, :], t1, t2, ALU.add, [f"yt{m % 2}", f"yt{2 + m % 2}"], [f"mg{m}"])
                    steps.append((load, comp))
                for m in range(8):
                    def load(m=m, ti=ti):
                        s = nslot("wo", ("wo", ti, m), 3)
                        P.dma(wo[s], wout_s[m], [], [f"wo{s}"])

                    def comp(m=m, ti=ti, x=x, xs=xs):
                        s = slot[("wo", ti, m)]
                        po = 5 + (m % 2)
                        for k in range(8):
                            mm(psb[po][:, :], wo[s][:, k, :], merged[:, k, :], k == 0, k == 7, [f"wo{s}", f"mg{k}"], [PSN[po]])
                        stt(x[:, m, :], psb[po][:, :], g2[:, m:m + 1], x[:, m, :], ALU.mult, ALU.add,
                            [PSN[po], f"x{xs}_{m}"], [f"x{xs}_{m}"])
                    steps.append((load, comp))
                pipeline(steps, 2)
            norm_mod(B, xs, a3, b3)
            pipeline(ffn_steps(B, xs, wgu2_s, wd2_s, g3h, ("C2", ti)), 2)
            norm_stats(B, xs)
            for k in range(8):
                stt(x[:, k, :], x[:, k, :], nfin[:, k:k + 1], B["rstd"], ALU.mult, ALU.mult,
                    [f"x{xs}_{k}", "rstd"], [f"x{xs}_{k}"])
            P.dma(outv[:, :, t0 - TO:t0 - TO + NT], x, XR(xs), [], queue="gpsimd", final=True)

    if "C" in stages:
        phase_C(("att" in stages) and ("ssm" in stages))

    P.finish()
    return nc


_CACHE = {}


def _vec8(v):
    return np.ascontiguousarray(np.asarray(v, np.float32).reshape(-1, 128).T)


def _host_inputs(inp, core):
    import ml_dtypes
    f = lambda a: np.ascontiguousarray(np.asarray(a, np.float32))
    m = {}
    b, half = core // 2, core % 2
    xb = np.asarray(inp["x"][b], np.float32)
    if half == 1:
        win = xb
    else:
        win = np.concatenate([np.zeros((TO, D), np.float32), xb[:TO]], axis=0)
    m["xT"] = np.ascontiguousarray(win.T)
    m["uflag"] = np.full((128, 1), float(half), np.float32)
    m["cT"] = _vec8(inp["c"][b])
    m["w_ada"] = f(inp["w_ada"][0])
    m["b_adaT"] = _vec8(inp["b_ada"][0])
    m["nf1"] = _vec8(inp["norm_ffn1"][0]); m["nmix"] = _vec8(inp["norm_mix"][0])
    m["nf2"] = _vec8(inp["norm_ffn2"][0]); m["nfin"] = _vec8(inp["norm_final"])
    for k in ("w_ffn1_in", "w_ffn1_out", "w_ffn2_in", "w_ffn2_out", "w_in", "w_glu", "w_br_att", "w_br_ssm", "w_out"):
        m[k] = f(inp[k][0])
    def gp(a):
        a = np.asarray(a, np.float32).reshape(16, 2, 64)
        return np.ascontiguousarray(a.transpose(1, 2, 0).reshape(128, 16))
    m["lamr"] = gp(inp["lam_re"][0]); m["lami"] = gp(inp["lam_im"][0])
    m["logdt"] = gp(np.repeat(np.asarray(inp["log_dt"][0], np.float32)[:, None], 64, axis=1))
    def bsrc(a):
        a = np.asarray(a, np.float32).reshape(16, 2, 64, 16)
        o = np.zeros((2, 64, 16, 2, 16), np.float32)
        for two in range(2):
            o[two, :, :, two, :] = a[:, two].transpose(1, 0, 2)
        return np.ascontiguousarray(o.reshape(128, 16, 32))
    m["bsrc_re"] = bsrc(inp["ssm_b_re"][0]); m["bsrc_im"] = bsrc(inp["ssm_b_im"][0])
    def csrc(a):
        a = np.asarray(a, np.float32).reshape(16, 2, 16, 64)
        o = np.zeros((2, 16, 16, 2, 64), np.float32)
        for two in range(2):
            o[two, :, :, two, :] = a[:, two].transpose(1, 0, 2)
        return np.ascontiguousarray(o.reshape(32, 16, 128))
    m["csrc_re"] = csrc(inp["ssm_c_re"][0]); m["csrc_im"] = csrc(inp["ssm_c_im"][0])
    m["dsk"] = _vec8(inp["ssm_d"][0]); m["bglu"] = _vec8(inp["b_glu"][0])
    t = np.arange(T)
    kaug = np.zeros((8, 4, T), np.float32); qaug = np.zeros((8, 4, T), np.float32)
    for h in range(8):
        sl = 2.0 ** (-(h + 1))
        kaug[h, 0] = 1.0; kaug[h, 1] = 1.0; kaug[h, 2] = sl * 256.0 * (t // 256); kaug[h, 3] = sl * (t % 256)
        qaug[h, 0] = -sl * 256.0 * (t // 256); qaug[h, 1] = -sl * (t % 256); qaug[h, 2] = 1.0; qaug[h, 3] = 1.0
    m["kaug"] = kaug.astype(ml_dtypes.bfloat16); m["qaug"] = qaug.astype(ml_dtypes.bfloat16)
    m["eind"] = (np.arange(32)[:, None] == (t // 256)[None, :]).astype(np.float32).astype(ml_dtypes.bfloat16)
    kk = np.arange(128)
    m["tri"] = np.where(kk[:, None] <= kk[None, :], 0.0, NEGM).astype(np.float32).astype(ml_dtypes.bfloat16)
    okn = np.arange(32)[None, :] < np.arange(32)[:, None]
    if half == 0:
        okn = okn & (np.arange(32)[None, :] >= 16)
    el = np.where(okn, 0.0, -1e30).astype(np.float32)
    m["elig"] = np.ascontiguousarray(np.broadcast_to(el[None], (128, 32, 32)))
    return m


STAGES = ("cast", "A", "ssm", "att", "C")


def kernel(**inputs):
    key = STAGES
    if key not in _CACHE:
        _CACHE[key] = build_program(STAGES)
    nc = _CACHE[key]
    in_maps = [_host_inputs(inputs, c) for c in range(8)]
    res = run_bass_kernel_spmd(nc, in_maps, core_ids=list(range(8)))
    out = np.empty((4, T, D), np.float32)
    for c in range(8):
        out[c // 2, (c % 2) * TO:(c % 2 + 1) * TO] = res.results[c]["outT"].T
    return out
```

```python
from contextlib import ExitStack
import numpy as np
import concourse.bass as bass
import concourse.mybir as mybir
from concourse.bass_utils import run_bass_kernel_spmd

F32 = mybir.dt.float32
BF16 = mybir.dt.bfloat16
AF = mybir.ActivationFunctionType
ALU = mybir.AluOpType
AX = mybir.AxisListType


class Prog:
    ENGINES = ["sync", "scalar", "vector", "gpsimd", "tensor"]

    def __init__(self, nc, n_dma_sems=40):
        self.nc = nc
        self.stack = ExitStack()
        self.streams = {e: [] for e in self.ENGINES}
        self.sem = {}
        for e in self.ENGINES:
            self.sem[("e", e)] = self.stack.enter_context(nc.semaphore(f"se_{e}"))
        for i in range(n_dma_sems):
            self.sem[("d", i)] = self.stack.enter_context(nc.semaphore(f"sd_{i}"))
        self.n_dma = n_dma_sems
        self.cnt = {k: 0 for k in self.sem}
        self.known = {e: {} for e in self.ENGINES}
        self.snap = {}
        self.res = {}
        self.dnext = 0
        self.finals = []
        self.nops = 0

    def sb(self, name, shape, dtype):
        return self.stack.enter_context(self.nc.sbuf_tensor(name, list(shape), dtype))

    def ps(self, name, shape, dtype):
        return self.stack.enter_context(self.nc.psum_tensor(name, list(shape), dtype))

    def _deps(self, engine, reads, writes):
        deps = {}
        def add(tok):
            if tok is None:
                return
            k, v = tok
            if engine == "tensor" and k == ("e", "tensor"):
                return
            if deps.get(k, 0) < v:
                deps[k] = v
        for r in reads:
            st = self.res.get(r)
            if st:
                add(st["w"])
        for w in writes:
            st = self.res.get(w)
            if st:
                add(st["w"])
                for t in st["r"]:
                    add(t)
        return deps

    def _emit_waits(self, engine, deps):
        kn = self.known[engine]
        for k, v in deps.items():
            if kn.get(k, 0) >= v:
                continue
            self.streams[engine].append(("w", self.sem[k], v))
            sn = self.snap.get((k, v))
            if sn:
                for kk, vv in sn.items():
                    if kn.get(kk, 0) < vv:
                        kn[kk] = vv
            kn[k] = v

    def _commit(self, engine, tok, reads, writes):
        sn = dict(self.known[engine])
        sn[tok[0]] = tok[1]
        self.snap[tok] = sn
        for r in reads:
            st = self.res.setdefault(r, {"w": None, "r": []})
            st["r"].append(tok)
        for w in writes:
            self.res[w] = {"w": tok, "r": []}
        self.nops += 1

    def op(self, engine, fn, reads=(), writes=()):
        deps = self._deps(engine, reads, writes)
        self._emit_waits(engine, deps)
        k = ("e", engine)
        self.cnt[k] += 1
        self.streams[engine].append(("o", fn, self.sem[k], 1))
        tok = (k, self.cnt[k])
        if engine != "tensor":
            pass
        self._commit(engine, tok, reads, writes)
        return tok

    def dma(self, out, in_, reads=(), writes=(), queue="sync", final=False, **kw):
        deps = self._deps(queue, reads, writes)
        idx = self.dnext
        self.dnext = (self.dnext + 1) % self.n_dma
        k = ("d", idx)
        if self.cnt[k] > 0:
            if deps.get(k, 0) < self.cnt[k]:
                deps[k] = self.cnt[k]
        self._emit_waits(queue, deps)
        self.cnt[k] += 16
        self.streams[queue].append(("o", lambda e: e.dma_start(out=out, in_=in_, **kw), self.sem[k], 16))
        tok = (k, self.cnt[k])
        self._commit(queue, tok, reads, writes)
        if final:
            self.finals.append(tok)
        return tok

    def barrier(self):
        for e in self.ENGINES:
            deps = {k: v for k, v in self.cnt.items() if v > 0}
            self._emit_waits(e, deps)
        self.res = {}

    def finish(self):
        deps = {}
        for k, v in self.finals:
            if deps.get(k, 0) < v:
                deps[k] = v
        self._emit_waits("sync", deps)
        with self.nc.Block() as block:
            for e in self.ENGINES:
                stream = self.streams[e]

                def body(eng, stream=stream):
                    for it in stream:
                        if it[0] == "w":
                            eng.wait_ge(it[1], it[2])
                        else:
                            it[1](eng).then_inc(it[2], it[3])

                getattr(block, e)(body)
        self.stack.close()


D = 1024
T = 8192
NT = 512
NTL = T // NT
DFF = 2816
FC = DFF // 128
NBLK = T // 256
TO = T // 2
OT0 = NTL // 2
NEGM = -30000.0
DBG = set()
DBG_NTL = NTL
DBG_NH = 8
DBG_NQT = 64
DBG_FLIP = 0
DBG_NPAIR = 16


class Arena:
    def __init__(self, P, name, n):
        self.t = P.sb(name, [128, n], F32)
        self.n = n
        self.off = 0

    def reset(self):
        self.off = 0

    def f32(self, n):
        ap = self.t[:, self.off:self.off + n]
        self.off += n
        assert self.off <= self.n, (self.off, self.n)
        return ap

    def bf16(self, n):
        m = (n + 1) // 2
        ap = self.t[:, self.off:self.off + m].bitcast(BF16)
        self.off += m
        assert self.off <= self.n, (self.off, self.n)
        return ap


def build_program(stages=("cast", "A", "ssm", "att", "C")):
    nc = bass.Bass("TRN2", target_bir_lowering=False)
    P = Prog(nc)

    def din(name, shape, dt=F32):
        return nc.dram_tensor(name, list(shape), dt, kind="ExternalInput").ap()

    def dscr(name, shape, dt):
        kind = "ExternalOutput" if name in DBG else "Internal"
        return nc.dram_tensor(name, list(shape), dt, kind=kind).ap()

    xT = din("xT", [D, T])
    cT = din("cT", [128, 8])
    w_ada = din("w_ada", [D, 9 * D])
    b_adaT = din("b_adaT", [128, 72])
    nvec = {n: din(n, [128, 8]) for n in ("nf1", "nmix", "nf2", "nfin")}
    w1i = din("w_ffn1_in", [D, 2 * DFF])
    w1o = din("w_ffn1_out", [DFF, D])
    w2i = din("w_ffn2_in", [D, 2 * DFF])
    w2o = din("w_ffn2_out", [DFF, D])
    w_in = din("w_in", [D, 4096])
    w_glu = din("w_glu", [512, 512])
    w_ba = din("w_br_att", [512, D])
    w_bs = din("w_br_ssm", [512, D])
    w_out = din("w_out", [D, D])
    lamr_d = din("lamr", [128, 16])
    lami_d = din("lami", [128, 16])
    logdt_d = din("logdt", [128, 16])
    bsrc_re_d = din("bsrc_re", [128, 16, 32])
    bsrc_im_d = din("bsrc_im", [128, 16, 32])
    csrc_re_d = din("csrc_re", [32, 16, 128])
    csrc_im_d = din("csrc_im", [32, 16, 128])
    dsk_d = din("dsk", [128, 4])
    bglu_d = din("bglu", [128, 4])
    kaug_d = din("kaug", [8, 4, T], BF16)
    qaug_d = din("qaug", [8, 4, T], BF16)
    eind_d = din("eind", [32, T], BF16)
    tri_d = din("tri", [128, 128], BF16)
    elig_d = din("elig", [128, NBLK, NBLK])
    uflag_d = din("uflag", [128, 1])
    outT = nc.dram_tensor("outT", [D, TO], F32, kind="ExternalOutput").ap()

    wgu1_s = dscr("wgu1_s", [2 * FC, 128, 8, 128], BF16)
    wd1_s = dscr("wd1_s", [8, 128, FC, 128], BF16)
    wgu2_s = dscr("wgu2_s", [2 * FC, 128, 8, 128], BF16)
    wd2_s = dscr("wd2_s", [8, 128, FC, 128], BF16)
    win_s = dscr("win_s", [32, 128, 8, 128], BF16)
    wba_s = dscr("wba_s", [8, 128, 4, 128], BF16)
    wbs_s = dscr("wbs_s", [8, 128, 4, 128], BF16)
    wout_s = dscr("wout_s", [8, 128, 8, 128], BF16)
    wglu_s = dscr("wglu_s", [4, 128, 4, 128], BF16)
    x1_s = dscr("x1_s", [D, T], F32)
    qT_s = dscr("qT_s", [512, T], BF16)
    kT_s = dscr("kT_s", [512, T], BF16)
    uT_s = dscr("uT_s", [512, T], BF16)
    v_s = dscr("v_s", [T, 512], BF16)
    sga_s = dscr("sga_s", [D, T], BF16)
    sgs_s = dscr("sgs_s", [D, T], BF16)
    kmean_s = dscr("kmean_s", [512, NBLK], F32)
    yattT_s = dscr("yattT_s", [512, T], BF16)
    ypre_s = dscr("ypre_s", [512, T], F32)

    ones_bf = P.sb("ones_bf", [128, 128], BF16)
    ident_bf = P.sb("ident_bf", [128, 128], BF16)
    ident_f = P.sb("ident_f", [128, 128], F32)
    eps_col = P.sb("eps_col", [128, 1], F32)
    uflag = P.sb("uflag_sb", [128, 1], F32)
    vecs = P.sb("vecs", [128, 32 + 72 + 9 * 8 + 8 + 8], F32)
    nf1 = vecs[:, 0:8]; nmix = vecs[:, 8:16]; nf2 = vecs[:, 16:24]; nfin = vecs[:, 24:32]
    adaT = vecs[:, 32:104]
    der = vecs[:, 104:176]
    a1, b1, g1h, a2, b2, g2, a3, b3, g3h = [der[:, i * 8:(i + 1) * 8] for i in range(9)]
    dsk = vecs[:, 176:180]; bglu = vecs[:, 180:184]
    kmacc = P.sb("kmacc", [128, 4, NBLK], F32)
    ar = Arena(P, "arena", 44600)
    psb = [P.ps(f"psb{i}", [128, 512], F32) for i in range(8)]
    PSN = [f"ps{i}" for i in range(8)]

    def mm(out, lhsT, rhs, start, stop, reads, writes):
        P.op("tensor", lambda e: e.matmul(out, lhsT=lhsT, rhs=rhs, start=start, stop=stop), reads, writes)

    def tr(out, in_, ident, reads, writes):
        P.op("tensor", lambda e: e.transpose(out, in_, ident), reads, writes)

    def act(out, in_, func, reads, writes, **kw):
        P.op("scalar", lambda e: e.activation(out=out, in_=in_, func=func, **kw), reads, writes)

    def ts(eng, out, in0, s1, s2, op0, op1, reads, writes):
        if op1 is None:
            P.op(eng, lambda e: e.tensor_scalar(out=out, in0=in0, scalar1=s1, scalar2=None, op0=op0), reads, writes)
        else:
            P.op(eng, lambda e: e.tensor_scalar(out=out, in0=in0, scalar1=s1, scalar2=s2, op0=op0, op1=op1), reads, writes)

    def stt(out, in0, scalar, in1, op0, op1, reads, writes):
        P.op("vector", lambda e: e.scalar_tensor_tensor(out=out, in0=in0, scalar=scalar, in1=in1, op0=op0, op1=op1), reads, writes)

    def tt(eng, out, in0, in1, op, reads, writes):
        P.op(eng, lambda e: e.tensor_tensor(out=out, in0=in0, in1=in1, op=op), reads, writes)

    def cp(eng, out, in_, reads, writes):
        if eng == "scalar":
            P.op(eng, lambda e: e.copy(out, in_), reads, writes)
        else:
            P.op(eng, lambda e: e.tensor_copy(out=out, in_=in_), reads, writes)

    def pipeline(steps, look=2):
        n = len(steps)
        for i in range(min(look, n)):
            steps[i][0]()
        for i in range(n):
            steps[i][1]()
            if i + look < n:
                steps[i + look][0]()

    P.op("gpsimd", lambda e: e.memset(ones_bf[:], 1.0), [], ["ones_bf"])
    P.op("gpsimd", lambda e: e.memset(ident_bf[:], 1.0), [], ["ident_bf"])
    P.op("gpsimd", lambda e: e.affine_select(out=ident_bf[:], in_=ident_bf[:], pattern=[[-1, 128]],
                                             compare_op=ALU.is_equal, fill=0.0, base=0, channel_multiplier=1),
         ["ident_bf"], ["ident_bf"])
    P.op("gpsimd", lambda e: e.memset(ident_f[:], 1.0), [], ["ident_f"])
    P.op("gpsimd", lambda e: e.affine_select(out=ident_f[:], in_=ident_f[:], pattern=[[-1, 128]],
                                             compare_op=ALU.is_equal, fill=0.0, base=0, channel_multiplier=1),
         ["ident_f"], ["ident_f"])
    P.op("gpsimd", lambda e: e.memset(eps_col[:], 1e-6), [], ["eps_col"])
    P.op("gpsimd", lambda e: e.memset(kmacc[:], 0.0), [], ["kmacc"])
    P.dma(nf1, nvec["nf1"], [], ["vecs"])
    P.dma(nmix, nvec["nmix"], [], ["vecs"])
    P.dma(nf2, nvec["nf2"], [], ["vecs"])
    P.dma(nfin, nvec["nfin"], [], ["vecs"])
    P.dma(dsk, dsk_d, [], ["vecs"])
    P.dma(bglu, bglu_d, [], ["vecs"])
    P.dma(uflag[:], uflag_d, [], ["uflag"])

    ar.reset()
    ct = ar.f32(8)
    sc2 = ar.f32(16).rearrange("p (k t) -> p k t", t=2)
    badaT = ar.f32(72)
    wst = [ar.f32(8 * 512).rearrange("p (k n) -> p k n", k=8) for _ in range(2)]
    P.dma(ct, cT, [], ["ct"])
    P.dma(badaT, b_adaT, [], ["badaT"])
    sgc = ar.f32(8)
    act(sgc, ct, AF.Sigmoid, ["ct"], ["sgc"])
    tt("vector", sc2[:, :, 0], ct, sgc, ALU.mult, ["ct", "sgc"], ["sc2"])
    tt("vector", sc2[:, :, 1], ct, sgc, ALU.mult, ["ct", "sgc"], ["sc2"])
    ps_ada = psb[0][:, 0:144].rearrange("p (j t) -> p j t", t=2)
    w_ada_v = w_ada.rearrange("(k p) n -> p k n", p=128)
    for sl in range(18):
        slot = sl % 2
        P.dma(wst[slot], w_ada_v[:, :, sl * 512:(sl + 1) * 512], [], [f"wst{slot}"])
        for jj in range(4):
            j = sl * 4 + jj
            for k in range(8):
                mm(ps_ada[:, j, :], wst[slot][:, k, jj * 128:(jj + 1) * 128], sc2[:, k, :], k == 0, k == 7,
                   [f"wst{slot}", "sc2"], [PSN[0]])
    tt("vector", adaT, ps_ada[:, :, 0], badaT, ALU.add, [PSN[0], "badaT", "vecs"], ["vecs"])

    def av(i):
        return adaT[:, i * 8:(i + 1) * 8]
    for (aa, bb, gg, nrm, base, gs) in ((a1, b1, g1h, nf1, 0, 0.5), (a2, b2, g2, nmix, 3, 1.0), (a3, b3, g3h, nf2, 6, 0.5)):
        stt(aa, av(base + 1), 1.0, nrm, ALU.add, ALU.mult, ["vecs"], ["vecs"])
        cp("vector", bb, av(base), ["vecs"], ["vecs"])
        ts("vector", gg, av(base + 2), gs, None, ALU.mult, None, ["vecs"], ["vecs"])
    P.barrier()

    CAST_WORDS = 2 * 2816 + 2 * 1408
    CAST_BASE = ar.n - CAST_WORDS
    stg = [ar.t[:, CAST_BASE + i * 2816:CAST_BASE + (i + 1) * 2816] for i in range(2)]
    cbf = [ar.t[:, CAST_BASE + 5632 + i * 1408:CAST_BASE + 5632 + (i + 1) * 1408].bitcast(BF16) for i in range(2)]
    cast_i = [0]

    def cast_slabs(src, dst, K, SW, ldq):
        kc = K // 128
        ncols = src.shape[1]
        srcv = src.rearrange("(k p) n -> p k n", p=128)
        assert kc * SW <= 2816
        for c0 in range(0, ncols, SW):
            def slab(c0=c0):
                i = cast_i[0]
                cast_i[0] += 1
                sl = i % 2
                s_ = stg[sl][:, 0:kc * SW].rearrange("p (k n) -> p k n", k=kc)
                b_ = cbf[sl][:, 0:kc * SW].rearrange("p (k n) -> p k n", k=kc)
                P.dma(s_, srcv[:, :, c0:c0 + SW], [], [f"stg{sl}"], queue=ldq)
                cp(("scalar", "vector")[i % 2], b_, s_, [f"stg{sl}"], [f"cbf{sl}"])
                for cc in range(SW // 128):
                    P.dma(dst[c0 // 128 + cc], b_[:, :, cc * 128:(cc + 1) * 128], [f"cbf{sl}"], [], queue="gpsimd")
            yield slab

    deferred = []
    if "cast" in stages:
        for sl_ in cast_slabs(w1i, wgu1_s, D, 256, "sync"):
            sl_()
        for sl_ in cast_slabs(w1o, wd1_s, DFF, 128, "sync"):
            sl_()
        for sl_ in cast_slabs(w_in, win_s, D, 256, "sync"):
            sl_()
        for (src_, dst_, K_, SW_) in ((w_glu, wglu_s, 512, 512), (w_ba, wba_s, 512, 512), (w_bs, wbs_s, 512, 512),
                                      (w_out, wout_s, D, 256), (w2i, wgu2_s, D, 256), (w2o, wd2_s, DFF, 128)):
            for sl_ in cast_slabs(src_, dst_, K_, SW_, "sync"):
                sl_()
        P.barrier()

    def alloc_common(nwd=3):
        B = {}
        B["x"] = [ar.f32(8 * NT).rearrange("p (k t) -> p k t", k=8) for _ in range(2)]
        B["h"] = ar.bf16(8 * NT).rearrange("p (k t) -> p k t", k=8)
        B["act"] = ar.bf16(FC * NT).rearrange("p (k t) -> p k t", k=FC)
        B["sq"] = ar.bf16(8 * NT).rearrange("p (k t) -> p k t", k=8)
        B["tmp"] = [ar.f32(NT) for _ in range(2)]
        B["rstd"] = ar.f32(NT)
        B["sd"] = ar.f32(NT)
        B["sg"] = [ar.f32(NT) for _ in range(2)]
        B["wg"] = [ar.bf16(1024).rearrange("p (k j) -> p k j", k=8) for _ in range(3)]
        B["wu"] = [ar.bf16(1024).rearrange("p (k j) -> p k j", k=8) for _ in range(3)]
        B["wd"] = [ar.bf16(FC * 128).rearrange("p (k j) -> p k j", k=FC) for _ in range(nwd)]
        return B

    cnt = {}
    slot = {}

    def nslot(cls, key, n):
        s = cnt.get(cls, 0) % n
        cnt[cls] = cnt.get(cls, 0) + 1
        slot[key] = s
        return s

    def norm_stats(B, xs):
        x = B["x"][xs]
        for k in range(8):
            act(B["sq"][:, k, :], x[:, k, :], AF.Square, [f"x{xs}_{k}"], [f"sq{k}"])
        for k in range(8):
            mm(psb[0][:, :], ones_bf[:], B["sq"][:, k, :], k == 0, k == 7, [f"sq{k}"], [PSN[0]])
        act(B["sd"], psb[0][:, :], AF.Sqrt, [PSN[0]], ["sd"], bias=eps_col[:, 0:1], scale=1.0 / D)
        P.op("vector", lambda e: e.reciprocal(out=B["rstd"], in_=B["sd"]), ["sd"], ["rstd"])

    def norm_mod(B, xs, a, b):
        x = B["x"][xs]
        norm_stats(B, xs)
        for k in range(8):
            t = B["tmp"][k % 2]
            tt("vector", t, x[:, k, :], B["rstd"], ALU.mult, [f"x{xs}_{k}", "rstd"], [f"tmp{k % 2}"])
            act(B["h"][:, k, :], t, AF.Identity, [f"tmp{k % 2}"], [f"h{k}"], bias=b[:, k:k + 1], scale=a[:, k:k + 1])

    def ffn_steps(B, xs, wgu_s, wd_s, gcol, tag):
        steps = []
        x = B["x"][xs]
        for c in range(FC):
            def load(c=c):
                s = nslot("gu", (tag, "gu", c), 3)
                P.dma(B["wg"][s], wgu_s[c], [], [f"wg{s}"])
                P.dma(B["wu"][s], wgu_s[FC + c], [], [f"wu{s}"])

            def comp(c=c):
                s = slot[(tag, "gu", c)]
                pg = 1 + (c % 2)
                pu = 3 + (c % 2)
                for k in range(8):
                    mm(psb[pg][:, :], B["wg"][s][:, k, :], B["h"][:, k, :], k == 0, k == 7, [f"wg{s}", f"h{k}"], [PSN[pg]])
                for k in range(8):
                    mm(psb[pu][:, :], B["wu"][s][:, k, :], B["h"][:, k, :], k == 0, k == 7, [f"wu{s}", f"h{k}"], [PSN[pu]])
                sg = B["sg"][c % 2]
                act(sg, psb[pg][:, :], AF.Silu, [PSN[pg]], [f"sg{c % 2}"])
                tt("vector", B["act"][:, c, :], sg, psb[pu][:, :], ALU.mult, [f"sg{c % 2}", PSN[pu]], [f"act{c}"])
            steps.append((load, comp))
        for m in range(8):
            def load(m=m):
                s = nslot("wd", (tag, "wd", m), len(B["wd"]))
                P.dma(B["wd"][s], wd_s[m], [], [f"wd{s}"])

            def comp(m=m):
                s = slot[(tag, "wd", m)]
                po = 5 + (m % 2)
                for k in range(FC):
                    mm(psb[po][:, :], B["wd"][s][:, k, :], B["act"][:, k, :], k == 0, k == FC - 1, [f"wd{s}", f"act{k}"], [PSN[po]])
                stt(x[:, m, :], psb[po][:, :], gcol[:, m:m + 1], x[:, m, :], ALU.mult, ALU.add,
                    [PSN[po], f"x{xs}_{m}"], [f"x{xs}_{m}"])
            steps.append((load, comp))
        return steps

    xTv = xT.rearrange("(k p) t -> p k t", p=128)
    x1v = x1_s.rearrange("(k p) t -> p k t", p=128)
    XR = lambda xs: [f"x{xs}_{k}" for k in range(8)]

    if "A" in stages:
        ar.reset()
        B = alloc_common()
        win = [ar.bf16(1024).rearrange("p (k j) -> p k j", k=8) for _ in range(3)]
        wv = ar.bf16(4096).rearrange("p (c k j) -> p c k j", c=4, k=8)
        ost = [ar.bf16(NT) for _ in range(4)]
        vst = [ar.bf16(512) for _ in range(2)]
        assert ar.off <= CAST_BASE, (ar.off, CAST_BASE)
        P.dma(B["x"][0], xTv[:, :, 0:NT], [], XR(0))
        for ti in range(DBG_NTL):
            xs = ti % 2
            t0 = ti * NT
            if ti + 1 < NTL:
                P.dma(B["x"][1 - xs], xTv[:, :, t0 + NT:t0 + 2 * NT], [], XR(1 - xs))
            norm_mod(B, xs, a1, b1)
            pipeline(ffn_steps(B, xs, wgu1_s, wd1_s, g1h, ("A1", ti)), 2)
            prefix = ti < OT0
            if not prefix:
                P.dma(x1v[:, :, t0:t0 + NT], B["x"][xs], XR(xs), [], queue="gpsimd")
            norm_mod(B, xs, a2, b2)
            steps = []
            if prefix:
                order = list(range(4, 8)) + ["v"] + list(range(12, 16))
            else:
                order = list(range(0, 8)) + ["v"] + list(range(12, 32))
            for ci, c in enumerate(order):
                if c == "v":
                    def load():
                        for cc in range(4):
                            P.dma(wv[:, cc], win_s[8 + cc], [], ["wv"] if cc == 0 else [f"wv_{cc}"])

                    def comp(ti=ti, t0=t0):
                        for q4 in range(4):
                            for k in range(8):
                                mm(psb[7][:, :].rearrange("p (c j) -> p c j", c=4), B["h"][:, k, q4 * 128:(q4 + 1) * 128],
                                   wv[:, :, k, :], k == 0, k == 7, ["wv", "wv_1", "wv_2", "wv_3", f"h{k}"], [PSN[7]])
                            s = nslot("vst", None, 2)
                            cp("vector", vst[s], psb[7][:, :], [PSN[7]], [f"vst{s}"])
                            P.dma(v_s[t0 + q4 * 128:t0 + (q4 + 1) * 128, :], vst[s], [f"vst{s}"], [], queue="gpsimd")
                    steps.append((load, comp))
                    continue

                def load(c=c, ti=ti):
                    s = nslot("win", ("A", ti, c), 3)
                    P.dma(win[s], win_s[c], [], [f"win{s}"])

                def comp(c=c, ci=ci, ti=ti, t0=t0, prefix=prefix):
                    s = slot[("A", ti, c)]
                    po = 5 + (ci % 2)
                    for k in range(8):
                        mm(psb[po][:, :], win[s][:, k, :], B["h"][:, k, :], k == 0, k == 7, [f"win{s}", f"h{k}"], [PSN[po]])
                    o = nslot("ost", None, 4)
                    if c < 4:
                        act(ost[o], psb[po][:, :], AF.Identity, [PSN[po]], [f"ost{o}"], scale=0.125)
                        dst = qT_s[c * 128:(c + 1) * 128, t0:t0 + NT]
                    elif c < 8:
                        cp("vector", ost[o], psb[po][:, :], [PSN[po]], [f"ost{o}"])
                        P.op("vector", lambda e: e.tensor_reduce(out=kmacc[:, c - 4, 2 * ti:2 * ti + 2],
                                                                 in_=psb[po][:, :].rearrange("p (b t) -> p b t", b=2),
                                                                 axis=AX.X, op=ALU.add), [PSN[po]], ["kmacc"])
                        dst = kT_s[(c - 4) * 128:(c - 3) * 128, t0:t0 + NT]
                    elif c < 16:
                        if prefix:
                            act(ost[o], psb[po][:, :], AF.Identity, [PSN[po]], [f"ost{o}"], scale=uflag[:, 0:1])
                        else:
                            act(ost[o], psb[po][:, :], AF.Identity, [PSN[po]], [f"ost{o}"])
                        dst = uT_s[(c - 12) * 128:(c - 11) * 128, t0:t0 + NT]
                    elif c < 24:
                        act(ost[o], psb[po][:, :], AF.Sigmoid, [PSN[po]], [f"ost{o}"])
                        dst = sga_s[(c - 16) * 128:(c - 15) * 128, t0:t0 + NT]
                    else:
                        act(ost[o], psb[po][:, :], AF.Sigmoid, [PSN[po]], [f"ost{o}"])
                        dst = sgs_s[(c - 24) * 128:(c - 23) * 128, t0:t0 + NT]
                    P.dma(dst, ost[o], [f"ost{o}"], [], queue="gpsimd")
                steps.append((load, comp))
            pipeline(steps, 2)
            for _ in range(3):
                if deferred:
                    deferred.pop(0)()
        while deferred:
            deferred.pop(0)()
        ts("vector", kmacc[:], kmacc[:], 1.0 / 256, None, ALU.mult, None, ["kmacc"], ["kmacc"])
        P.dma(kmean_s.rearrange("(c p) n -> p c n", p=128), kmacc[:], ["kmacc"], [])
        P.barrier()


    ssm_gen = None
    if "ssm" in stages:
        ar.reset()
        hpi = ar.f32(1)
        P.op("gpsimd", lambda e: e.memset(hpi, float(np.pi / 2)), [], ["sm"])
        SM = {}
        def smt(name, n=16):
            SM[name] = ar.f32(n)
            return SM[name]
        for nm in ("lr", "li", "ld", "dt", "mag", "ang", "c", "s", "t1", "t2", "t3", "abr", "abi", "nr", "den", "fre", "fim", "nfim"):
            smt(nm)
        AR = ar.f32(13 * 16).rearrange("p (k g) -> p k g", k=13)
        AI = ar.f32(13 * 16).rearrange("p (k g) -> p k g", k=13)
        NAI = ar.f32(13 * 16).rearrange("p (k g) -> p k g", k=13)
        UR = ar.f32(9 * 16).rearrange("p (k g) -> p k g", k=9)
        UI = ar.f32(9 * 16).rearrange("p (k g) -> p k g", k=9)
        NUI = ar.f32(9 * 16).rearrange("p (k g) -> p k g", k=9)
        R = ["sm"]
        P.dma(SM["lr"], lamr_d, [], R)
        P.dma(SM["li"], lami_d, [], R)
        P.dma(SM["ld"], logdt_d, [], R)
        act(SM["dt"], SM["ld"], AF.Exp, R, R)
        tt("vector", SM["t1"], SM["lr"], SM["dt"], ALU.mult, R, R)
        act(SM["mag"], SM["t1"], AF.Exp, R, R)
        tt("vector", SM["ang"], SM["li"], SM["dt"], ALU.mult, R, R)
        act(SM["s"], SM["ang"], AF.Sin, R, R, scale=1.0 / 16)
        act(SM["c"], SM["ang"], AF.Sin, R, R, scale=1.0 / 16, bias=hpi)
        for _ in range(4):
            tt("vector", SM["t1"], SM["c"], SM["c"], ALU.mult, R, R)
            tt("vector", SM["t2"], SM["s"], SM["s"], ALU.mult, R, R)
            tt("vector", SM["t3"], SM["c"], SM["s"], ALU.mult, R, R)
            tt("vector", SM["c"], SM["t1"], SM["t2"], ALU.subtract, R, R)
            ts("vector", SM["s"], SM["t3"], 2.0, None, ALU.mult, None, R, R)
        cp("vector", UR[:, 0, :], SM["c"], R, R)
        cp("vector", UI[:, 0, :], SM["s"], R, R)
        for k in range(8):
            tt("vector", SM["t1"], UR[:, k, :], UR[:, k, :], ALU.mult, R, R)
            tt("vector", SM["t2"], UI[:, k, :], UI[:, k, :], ALU.mult, R, R)
            tt("vector", SM["t3"], UR[:, k, :], UI[:, k, :], ALU.mult, R, R)
            tt("vector", UR[:, k + 1, :], SM["t1"], SM["t2"], ALU.subtract, R, R)
            ts("vector", UI[:, k + 1, :], SM["t3"], 2.0, None, ALU.mult, None, R, R)
        ts("vector", NUI.rearrange("p k g -> p (k g)"), UI.rearrange("p k g -> p (k g)"), -1.0, None, ALU.mult, None, R, R)
        tt("vector", AR[:, 0, :], SM["mag"], SM["c"], ALU.mult, R, R)
        tt("vector", AI[:, 0, :], SM["mag"], SM["s"], ALU.mult, R, R)
        for k in range(12):
            tt("vector", SM["t1"], AR[:, k, :], AR[:, k, :], ALU.mult, R, R)
            tt("vector", SM["t2"], AI[:, k, :], AI[:, k, :], ALU.mult, R, R)
            tt("vector", SM["t3"], AR[:, k, :], AI[:, k, :], ALU.mult, R, R)
            tt("vector", AR[:, k + 1, :], SM["t1"], SM["t2"], ALU.subtract, R, R)
            ts("vector", AI[:, k + 1, :], SM["t3"], 2.0, None, ALU.mult, None, R, R)
        ts("vector", NAI.rearrange("p k g -> p (k g)"), AI.rearrange("p k g -> p (k g)"), -1.0, None, ALU.mult, None, R, R)
        ts("vector", SM["nr"], AR[:, 0, :], -1.0, None, ALU.add, None, R, R)
        tt("vector", SM["t1"], SM["lr"], SM["lr"], ALU.mult, R, R)
        tt("vector", SM["t2"], SM["li"], SM["li"], ALU.mult, R, R)
        tt("vector", SM["den"], SM["t1"], SM["t2"], ALU.add, R, R)
        P.op("vector", lambda e: e.reciprocal(out=SM["den"], in_=SM["den"]), R, R)
        tt("vector", SM["t1"], SM["nr"], SM["lr"], ALU.mult, R, R)
        tt("vector", SM["t2"], AI[:, 0, :], SM["li"], ALU.mult, R, R)
        tt("vector", SM["t1"], SM["t1"], SM["t2"], ALU.add, R, R)
        tt("vector", SM["fre"], SM["t1"], SM["den"], ALU.mult, R, R)
        tt("vector", SM["t1"], AI[:, 0, :], SM["lr"], ALU.mult, R, R)
        tt("vector", SM["t2"], SM["nr"], SM["li"], ALU.mult, R, R)
        tt("vector", SM["t1"], SM["t1"], SM["t2"], ALU.subtract, R, R)
        tt("vector", SM["fim"], SM["t1"], SM["den"], ALU.mult, R, R)
        BTre = ar.bf16(16 * 128).rearrange("p (q j) -> p q j", q=16)
        BTim = ar.bf16(16 * 128).rearrange("p (q j) -> p q j", q=16)
        CBre = ar.bf16(16 * 32).rearrange("p (g c) -> p g c", g=16)
        CBren = ar.bf16(16 * 32).rearrange("p (g c) -> p g c", g=16)
        CBimn = ar.bf16(16 * 32).rearrange("p (g c) -> p g c", g=16)
        ssm_mark = ar.off
        CTre = ar.f32(16 * 32).rearrange("p (g c) -> p g c", g=16)
        CTimn = ar.f32(16 * 32).rearrange("p (g c) -> p g c", g=16)
        CTren = ar.f32(16 * 32).rearrange("p (g c) -> p g c", g=16)
        bre = ar.f32(16 * 32).rearrange("p (g c) -> p g c", g=16)
        bim = ar.f32(16 * 32).rearrange("p (g c) -> p g c", g=16)
        btmp = ar.f32(32)
        bbre = ar.bf16(16 * 32).rearrange("p (g c) -> p g c", g=16)
        bbim = ar.bf16(16 * 32).rearrange("p (g c) -> p g c", g=16)
        csr = ar.f32(16 * 128).rearrange("p (g j) -> p g j", g=16)
        csi = ar.f32(16 * 128).rearrange("p (g j) -> p g j", g=16)
        P.dma(bre, bsrc_re_d, [], ["bre"])
        P.dma(bim, bsrc_im_d, [], ["bim"])
        P.dma(csr[0:32], csrc_re_d, [], ["csr"])
        P.dma(csi[0:32], csrc_im_d, [], ["csi"])
        for g in range(16):
            ts("vector", btmp, bim[:, g, :], SM["fim"][:, g:g + 1], None, ALU.mult, None, ["bim"] + R, ["btmp"])
            stt(bbre[:, g, :], bre[:, g, :], SM["fre"][:, g:g + 1], btmp, ALU.mult, ALU.subtract, ["bre", "btmp"] + R, ["bbre"])
            ts("vector", btmp, bre[:, g, :], SM["fim"][:, g:g + 1], None, ALU.mult, None, ["bre"] + R, ["btmp"])
            stt(bbim[:, g, :], bim[:, g, :], SM["fre"][:, g:g + 1], btmp, ALU.mult, ALU.add, ["bim", "btmp"] + R, ["bbim"])
        psbf = [psb[i][:, :].bitcast(BF16) for i in range(8)]
        for g in range(16):
            tr(psbf[0][0:32, 0:128], bbre[:, g, :], ident_bf[:], ["bbre"], [PSN[0]])
            tr(psbf[0][0:32, 128:256], bbim[:, g, :], ident_bf[:], ["bbim"], [PSN[0]])
            cp("vector", BTre[0:32, g, :], psbf[0][0:32, 0:128], [PSN[0]], ["BT"])
            cp("vector", BTim[0:32, g, :], psbf[0][0:32, 128:256], [PSN[0]], ["BT"])
        for g in range(16):
            tr(psb[1][:, 0:32], csr[0:32, g, :], ident_f[0:32, 0:32], ["csr"], [PSN[1]])
            tr(psb[1][:, 32:64], csi[0:32, g, :], ident_f[0:32, 0:32], ["csi"], [PSN[1]])
            cp("vector", CTre[:, g, :], psb[1][:, 0:32], [PSN[1]], ["CT"])
            ts("vector", CTimn[:, g, :], psb[1][:, 32:64], -1.0, None, ALU.mult, None, [PSN[1]], ["CT"])
            ts("vector", CTren[:, g, :], psb[1][:, 0:32], -1.0, None, ALU.mult, None, [PSN[1]], ["CT"])
            cp("vector", CBre[:, g, :], CTre[:, g, :], ["CT"], ["CT"])
            cp("vector", CBren[:, g, :], CTren[:, g, :], ["CT"], ["CT"])
            cp("vector", CBimn[:, g, :], CTimn[:, g, :], ["CT"], ["CT"])
        P.barrier()
        ar.off = ssm_mark
        H = TO
        LC = 256
        NCH = H // LC
        ZTr, ZTi, XTr, XTi = ar.f32(H), ar.f32(H), ar.f32(H), ar.f32(H)
        up1 = ar.bf16(T)
        yst = [ar.f32(512) for _ in range(2)]
        Dc = [ar.f32(512) for _ in range(2)]
        Ds = [ar.f32(512) for _ in range(2)]
        dtmp = ar.f32(128)
        ptm = [ar.f32(512) for _ in range(2)]
        pbf = [ar.bf16(512) for _ in range(8)]
        ones_l = ar.f32(LC)
        rtile = ar.f32(LC)
        ini = ar.f32(4)
        Gs = ar.f32(2)
        P.op("gpsimd", lambda e: e.memset(ones_l, 1.0), [], ["ones_l"])
        ZB_R, ZB_I, YB = 5, 6, 7

        def bn(pref, lo, hi):
            return [f"{pref}{b}" for b in range(lo // 512, (hi + 511) // 512)]

        def build_D(g):
            d = g % 2
            dc, ds_ = Dc[d], Ds[d]
            rn = [f"D{d}"]
            P.op("vector", lambda e: e.memset(dc[:, 0:1], 1.0), rn, rn)
            P.op("vector", lambda e: e.memset(ds_[:, 0:1], 0.0), rn, rn)
            for k in range(8):
                n = 1 << k
                ur, ui, nui = UR[:, k, g:g + 1], UI[:, k, g:g + 1], NUI[:, k, g:g + 1]
                ts("vector", dtmp[:, 0:n], ds_[:, 0:n], nui, None, ALU.mult, None, rn + R, ["dtmp"])
                ts("vector", dc[:, n:2 * n], dc[:, 0:n], ur, None, ALU.mult, None, rn + R, rn)
                tt("vector", dc[:, n:2 * n], dc[:, n:2 * n], dtmp[:, 0:n], ALU.add, rn + ["dtmp"], rn)
                ts("vector", dtmp[:, 0:n], ds_[:, 0:n], ur, None, ALU.mult, None, rn + R, ["dtmp"])
                ts("vector", ds_[:, n:2 * n], dc[:, 0:n], ui, None, ALU.mult, None, rn + R, rn)
                tt("vector", ds_[:, n:2 * n], ds_[:, n:2 * n], dtmp[:, 0:n], ALU.add, rn + ["dtmp"], rn)
            cp("vector", dc[:, 256:512], dc[:, 0:256], rn, rn)
            cp("vector", ds_[:, 256:512], ds_[:, 0:256], rn, rn)

        def ssm_main():
            build_D(0)
            yield
            for g in range(DBG_NPAIR):
                d = g % 2
                dc, ds_ = Dc[d], Ds[d]
                DN = [f"D{d}"]
                P.dma(up1[0:32, :], uT_s[g * 32:(g + 1) * 32, :], [], ["up0"])
                if g + 1 < 16:
                    build_D(g + 1)
                ts("vector", rtile, ones_l, SM["mag"][:, g:g + 1], None, ALU.mult, None, ["ones_l"] + R, ["rtile"])
                yield
                for blk in range(8):
                    cs = slice(blk * 512, (blk + 1) * 512)
                    mm(psb[ZB_R][:, :], BTre[0:32, g, :], up1[0:32, cs], True, True, ["up0", "BT"], [PSN[ZB_R]])
                    mm(psb[ZB_I][:, :], BTim[0:32, g, :], up1[0:32, cs], True, True, ["up0", "BT"], [PSN[ZB_I]])
                    cp("scalar", XTr[:, cs], psb[ZB_R][:, :], [PSN[ZB_R]], [f"xtr{blk}"])
                    cp("scalar", XTi[:, cs], psb[ZB_I][:, :], [PSN[ZB_I]], [f"xti{blk}"])
                    yield
                tb = [(XTr, XTi, "xtr", "xti"), (ZTr, ZTi, "ztr", "zti")]
                n = H
                for k in range(12):
                    sr, si, pr_, pi2 = tb[k % 2]
                    dr, di, qr_, qi2 = tb[(k + 1) % 2]
                    m = n // 2
                    svr = sr[:, 0:n].rearrange("p (j two) -> p j two", two=2)
                    svi = si[:, 0:n].rearrange("p (j two) -> p j two", two=2)
                    srcn = bn(pr_, 0, n) + bn(pi2, 0, n)
                    stt(dr[:, 0:m], svi[:, :, 0], NAI[:, k, g:g + 1], svr[:, :, 1], ALU.mult, ALU.add, srcn + R, bn(qr_, 0, m))
                    stt(dr[:, 0:m], svr[:, :, 0], AR[:, k, g:g + 1], dr[:, 0:m], ALU.mult, ALU.add, srcn + bn(qr_, 0, m) + R, bn(qr_, 0, m))
                    stt(di[:, 0:m], svr[:, :, 0], AI[:, k, g:g + 1], svi[:, :, 1], ALU.mult, ALU.add, srcn + R, bn(qi2, 0, m))
                    stt(di[:, 0:m], svi[:, :, 0], AR[:, k, g:g + 1], di[:, 0:m], ALU.mult, ALU.add, srcn + bn(qi2, 0, m) + R, bn(qi2, 0, m))
                    n = m
                    if k < 4:
                        yield
                cp("vector", Gs[:, 0:1], XTr[:, 0:1], ["xtr0"], ["Gs"])
                cp("vector", Gs[:, 1:2], XTi[:, 0:1], ["xti0"], ["Gs"])
                Gr, Gi = Gs[:, 0:1], Gs[:, 1:2]
                yield
                for ob in range(8):
                    blk = 8 + ob
                    cs = slice(blk * 512, (blk + 1) * 512)
                    co = slice(ob * 512, (ob + 1) * 512)
                    mm(psb[ZB_R][:, :], BTre[0:32, g, :], up1[0:32, cs], True, True, ["up0", "BT"], [PSN[ZB_R]])
                    mm(psb[ZB_I][:, :], BTim[0:32, g, :], up1[0:32, cs], True, True, ["up0", "BT"], [PSN[ZB_I]])
                    t1, t2 = ptm[0], ptm[1]
                    yield
                    tt("vector", ZTr[:, co], psb[ZB_R][:, :], dc, ALU.mult, [PSN[ZB_R]] + DN, [f"ztr{ob}"])
                    tt("vector", t1, psb[ZB_I][:, :], ds_, ALU.mult, [PSN[ZB_I]] + DN, ["ptm0"])
                    tt("vector", ZTi[:, co], psb[ZB_I][:, :], dc, ALU.mult, [PSN[ZB_I]] + DN, [f"zti{ob}"])
                    tt("vector", t2, psb[ZB_R][:, :], ds_, ALU.mult, [PSN[ZB_R]] + DN, ["ptm1"])
                    tt("vector", ZTr[:, co], ZTr[:, co], t1, ALU.add, [f"ztr{ob}", "ptm0"], [f"ztr{ob}"])
                    tt("vector", ZTi[:, co], ZTi[:, co], t2, ALU.subtract, [f"zti{ob}", "ptm1"], [f"zti{ob}"])
                    yield
                stt(ZTr[:, 0:1], Gi, NAI[:, 0, g:g + 1], ZTr[:, 0:1], ALU.mult, ALU.add, ["Gs", "ztr0"] + R, ["ztr0"])
                stt(ZTr[:, 0:1], Gr, AR[:, 0, g:g + 1], ZTr[:, 0:1], ALU.mult, ALU.add, ["Gs", "ztr0"] + R, ["ztr0"])
                stt(ZTi[:, 0:1], Gr, AI[:, 0, g:g + 1], ZTi[:, 0:1], ALU.mult, ALU.add, ["Gs", "zti0"] + R, ["zti0"])
                stt(ZTi[:, 0:1], Gi, AR[:, 0, g:g + 1], ZTi[:, 0:1], ALU.mult, ALU.add, ["Gs", "zti0"] + R, ["zti0"])
                for c in range(NCH):
                    cc = slice(c * LC, (c + 1) * LC)
                    ob = c // 2
                    if c == 0:
                        i_r, i_i = 0.0, 0.0
                    else:
                        er, ei = XTr[:, c * LC - 1:c * LC], XTi[:, c * LC - 1:c * LC]
                        ul_r, ul_i, nul_i = UR[:, 8, g:g + 1], UI[:, 8, g:g + 1], NUI[:, 8, g:g + 1]
                        ts("vector", ini[:, 2:3], er, ul_r, None, ALU.mult, None, [f"xtr{(c - 1) // 2}"] + R, ["ini_t"])
                        stt(ini[:, 0:1], ei, nul_i, ini[:, 2:3], ALU.mult, ALU.add, [f"xti{(c - 1) // 2}", "ini_t"] + R, ["ini_r"])
                        ts("vector", ini[:, 3:4], ei, ul_r, None, ALU.mult, None, [f"xti{(c - 1) // 2}"] + R, ["ini_u"])
                        stt(ini[:, 1:2], er, ul_i, ini[:, 3:4], ALU.mult, ALU.add, [f"xtr{(c - 1) // 2}", "ini_u"] + R, ["ini_i"])
                        i_r, i_i = ini[:, 0:1], ini[:, 1:2]
                    P.op("vector", lambda e, cc=cc, i_r=i_r: e.tensor_tensor_scan(out=XTr[:, cc], data0=rtile, data1=ZTr[:, cc], initial=i_r,
                                                                              op0=ALU.mult, op1=ALU.add),
                         [f"ztr{ob}", "rtile", "ini_r"], [f"xtr{ob}"])
                    P.op("vector", lambda e, cc=cc, i_i=i_i: e.tensor_tensor_scan(out=XTi[:, cc], data0=rtile, data1=ZTi[:, cc], initial=i_i,
                                                                              op0=ALU.mult, op1=ALU.add),
                         [f"zti{ob}", "rtile", "ini_i"], [f"xti{ob}"])
                    if c % 2 == 1:
                        yield
                pend = None
                for ob in range(8):
                    co = slice(ob * 512, (ob + 1) * 512)
                    q0 = 4 * (ob % 2)
                    m1, m2, m3, m4 = pbf[q0], pbf[q0 + 1], pbf[q0 + 2], pbf[q0 + 3]
                    nn = [f"pbf{q0 + j}" for j in range(4)]
                    tt("vector", m1, XTr[:, co], dc, ALU.mult, [f"xtr{ob}"] + DN, [nn[0]])
                    tt("vector", m2, XTi[:, co], ds_, ALU.mult, [f"xti{ob}"] + DN, [nn[1]])
                    tt("vector", m3, XTr[:, co], ds_, ALU.mult, [f"xtr{ob}"] + DN, [nn[2]])
                    tt("vector", m4, XTi[:, co], dc, ALU.mult, [f"xti{ob}"] + DN, [nn[3]])

                    def ymm(ob=ob, m1=m1, m2=m2, m3=m3, m4=m4, nn=nn, g=g):
                        mm(psb[YB][0:32, :], CBre[:, g, :], m1, True, False, [nn[0], "CT"], [PSN[YB]])
                        mm(psb[YB][0:32, :], CBren[:, g, :], m2, False, False, [nn[1], "CT"], [PSN[YB]])
                        mm(psb[YB][0:32, :], CBimn[:, g, :], m3, False, False, [nn[2], "CT"], [PSN[YB]])
                        mm(psb[YB][0:32, :], CBimn[:, g, :], m4, False, True, [nn[3], "CT"], [PSN[YB]])
                        ys = nslot("yst", None, 2)
                        cp("scalar", yst[ys][0:32, :], psb[YB][0:32, :], [PSN[YB]], [f"yst{ys}"])
                        P.dma(ypre_s[g * 32:(g + 1) * 32, TO + ob * 512:TO + (ob + 1) * 512], yst[ys][0:32, :], [f"yst{ys}"], [], queue="gpsimd")
                    if pend is not None:
                        pend()
                    pend = ymm
                    yield
                pend()
                yield

        ssm_gen = ssm_main()
        if "att" not in stages:
            for _ in ssm_gen:
                pass
            P.barrier()

    if "att" in stages:
        if ssm_gen is None:
            ar.reset()
        Kaug = ar.bf16(T)
        Qaug = ar.bf16(TO)
        Vh = ar.bf16(64 * 66).rearrange("p (t d) -> p t d", t=64)
        kmf = ar.f32(32)
        kmb = ar.bf16(32)
        elig = ar.f32(32 * 32).rearrange("p (o n) -> p o n", o=32)
        tri = ar.bf16(128)
        gsc = [ar.f32(32) for _ in range(2)]
        top8 = [ar.f32(8) for _ in range(2)]
        thr = [ar.f32(2) for _ in range(2)]
        mb = [ar.bf16(32) for _ in range(2)]
        pT = [ar.bf16(512) for _ in range(3)]
        rs = [ar.f32(2) for _ in range(2)]
        yh = [ar.bf16(64) for _ in range(2)]
        yattT = ar.bf16(TO)
        psbf = [psb[i][:, :].bitcast(BF16) for i in range(8)]
        P.dma(Kaug[64:96, :], eind_d, [], ["Kind"])
        P.dma(tri, tri_d, [], ["tri"])
        P.dma(elig, elig_d, [], ["elig"])
        P.op("gpsimd", lambda e: e.memset(Vh[:, :, 64:66], 1.0), [], ["Vones"])
        SB = [1, 2]
        B0 = "psB0"
        tick_n = [0]

        def tick():
            tick_n[0] += 1
            if ssm_gen is not None and tick_n[0] % 3 == 0:
                next(ssm_gen, None)

        for h in range(DBG_NH):
            P.dma(Kaug[0:64, :], kT_s[h * 64:(h + 1) * 64, :], [], ["Kaug"])
            P.dma(Kaug[96:100, :], kaug_d[h], [], ["Kaug2"])
            P.dma(Qaug[0:64, :], qT_s[h * 64:(h + 1) * 64, TO:], [], ["Qaug"])
            P.dma(Qaug[96:100, :], qaug_d[h][:, TO:], [], ["Qaug2"])
            vsrc = v_s[:, h * 64:(h + 1) * 64].rearrange("(t p) d -> p t d", p=128)
            for v4 in range(4):
                P.dma(Vh[:, v4 * 16:(v4 + 1) * 16, 0:64], vsrc[:, v4 * 16:(v4 + 1) * 16, :], [], ["Vh"] if v4 == 0 else [f"Vh{v4}"])
            P.dma(kmf[0:64, :], kmean_s[h * 64:(h + 1) * 64, :], [], ["kmf"])
            cp("vector", kmb[0:64, :], kmf[0:64, :], ["kmf"], ["kmb"])
            KR = ["Kaug", "Kaug2", "Kind"]
            QR = ["Qaug", "Qaug2"]
            VR = ["Vh", "Vh1", "Vh2", "Vh3", "Vones"]

            def gate1(qt):
                own = qt // 2
                qc = slice((qt - 32) * 128, (qt - 31) * 128)
                p = qt % 2
                mm(psb[0][:, p * 32:(p + 1) * 32], Qaug[0:64, qc], kmb[0:64, :], True, True, ["Qaug", "kmb"], [B0])
                tt("vector", gsc[p], psb[0][:, p * 32:(p + 1) * 32], elig[:, own, :], ALU.add, [B0, "elig"], [f"g2{p}"])
                P.op("vector", lambda e: e.max(out=top8[p], in_=gsc[p]), [f"g2{p}"], [f"top8{p}"])
                ts("vector", thr[p][:, 0:1], top8[p][:, 2:3], -1e29, None, ALU.max, None, [f"top8{p}"], [f"thr{p}"])
                ts("vector", mb[p], gsc[p], thr[p][:, 0:1], NEGM, ALU.is_lt, ALU.mult, [f"g2{p}", f"thr{p}"], [f"mb{p}"])
                P.op("vector", lambda e: e.memset(mb[p][:, own:own + 1], 0.0), [f"mb{p}"], [f"mb{p}"])

            def gate2(qt):
                qc = slice((qt - 32) * 128, (qt - 31) * 128)
                p = qt % 2
                tr(psbf[0][64:96, 256 + p * 128:256 + (p + 1) * 128], mb[p], ident_bf[:], [f"mb{p}"], [B0])
                cp("scalar", Qaug[64:96, qc], psbf[0][64:96, 256 + p * 128:256 + (p + 1) * 128], [B0], [f"qm{qt}"])

            gate1(32)
            gate2(32)
            sctr = 0
            for qt in range(32, 64):
                own = qt // 2
                i = qt % 2
                qc = slice((qt - 32) * 128, (qt - 31) * 128)
                diag = 2 * own + i
                if qt + 1 < 64:
                    gate1(qt + 1)
                kts = list(range(diag + 1))
                groups = [kts[a:a + 4] for a in range(0, len(kts), 4)]
                po = 3 + ((qt + DBG_FLIP) % 2)
                psO = psb[po][:, 0:65]

                def emit_S(gi):
                    bank = SB[(sctr + gi) % 2]
                    psS = psb[bank][:, :].rearrange("p (s q) -> p s q", s=4)
                    for sl, kt in enumerate(groups[gi]):
                        kc = slice(kt * 128, (kt + 1) * 128)
                        mm(psS[:, sl, :], Kaug[0:100, kc], Qaug[0:100, qc], True, kt != diag, KR + QR + [f"qm{qt}"], [PSN[bank]])
                        if kt == diag:
                            mm(psS[:, sl, :], ident_bf[:], tri, False, True, ["tri"], [PSN[bank]])

                def emit_EP(gi):
                    bank = SB[(sctr + gi) % 2]
                    n = len(groups[gi])
                    pt = pT[(sctr + gi) % 3]
                    act(pt[:, 0:n * 128], psb[bank][:, 0:n * 128], AF.Exp, [PSN[bank]], [f"pT{(sctr + gi) % 3}"])
                    for sl, kt in enumerate(groups[gi]):
                        mm(psO, pt[:, sl * 128:(sl + 1) * 128], Vh[:, kt, 0:65], kt == 0, kt == diag,
                           [f"pT{(sctr + gi) % 3}"] + VR, [PSN[po]])

                emit_S(0)
                for gi in range(len(groups)):
                    if gi + 1 < len(groups):
                        emit_S(gi + 1)
                    emit_EP(gi)
                    tick()
                sctr += len(groups)
                p = qt % 2
                P.op("vector", lambda e, p=p, po=po: e.reciprocal(out=rs[p][:, 0:1], in_=psb[po][:, 64:65]), [PSN[po]], [f"rs{p}"])
                ts("vector", yh[p], psb[po][:, 0:64], rs[p][:, 0:1], None, ALU.mult, None, [PSN[po], f"rs{p}"], [f"yh{p}"])
                tr(psbf[0][0:64, 512 + p * 128:512 + (p + 1) * 128], yh[p], ident_bf[:], [f"yh{p}"], [B0])
                cp("scalar", yattT[0:64, qc], psbf[0][0:64, 512 + p * 128:512 + (p + 1) * 128], [B0], ["yattT"])
                if qt + 1 < 64:
                    gate2(qt + 1)
            P.dma(yattT_s[h * 64:(h + 1) * 64, TO:], yattT[0:64, :], ["yattT"], [], queue="gpsimd")
        if ssm_gen is not None:
            for _ in ssm_gen:
                pass
        P.barrier()

    def phase_C(mix):
        ar.reset()
        B = alloc_common(2 if mix else 3)
        outv = outT.rearrange("(k p) t -> p k t", p=128)
        if mix:
            yatt = ar.bf16(4 * NT).rearrange("p (k t) -> p k t", k=4)
            ypre = ar.f32(4 * NT).rearrange("p (k t) -> p k t", k=4)
            uTt = ar.bf16(4 * NT).rearrange("p (k t) -> p k t", k=4)
            sga = ar.bf16(8 * NT).rearrange("p (k t) -> p k t", k=8)
            sgs = ar.bf16(8 * NT).rearrange("p (k t) -> p k t", k=8)
            merged = ar.bf16(8 * NT).rearrange("p (k t) -> p k t", k=8)
            gT = ar.bf16(4 * NT).rearrange("p (k t) -> p k t", k=4)
            yssm = ar.bf16(4 * NT).rearrange("p (k t) -> p k t", k=4)
            yt = [ar.f32(NT) for _ in range(4)]
            wb = [ar.bf16(512).rearrange("p (k j) -> p k j", k=4) for _ in range(6)]
            wo = [ar.bf16(1024).rearrange("p (k j) -> p k j", k=8) for _ in range(3)]
            v4 = lambda d: d.rearrange("(k p) t -> p k t", p=128)
        P.dma(B["x"][0], x1v[:, :, OT0 * NT:(OT0 + 1) * NT], [], XR(0))
        for ti in range(OT0, NTL):
            xs = ti % 2
            t0 = ti * NT
            x = B["x"][xs]
            if ti + 1 < NTL:
                P.dma(B["x"][1 - xs], x1v[:, :, t0 + NT:t0 + 2 * NT], [], XR(1 - xs))
            if mix:
                P.dma(yatt, v4(yattT_s)[:, :, t0:t0 + NT], [], ["yatt"])
                P.dma(ypre, v4(ypre_s)[:, :, t0:t0 + NT], [], ["ypre"])
                P.dma(uTt, v4(uT_s)[:, :, t0:t0 + NT], [], ["uTt"])
                P.dma(sga, v4(sga_s)[:, :, t0:t0 + NT], [], ["sga"])
                P.dma(sgs, v4(sgs_s)[:, :, t0:t0 + NT], [], ["sgs"])
                for k in range(4):
                    ya = yt[k % 2]
                    yb = yt[2 + k % 2]
                    stt(ya, uTt[:, k, :], dsk[:, k:k + 1], ypre[:, k, :], ALU.mult, ALU.add, ["uTt", "ypre"], [f"yt{k % 2}"])
                    act(yb, ya, AF.Square, [f"yt{k % 2}"], [f"yt{2 + k % 2}"])
                    ts("vector", yb, yb, 0.044715, 1.0, ALU.mult, ALU.add, [f"yt{2 + k % 2}"], [f"yt{2 + k % 2}"])
                    tt("vector", yb, yb, ya, ALU.mult, [f"yt{2 + k % 2}", f"yt{k % 2}"], [f"yt{2 + k % 2}"])
                    act(yb, yb, AF.Sigmoid, [f"yt{2 + k % 2}"], [f"yt{2 + k % 2}"], scale=1.5957691216057308)
                    tt("vector", gT[:, k, :], ya, yb, ALU.mult, [f"yt{k % 2}", f"yt{2 + k % 2}"], [f"gT{k}"])
                steps = []
                for m in range(4):
                    def load(m=m, ti=ti):
                        s = nslot("wb", ("glu", ti, m), 6)
                        P.dma(wb[s], wglu_s[m], [], [f"wb{s}"])

                    def comp(m=m, ti=ti):
                        s = slot[("glu", ti, m)]
                        po = 5 + (m % 2)
                        for k in range(4):
                            mm(psb[po][:, :], wb[s][:, k, :], gT[:, k, :], k == 0, k == 3, [f"wb{s}", f"gT{k}"], [PSN[po]])
                        sgt = B["sg"][m % 2]
                        act(sgt, psb[po][:, :], AF.Sigmoid, [PSN[po]], [f"sg{m % 2}"], bias=bglu[:, m:m + 1])
                        tt("vector", yssm[:, m, :], gT[:, m, :], sgt, ALU.mult, [f"gT{m}", f"sg{m % 2}"], [f"yssm{m}"])
                    steps.append((load, comp))
                for m in range(8):
                    def load(m=m, ti=ti):
                        s = nslot("wb", ("ba", ti, m), 6)
                        P.dma(wb[s], wba_s[m], [], [f"wb{s}"])
                        s = nslot("wb", ("bs", ti, m), 6)
                        P.dma(wb[s], wbs_s[m], [], [f"wb{s}"])

                    def comp(m=m, ti=ti):
                        sa = slot[("ba", ti, m)]
                        ss = slot[("bs", ti, m)]
                        pa = 1 + (m % 2)
                        pss = 3 + (m % 2)
                        for k in range(4):
                            mm(psb[pa][:, :], wb[sa][:, k, :], yatt[:, k, :], k == 0, k == 3, [f"wb{sa}", "yatt"], [PSN[pa]])
                        for k in range(4):
                            mm(psb[pss][:, :], wb[ss][:, k, :], yssm[:, k, :], k == 0, k == 3, [f"wb{ss}", f"yssm{k}"], [PSN[pss]])
                        t1 = yt[m % 2]
                        t2 = yt[2 + m % 2]
                        tt("vector", t1, psb[pa][:, :], sga[:, m, :], ALU.mult, [PSN[pa], "sga"], [f"yt{m % 2}"])
                        tt("vector", t2, psb[pss][:, :], sgs[:, m, :], ALU.mult, [PSN[pss], "sgs"], [f"yt{2 + m % 2}"])
                        tt("gpsimd", merged[:, m, :], t1, t2, ALU.add, [f"yt{m % 2}", f"yt{2 + m % 2}"], [f"mg{m}"])
                    steps.append((load, comp))
                for m in range(8):
                    def load(m=m, ti=ti):
                        s = nslot("wo", ("wo", ti, m), 3)
                        P.dma(wo[s], wout_s[m], [], [f"wo{s}"])

                    def comp(m=m, ti=ti, x=x, xs=xs):
                        s = slot[("wo", ti, m)]
                        po = 5 + (m % 2)
                        for k in range(8):
                            mm(psb[po][:, :], wo[s][:, k, :], merged[:, k, :], k == 0, k == 7, [f"wo{s}", f"mg{k}"], [PSN[po]])
                        stt(x[:, m, :], psb[po][:, :], g2[:, m:m + 1], x[:, m, :], ALU.mult, ALU.add,
                            [PSN[po], f"x{xs}_{m}"], [f"x{xs}_{m}"])
                    steps.append((load, comp))
                pipeline(steps, 2)
            norm_mod(B, xs, a3, b3)
            pipeline(ffn_steps(B, xs, wgu2_s, wd2_s, g3h, ("C2", ti)), 2)
            norm_stats(B, xs)
            for k in range(8):
                stt(x[:, k, :], x[:, k, :], nfin[:, k:k + 1], B["rstd"], ALU.mult, ALU.mult,
                    [f"x{xs}_{k}", "rstd"], [f"x{xs}_{k}"])
            P.dma(outv[:, :, t0 - TO:t0 - TO + NT], x, XR(xs), [], queue="gpsimd", final=True)

    if "C" in stages:
        phase_C(("att" in stages) and ("ssm" in stages))

    P.finish()
    return nc


_CACHE = {}


def _vec8(v):
    return np.ascontiguousarray(np.asarray(v, np.float32).reshape(-1, 128).T)


def _host_inputs(inp, core):
    import ml_dtypes
    f = lambda a: np.ascontiguousarray(np.asarray(a, np.float32))
    m = {}
    b, half = core // 2, core % 2
    xb = np.asarray(inp["x"][b], np.float32)
    if half == 1:
        win = xb
    else:
        win = np.concatenate([np.zeros((TO, D), np.float32), xb[:TO]], axis=0)
    m["xT"] = np.ascontiguousarray(win.T)
    m["uflag"] = np.full((128, 1), float(half), np.float32)
    m["cT"] = _vec8(inp["c"][b])
    m["w_ada"] = f(inp["w_ada"][0])
    m["b_adaT"] = _vec8(inp["b_ada"][0])
    m["nf1"] = _vec8(inp["norm_ffn1"][0]); m["nmix"] = _vec8(inp["norm_mix"][0])
    m["nf2"] = _vec8(inp["norm_ffn2"][0]); m["nfin"] = _vec8(inp["norm_final"])
    for k in ("w_ffn1_in", "w_ffn1_out", "w_ffn2_in", "w_ffn2_out", "w_in", "w_glu", "w_br_att", "w_br_ssm", "w_out"):
        m[k] = f(inp[k][0])
    def gp(a):
        a = np.asarray(a, np.float32).reshape(16, 2, 64)
        return np.ascontiguousarray(a.transpose(1, 2, 0).reshape(128, 16))
    m["lamr"] = gp(inp["lam_re"][0]); m["lami"] = gp(inp["lam_im"][0])
    m["logdt"] = gp(np.repeat(np.asarray(inp["log_dt"][0], np.float32)[:, None], 64, axis=1))
    def bsrc(a):
        a = np.asarray(a, np.float32).reshape(16, 2, 64, 16)
        o = np.zeros((2, 64, 16, 2, 16), np.float32)
        for two in range(2):
            o[two, :, :, two, :] = a[:, two].transpose(1, 0, 2)
        return np.ascontiguousarray(o.reshape(128, 16, 32))
    m["bsrc_re"] = bsrc(inp["ssm_b_re"][0]); m["bsrc_im"] = bsrc(inp["ssm_b_im"][0])
    def csrc(a):
        a = np.asarray(a, np.float32).reshape(16, 2, 16, 64)
        o = np.zeros((2, 16, 16, 2, 64), np.float32)
        for two in range(2):
            o[two, :, :, two, :] = a[:, two].transpose(1, 0, 2)
        return np.ascontiguousarray(o.reshape(32, 16, 128))
    m["csrc_re"] = csrc(inp["ssm_c_re"][0]); m["csrc_im"] = csrc(inp["ssm_c_im"][0])
    m["dsk"] = _vec8(inp["ssm_d"][0]); m["bglu"] = _vec8(inp["b_glu"][0])
    t = np.arange(T)
    kaug = np.zeros((8, 4, T), np.float32); qaug = np.zeros((8, 4, T), np.float32)
    for h in range(8):
        sl = 2.0 ** (-(h + 1))
        kaug[h, 0] = 1.0; kaug[h, 1] = 1.0; kaug[h, 2] = sl * 256.0 * (t // 256); kaug[h, 3] = sl * (t % 256)
        qaug[h, 0] = -sl * 256.0 * (t // 256); qaug[h, 1] = -sl * (t % 256); qaug[h, 2] = 1.0; qaug[h, 3] = 1.0
    m["kaug"] = kaug.astype(ml_dtypes.bfloat16); m["qaug"] = qaug.astype(ml_dtypes.bfloat16)
    m["eind"] = (np.arange(32)[:, None] == (t // 256)[None, :]).astype(np.float32).astype(ml_dtypes.bfloat16)
    kk = np.arange(128)
    m["tri"] = np.where(kk[:, None] <= kk[None, :], 0.0, NEGM).astype(np.float32).astype(ml_dtypes.bfloat16)
    okn = np.arange(32)[None, :] < np.arange(32)[:, None]
    if half == 0:
        okn = okn & (np.arange(32)[None, :] >= 16)
    el = np.where(okn, 0.0, -1e30).astype(np.float32)
    m["elig"] = np.ascontiguousarray(np.broadcast_to(el[None], (128, 32, 32)))
    return m


STAGES = ("cast", "A", "ssm", "att", "C")


def kernel(**inputs):
    key = STAGES
    if key not in _CACHE:
        _CACHE[key] = build_program(STAGES)
    nc = _CACHE[key]
    in_maps = [_host_inputs(inputs, c) for c in range(8)]
    res = run_bass_kernel_spmd(nc, in_maps, core_ids=list(range(8)))
    out = np.empty((4, T, D), np.float32)
    for c in range(8):
        out[c // 2, (c % 2) * TO:(c % 2 + 1) * TO] = res.results[c]["outT"].T
    return out
```

```python
from contextlib import ExitStack
import numpy as np
import concourse.bass as bass
import concourse.mybir as mybir
from concourse.bass_utils import run_bass_kernel_spmd

F32 = mybir.dt.float32
BF16 = mybir.dt.bfloat16
AF = mybir.ActivationFunctionType
ALU = mybir.AluOpType
AX = mybir.AxisListType


class Prog:
    ENGINES = ["sync", "scalar", "vector", "gpsimd", "tensor"]

    def __init__(self, nc, n_dma_sems=40):
        self.nc = nc
        self.stack = ExitStack()
        self.streams = {e: [] for e in self.ENGINES}
        self.sem = {}
        for e in self.ENGINES:
            self.sem[("e", e)] = self.stack.enter_context(nc.semaphore(f"se_{e}"))
        for i in range(n_dma_sems):
            self.sem[("d", i)] = self.stack.enter_context(nc.semaphore(f"sd_{i}"))
        self.n_dma = n_dma_sems
        self.cnt = {k: 0 for k in self.sem}
        self.known = {e: {} for e in self.ENGINES}
        self.snap = {}
        self.res = {}
        self.dnext = 0
        self.finals = []
        self.nops = 0

    def sb(self, name, shape, dtype):
        return self.stack.enter_context(self.nc.sbuf_tensor(name, list(shape), dtype))

    def ps(self, name, shape, dtype):
        return self.stack.enter_context(self.nc.psum_tensor(name, list(shape), dtype))

    def _deps(self, engine, reads, writes):
        deps = {}
        def add(tok):
            if tok is None:
                return
            k, v = tok
            if engine == "tensor" and k == ("e", "tensor"):
                return
            if deps.get(k, 0) < v:
                deps[k] = v
        for r in reads:
            st = self.res.get(r)
            if st:
                add(st["w"])
        for w in writes:
            st = self.res.get(w)
            if st:
                add(st["w"])
                for t in st["r"]:
                    add(t)
        return deps

    def _emit_waits(self, engine, deps):
        kn = self.known[engine]
        for k, v in deps.items():
            if kn.get(k, 0) >= v:
                continue
            self.streams[engine].append(("w", self.sem[k], v))
            sn = self.snap.get((k, v))
            if sn:
                for kk, vv in sn.items():
                    if kn.get(kk, 0) < vv:
                        kn[kk] = vv
            kn[k] = v

    def _commit(self, engine, tok, reads, writes):
        sn = dict(self.known[engine])
        sn[tok[0]] = tok[1]
        self.snap[tok] = sn
        for r in reads:
            st = self.res.setdefault(r, {"w": None, "r": []})
            st["r"].append(tok)
        for w in writes:
            self.res[w] = {"w": tok, "r": []}
        self.nops += 1

    def op(self, engine, fn, reads=(), writes=()):
        deps = self._deps(engine, reads, writes)
        self._emit_waits(engine, deps)
        k = ("e", engine)
        self.cnt[k] += 1
        self.streams[engine].append(("o", fn, self.sem[k], 1))
        tok = (k, self.cnt[k])
        if engine != "tensor":
            pass
        self._commit(engine, tok, reads, writes)
        return tok

    def dma(self, out, in_, reads=(), writes=(), queue="sync", final=False, **kw):
        deps = self._deps(queue, reads, writes)
        idx = self.dnext
        self.dnext = (self.dnext + 1) % self.n_dma
        k = ("d", idx)
        if self.cnt[k] > 0:
            if deps.get(k, 0) < self.cnt[k]:
                deps[k] = self.cnt[k]
        self._emit_waits(queue, deps)
        self.cnt[k] += 16
        self.streams[queue].append(("o", lambda e: e.dma_start(out=out, in_=in_, **kw), self.sem[k], 16))
        tok = (k, self.cnt[k])
        self._commit(queue, tok, reads, writes)
        if final:
            self.finals.append(tok)
        return tok

    def barrier(self):
        for e in self.ENGINES:
            deps = {k: v for k, v in self.cnt.items() if v > 0}
            self._emit_waits(e, deps)
        self.res = {}

    def finish(self):
        deps = {}
        for k, v in self.finals:
            if deps.get(k, 0) < v:
                deps[k] = v
        self._emit_waits("sync", deps)
        with self.nc.Block() as block:
            for e in self.ENGINES:
                stream = self.streams[e]

                def body(eng, stream=stream):
                    for it in stream:
                        if it[0] == "w":
                            eng.wait_ge(it[1], it[2])
                        else:
                            it[1](eng).then_inc(it[2], it[3])

                getattr(block, e)(body)
        self.stack.close()


D = 1024
T = 8192
NT = 512
NTL = T // NT
DFF = 2816
FC = DFF // 128
NBLK = T // 256
TO = T // 2
OT0 = NTL // 2
NEGM = -30000.0
DBG = set()
DBG_NTL = NTL
DBG_NH = 8
DBG_NQT = 64
DBG_FLIP = 0
DBG_NPAIR = 16


class Arena:
    def __init__(self, P, name, n):
        self.t = P.sb(name, [128, n], F32)
        self.n = n
        self.off = 0

    def reset(self):
        self.off = 0

    def f32(self, n):
        ap = self.t[:, self.off:self.off + n]
        self.off += n
        assert self.off <= self.n, (self.off, self.n)
        return ap

    def bf16(self, n):
        m = (n + 1) // 2
        ap = self.t[:, self.off:self.off + m].bitcast(BF16)
        self.off += m
        assert self.off <= self.n, (self.off, self.n)
        return ap


def build_program(stages=("cast", "A", "ssm", "att", "C")):
    nc = bass.Bass("TRN2", target_bir_lowering=False)
    P = Prog(nc)

    def din(name, shape, dt=F32):
        return nc.dram_tensor(name, list(shape), dt, kind="ExternalInput").ap()

    def dscr(name, shape, dt):
        kind = "ExternalOutput" if name in DBG else "Internal"
        return nc.dram_tensor(name, list(shape), dt, kind=kind).ap()

    xT = din("xT", [D, T])
    cT = din("cT", [128, 8])
    w_ada = din("w_ada", [D, 9 * D])
    b_adaT = din("b_adaT", [128, 72])
    nvec = {n: din(n, [128, 8]) for n in ("nf1", "nmix", "nf2", "nfin")}
    w1i = din("w_ffn1_in", [D, 2 * DFF])
    w1o = din("w_ffn1_out", [DFF, D])
    w2i = din("w_ffn2_in", [D, 2 * DFF])
    w2o = din("w_ffn2_out", [DFF, D])
    w_in = din("w_in", [D, 4096])
    w_glu = din("w_glu", [512, 512])
    w_ba = din("w_br_att", [512, D])
    w_bs = din("w_br_ssm", [512, D])
    w_out = din("w_out", [D, D])
    lamr_d = din("lamr", [128, 16])
    lami_d = din("lami", [128, 16])
    logdt_d = din("logdt", [128, 16])
    bsrc_re_d = din("bsrc_re", [128, 16, 32])
    bsrc_im_d = din("bsrc_im", [128, 16, 32])
    csrc_re_d = din("csrc_re", [32, 16, 128])
    csrc_im_d = din("csrc_im", [32, 16, 128])
    dsk_d = din("dsk", [128, 4])
    bglu_d = din("bglu", [128, 4])
    kaug_d = din("kaug", [8, 4, T], BF16)
    qaug_d = din("qaug", [8, 4, T], BF16)
    eind_d = din("eind", [32, T], BF16)
    tri_d = din("tri", [128, 128], BF16)
    elig_d = din("elig", [128, NBLK, NBLK])
    uflag_d = din("uflag", [128, 1])
    outT = nc.dram_tensor("outT", [D, TO], F32, kind="ExternalOutput").ap()

    wgu1_s = dscr("wgu1_s", [2 * FC, 128, 8, 128], BF16)
    wd1_s = dscr("wd1_s", [8, 128, FC, 128], BF16)
    wgu2_s = dscr("wgu2_s", [2 * FC, 128, 8, 128], BF16)
    wd2_s = dscr("wd2_s", [8, 128, FC, 128], BF16)
    win_s = dscr("win_s", [32, 128, 8, 128], BF16)
    wba_s = dscr("wba_s", [8, 128, 4, 128], BF16)
    wbs_s = dscr("wbs_s", [8, 128, 4, 128], BF16)
    wout_s = dscr("wout_s", [8, 128, 8, 128], BF16)
    wglu_s = dscr("wglu_s", [4, 128, 4, 128], BF16)
    x1_s = dscr("x1_s", [D, T], F32)
    qT_s = dscr("qT_s", [512, T], BF16)
    kT_s = dscr("kT_s", [512, T], BF16)
    uT_s = dscr("uT_s", [512, T], BF16)
    v_s = dscr("v_s", [T, 512], BF16)
    sga_s = dscr("sga_s", [D, T], BF16)
    sgs_s = dscr("sgs_s", [D, T], BF16)
    kmean_s = dscr("kmean_s", [512, NBLK], F32)
    yattT_s = dscr("yattT_s", [512, T], BF16)
    ypre_s = dscr("ypre_s", [512, T], F32)

    ones_bf = P.sb("ones_bf", [128, 128], BF16)
    ident_bf = P.sb("ident_bf", [128, 128], BF16)
    ident_f = P.sb("ident_f", [128, 128], F32)
    eps_col = P.sb("eps_col", [128, 1], F32)
    uflag = P.sb("uflag_sb", [128, 1], F32)
    vecs = P.sb("vecs", [128, 32 + 72 + 9 * 8 + 8 + 8], F32)
    nf1 = vecs[:, 0:8]; nmix = vecs[:, 8:16]; nf2 = vecs[:, 16:24]; nfin = vecs[:, 24:32]
    adaT = vecs[:, 32:104]
    der = vecs[:, 104:176]
    a1, b1, g1h, a2, b2, g2, a3, b3, g3h = [der[:, i * 8:(i + 1) * 8] for i in range(9)]
    dsk = vecs[:, 176:180]; bglu = vecs[:, 180:184]
    kmacc = P.sb("kmacc", [128, 4, NBLK], F32)
    ar = Arena(P, "arena", 44600)
    psb = [P.ps(f"psb{i}", [128, 512], F32) for i in range(8)]
    PSN = [f"ps{i}" for i in range(8)]

    def mm(out, lhsT, rhs, start, stop, reads, writes):
        P.op("tensor", lambda e: e.matmul(out, lhsT=lhsT, rhs=rhs, start=start, stop=stop), reads, writes)

    def tr(out, in_, ident, reads, writes):
        P.op("tensor", lambda e: e.transpose(out, in_, ident), reads, writes)

    def act(out, in_, func, reads, writes, **kw):
        P.op("scalar", lambda e: e.activation(out=out, in_=in_, func=func, **kw), reads, writes)

    def ts(eng, out, in0, s1, s2, op0, op1, reads, writes):
        if op1 is None:
            P.op(eng, lambda e: e.tensor_scalar(out=out, in0=in0, scalar1=s1, scalar2=None, op0=op0), reads, writes)
        else:
            P.op(eng, lambda e: e.tensor_scalar(out=out, in0=in0, scalar1=s1, scalar2=s2, op0=op0, op1=op1), reads, writes)

    def stt(out, in0, scalar, in1, op0, op1, reads, writes):
        P.op("vector", lambda e: e.scalar_tensor_tensor(out=out, in0=in0, scalar=scalar, in1=in1, op0=op0, op1=op1), reads, writes)

    def tt(eng, out, in0, in1, op, reads, writes):
        P.op(eng, lambda e: e.tensor_tensor(out=out, in0=in0, in1=in1, op=op), reads, writes)

    def cp(eng, out, in_, reads, writes):
        if eng == "scalar":
            P.op(eng, lambda e: e.copy(out, in_), reads, writes)
        else:
            P.op(eng, lambda e: e.tensor_copy(out=out, in_=in_), reads, writes)

    def pipeline(steps, look=2):
        n = len(steps)
        for i in range(min(look, n)):
            steps[i][0]()
        for i in range(n):
            steps[i][1]()
            if i + look < n:
                steps[i + look][0]()

    P.op("gpsimd", lambda e: e.memset(ones_bf[:], 1.0), [], ["ones_bf"])
    P.op("gpsimd", lambda e: e.memset(ident_bf[:], 1.0), [], ["ident_bf"])
    P.op("gpsimd", lambda e: e.affine_select(out=ident_bf[:], in_=ident_bf[:], pattern=[[-1, 128]],
                                             compare_op=ALU.is_equal, fill=0.0, base=0, channel_multiplier=1),
         ["ident_bf"], ["ident_bf"])
    P.op("gpsimd", lambda e: e.memset(ident_f[:], 1.0), [], ["ident_f"])
    P.op("gpsimd", lambda e: e.affine_select(out=ident_f[:], in_=ident_f[:], pattern=[[-1, 128]],
                                             compare_op=ALU.is_equal, fill=0.0, base=0, channel_multiplier=1),
         ["ident_f"], ["ident_f"])
    P.op("gpsimd", lambda e: e.memset(eps_col[:], 1e-6), [], ["eps_col"])
    P.op("gpsimd", lambda e: e.memset(kmacc[:], 0.0), [], ["kmacc"])
    P.dma(nf1, nvec["nf1"], [], ["vecs"])
    P.dma(nmix, nvec["nmix"], [], ["vecs"])
    P.dma(nf2, nvec["nf2"], [], ["vecs"])
    P.dma(nfin, nvec["nfin"], [], ["vecs"])
    P.dma(dsk, dsk_d, [], ["vecs"])
    P.dma(bglu, bglu_d, [], ["vecs"])
    P.dma(uflag[:], uflag_d, [], ["uflag"])

    ar.reset()
    ct = ar.f32(8)
    sc2 = ar.f32(16).rearrange("p (k t) -> p k t", t=2)
    badaT = ar.f32(72)
    wst = [ar.f32(8 * 512).rearrange("p (k n) -> p k n", k=8) for _ in range(2)]
    P.dma(ct, cT, [], ["ct"])
    P.dma(badaT, b_adaT, [], ["badaT"])
    sgc = ar.f32(8)
    act(sgc, ct, AF.Sigmoid, ["ct"], ["sgc"])
    tt("vector", sc2[:, :, 0], ct, sgc, ALU.mult, ["ct", "sgc"], ["sc2"])
    tt("vector", sc2[:, :, 1], ct, sgc, ALU.mult, ["ct", "sgc"], ["sc2"])
    ps_ada = psb[0][:, 0:144].rearrange("p (j t) -> p j t", t=2)
    w_ada_v = w_ada.rearrange("(k p) n -> p k n", p=128)
    for sl in range(18):
        slot = sl % 2
        P.dma(wst[slot], w_ada_v[:, :, sl * 512:(sl + 1) * 512], [], [f"wst{slot}"])
        for jj in range(4):
            j = sl * 4 + jj
            for k in range(8):
                mm(ps_ada[:, j, :], wst[slot][:, k, jj * 128:(jj + 1) * 128], sc2[:, k, :], k == 0, k == 7,
                   [f"wst{slot}", "sc2"], [PSN[0]])
    tt("vector", adaT, ps_ada[:, :, 0], badaT, ALU.add, [PSN[0], "badaT", "vecs"], ["vecs"])

    def av(i):
        return adaT[:, i * 8:(i + 1) * 8]
    for (aa, bb, gg, nrm, base, gs) in ((a1, b1, g1h, nf1, 0, 0.5), (a2, b2, g2, nmix, 3, 1.0), (a3, b3, g3h, nf2, 6, 0.5)):
        stt(aa, av(base + 1), 1.0, nrm, ALU.add, ALU.mult, ["vecs"], ["vecs"])
        cp("vector", bb, av(base), ["vecs"], ["vecs"])
        ts("vector", gg, av(base + 2), gs, None, ALU.mult, None, ["vecs"], ["vecs"])
    P.barrier()

    CAST_WORDS = 2 * 2816 + 2 * 1408
    CAST_BASE = ar.n - CAST_WORDS
    stg = [ar.t[:, CAST_BASE + i * 2816:CAST_BASE + (i + 1) * 2816] for i in range(2)]
    cbf = [ar.t[:, CAST_BASE + 5632 + i * 1408:CAST_BASE + 5632 + (i + 1) * 1408].bitcast(BF16) for i in range(2)]
    cast_i = [0]

    def cast_slabs(src, dst, K, SW, ldq):
        kc = K // 128
        ncols = src.shape[1]
        srcv = src.rearrange("(k p) n -> p k n", p=128)
        assert kc * SW <= 2816
        for c0 in range(0, ncols, SW):
            def slab(c0=c0):
                i = cast_i[0]
                cast_i[0] += 1
                sl = i % 2
                s_ = stg[sl][:, 0:kc * SW].rearrange("p (k n) -> p k n", k=kc)
                b_ = cbf[sl][:, 0:kc * SW].rearrange("p (k n) -> p k n", k=kc)
                P.dma(s_, srcv[:, :, c0:c0 + SW], [], [f"stg{sl}"], queue=ldq)
                cp(("scalar", "vector")[i % 2], b_, s_, [f"stg{sl}"], [f"cbf{sl}"])
                for cc in range(SW // 128):
                    P.dma(dst[c0 // 128 + cc], b_[:, :, cc * 128:(cc + 1) * 128], [f"cbf{sl}"], [], queue="gpsimd")
            yield slab

    deferred = []
    if "cast" in stages:
        for sl_ in cast_slabs(w1i, wgu1_s, D, 256, "sync"):
            sl_()
        for sl_ in cast_slabs(w1o, wd1_s, DFF, 128, "sync"):
            sl_()
        for sl_ in cast_slabs(w_in, win_s, D, 256, "sync"):
            sl_()
        for (src_, dst_, K_, SW_) in ((w_glu, wglu_s, 512, 512), (w_ba, wba_s, 512, 512), (w_bs, wbs_s, 512, 512),
                                      (w_out, wout_s, D, 256), (w2i, wgu2_s, D, 256), (w2o, wd2_s, DFF, 128)):
            for sl_ in cast_slabs(src_, dst_, K_, SW_, "sync"):
                sl_()
        P.barrier()

    def alloc_common(nwd=3):
        B = {}
        B["x"] = [ar.f32(8 * NT).rearrange("p (k t) -> p k t", k=8) for _ in range(2)]
        B["h"] = ar.bf16(8 * NT).rearrange("p (k t) -> p k t", k=8)
        B["act"] = ar.bf16(FC * NT).rearrange("p (k t) -> p k t", k=FC)
        B["sq"] = ar.bf16(8 * NT).rearrange("p (k t) -> p k t", k=8)
        B["tmp"] = [ar.f32(NT) for _ in range(2)]
        B["rstd"] = ar.f32(NT)
        B["sd"] = ar.f32(NT)
        B["sg"] = [ar.f32(NT) for _ in range(2)]
        B["wg"] = [ar.bf16(1024).rearrange("p (k j) -> p k j", k=8) for _ in range(3)]
        B["wu"] = [ar.bf16(1024).rearrange("p (k j) -> p k j", k=8) for _ in range(3)]
        B["wd"] = [ar.bf16(FC * 128).rearrange("p (k j) -> p k j", k=FC) for _ in range(nwd)]
        return B

    cnt = {}
    slot = {}

    def nslot(cls, key, n):
        s = cnt.get(cls, 0) % n
        cnt[cls] = cnt.get(cls, 0) + 1
        slot[key] = s
        return s

    def norm_stats(B, xs):
        x = B["x"][xs]
        for k in range(8):
            act(B["sq"][:, k, :], x[:, k, :], AF.Square, [f"x{xs}_{k}"], [f"sq{k}"])
        for k in range(8):
            mm(psb[0][:, :], ones_bf[:], B["sq"][:, k, :], k == 0, k == 7, [f"sq{k}"], [PSN[0]])
        act(B["sd"], psb[0][:, :], AF.Sqrt, [PSN[0]], ["sd"], bias=eps_col[:, 0:1], scale=1.0 / D)
        P.op("vector", lambda e: e.reciprocal(out=B["rstd"], in_=B["sd"]), ["sd"], ["rstd"])

    def norm_mod(B, xs, a, b):
        x = B["x"][xs]
        norm_stats(B, xs)
        for k in range(8):
            t = B["tmp"][k % 2]
            tt("vector", t, x[:, k, :], B["rstd"], ALU.mult, [f"x{xs}_{k}", "rstd"], [f"tmp{k % 2}"])
            act(B["h"][:, k, :], t, AF.Identity, [f"tmp{k % 2}"], [f"h{k}"], bias=b[:, k:k + 1], scale=a[:, k:k + 1])

    def ffn_steps(B, xs, wgu_s, wd_s, gcol, tag):
        steps = []
        x = B["x"][xs]
        for c in range(FC):
            def load(c=c):
                s = nslot("gu", (tag, "gu", c), 3)
                P.dma(B["wg"][s], wgu_s[c], [], [f"wg{s}"])
                P.dma(B["wu"][s], wgu_s[FC + c], [], [f"wu{s}"])

            def comp(c=c):
                s = slot[(tag, "gu", c)]
                pg = 1 + (c % 2)
                pu = 3 + (c % 2)
                for k in range(8):
                    mm(psb[pg][:, :], B["wg"][s][:, k, :], B["h"][:, k, :], k == 0, k == 7, [f"wg{s}", f"h{k}"], [PSN[pg]])
                for k in range(8):
                    mm(psb[pu][:, :], B["wu"][s][:, k, :], B["h"][:, k, :], k == 0, k == 7, [f"wu{s}", f"h{k}"], [PSN[pu]])
                sg = B["sg"][c % 2]
                act(sg, psb[pg][:, :], AF.Silu, [PSN[pg]], [f"sg{c % 2}"])
                tt("vector", B["act"][:, c, :], sg, psb[pu][:, :], ALU.mult, [f"sg{c % 2}", PSN[pu]], [f"act{c}"])
            steps.append((load, comp))
        for m in range(8):
            def load(m=m):
                s = nslot("wd", (tag, "wd", m), len(B["wd"]))
                P.dma(B["wd"][s], wd_s[m], [], [f"wd{s}"])

            def comp(m=m):
                s = slot[(tag, "wd", m)]
                po = 5 + (m % 2)
                for k in range(FC):
                    mm(psb[po][:, :], B["wd"][s][:, k, :], B["act"][:, k, :], k == 0, k == FC - 1, [f"wd{s}", f"act{k}"], [PSN[po]])
                stt(x[:, m, :], psb[po][:, :], gcol[:, m:m + 1], x[:, m, :], ALU.mult, ALU.add,
                    [PSN[po], f"x{xs}_{m}"], [f"x{xs}_{m}"])
            steps.append((load, comp))
        return steps

    xTv = xT.rearrange("(k p) t -> p k t", p=128)
    x1v = x1_s.rearrange("(k p) t -> p k t", p=128)
    XR = lambda xs: [f"x{xs}_{k}" for k in range(8)]

    if "A" in stages:
        ar.reset()
        B = alloc_common()
        win = [ar.bf16(1024).rearrange("p (k j) -> p k j", k=8) for _ in range(3)]
        wv = ar.bf16(4096).rearrange("p (c k j) -> p c k j", c=4, k=8)
        ost = [ar.bf16(NT) for _ in range(4)]
        vst = [ar.bf16(512) for _ in range(2)]
        assert ar.off <= CAST_BASE, (ar.off, CAST_BASE)
        P.dma(B["x"][0], xTv[:, :, 0:NT], [], XR(0))
        for ti in range(DBG_NTL):
            xs = ti % 2
            t0 = ti * NT
            if ti + 1 < NTL:
                P.dma(B["x"][1 - xs], xTv[:, :, t0 + NT:t0 + 2 * NT], [], XR(1 - xs))
            norm_mod(B, xs, a1, b1)
            pipeline(ffn_steps(B, xs, wgu1_s, wd1_s, g1h, ("A1", ti)), 2)
            prefix = ti < OT0
            if not prefix:
                P.dma(x1v[:, :, t0:t0 + NT], B["x"][xs], XR(xs), [], queue="gpsimd")
            norm_mod(B, xs, a2, b2)
            steps = []
            if prefix:
                order = list(range(4, 8)) + ["v"] + list(range(12, 16))
            else:
                order = list(range(0, 8)) + ["v"] + list(range(12, 32))
            for ci, c in enumerate(order):
                if c == "v":
                    def load():
                        for cc in range(4):
                            P.dma(wv[:, cc], win_s[8 + cc], [], ["wv"] if cc == 0 else [f"wv_{cc}"])

                    def comp(ti=ti, t0=t0):
                        for q4 in range(4):
                            for k in range(8):
                                mm(psb[7][:, :].rearrange("p (c j) -> p c j", c=4), B["h"][:, k, q4 * 128:(q4 + 1) * 128],
                                   wv[:, :, k, :], k == 0, k == 7, ["wv", "wv_1", "wv_2", "wv_3", f"h{k}"], [PSN[7]])
                            s = nslot("vst", None, 2)
                            cp("vector", vst[s], psb[7][:, :], [PSN[7]], [f"vst{s}"])
                            P.dma(v_s[t0 + q4 * 128:t0 + (q4 + 1) * 128, :], vst[s], [f"vst{s}"], [], queue="gpsimd")
                    steps.append((load, comp))
                    continue

                def load(c=c, ti=ti):
                    s = nslot("win", ("A", ti, c), 3)
                    P.dma(win[s], win_s[c], [], [f"win{s}"])

                def comp(c=c, ci=ci, ti=ti, t0=t0, prefix=prefix):
                    s = slot[("A", ti, c)]
                    po = 5 + (ci % 2)
                    for k in range(8):
                        mm(psb[po][:, :], win[s][:, k, :], B["h"][:, k, :], k == 0, k == 7, [f"win{s}", f"h{k}"], [PSN[po]])
                    o = nslot("ost", None, 4)
                    if c < 4:
                        act(ost[o], psb[po][:, :], AF.Identity, [PSN[po]], [f"ost{o}"], scale=0.125)
                        dst = qT_s[c * 128:(c + 1) * 128, t0:t0 + NT]
                    elif c < 8:
                        cp("vector", ost[o], psb[po][:, :], [PSN[po]], [f"ost{o}"])
                        P.op("vector", lambda e: e.tensor_reduce(out=kmacc[:, c - 4, 2 * ti:2 * ti + 2],
                                                                 in_=psb[po][:, :].rearrange("p (b t) -> p b t", b=2),
                                                                 axis=AX.X, op=ALU.add), [PSN[po]], ["kmacc"])
                        dst = kT_s[(c - 4) * 128:(c - 3) * 128, t0:t0 + NT]
                    elif c < 16:
                        if prefix:
                            act(ost[o], psb[po][:, :], AF.Identity, [PSN[po]], [f"ost{o}"], scale=uflag[:, 0:1])
                        else:
                            act(ost[o], psb[po][:, :], AF.Identity, [PSN[po]], [f"ost{o}"])
                        dst = uT_s[(c - 12) * 128:(c - 11) * 128, t0:t0 + NT]
                    elif c < 24:
                        act(ost[o], psb[po][:, :], AF.Sigmoid, [PSN[po]], [f"ost{o}"])
                        dst = sga_s[(c - 16) * 128:(c - 15) * 128, t0:t0 + NT]
                    else:
                        act(ost[o], psb[po][:, :], AF.Sigmoid, [PSN[po]], [f"ost{o}"])
                        dst = sgs_s[(c - 24) * 128:(c - 23) * 128, t0:t0 + NT]
                    P.dma(dst, ost[o], [f"ost{o}"], [], queue="gpsimd")
                steps.append((load, comp))
            pipeline(steps, 2)
            for _ in range(3):
                if deferred:
                    deferred.pop(0)()
        while deferred:
            deferred.pop(0)()
        ts("vector", kmacc[:], kmacc[:], 1.0 / 256, None, ALU.mult, None, ["kmacc"], ["kmacc"])
        P.dma(kmean_s.rearrange("(c p) n -> p c n", p=128), kmacc[:], ["kmacc"], [])
        P.barrier()


    ssm_gen = None
    if "ssm" in stages:
        ar.reset()
        hpi = ar.f32(1)
        P.op("gpsimd", lambda e: e.memset(hpi, float(np.pi / 2)), [], ["sm"])
        SM = {}
        def smt(name, n=16):
            SM[name] = ar.f32(n)
            return SM[name]
        for nm in ("lr", "li", "ld", "dt", "mag", "ang", "c", "s", "t1", "t2", "t3", "abr", "abi", "nr", "den", "fre", "fim", "nfim"):
            smt(nm)
        AR = ar.f32(13 * 16).rearrange("p (k g) -> p k g", k=13)
        AI = ar.f32(13 * 16).rearrange("p (k g) -> p k g", k=13)
        NAI = ar.f32(13 * 16).rearrange("p (k g) -> p k g", k=13)
        UR = ar.f32(9 * 16).rearrange("p (k g) -> p k g", k=9)
        UI = ar.f32(9 * 16).rearrange("p (k g) -> p k g", k=9)
        NUI = ar.f32(9 * 16).rearrange("p (k g) -> p k g", k=9)
        R = ["sm"]
        P.dma(SM["lr"], lamr_d, [], R)
        P.dma(SM["li"], lami_d, [], R)
        P.dma(SM["ld"], logdt_d, [], R)
        act(SM["dt"], SM["ld"], AF.Exp, R, R)
        tt("vector", SM["t1"], SM["lr"], SM["dt"], ALU.mult, R, R)
        act(SM["mag"], SM["t1"], AF.Exp, R, R)
        tt("vector", SM["ang"], SM["li"], SM["dt"], ALU.mult, R, R)
        act(SM["s"], SM["ang"], AF.Sin, R, R, scale=1.0 / 16)
        act(SM["c"], SM["ang"], AF.Sin, R, R, scale=1.0 / 16, bias=hpi)
        for _ in range(4):
            tt("vector", SM["t1"], SM["c"], SM["c"], ALU.mult, R, R)
            tt("vector", SM["t2"], SM["s"], SM["s"], ALU.mult, R, R)
            tt("vector", SM["t3"], SM["c"], SM["s"], ALU.mult, R, R)
            tt("vector", SM["c"], SM["t1"], SM["t2"], ALU.subtract, R, R)
            ts("vector", SM["s"], SM["t3"], 2.0, None, ALU.mult, None, R, R)
        cp("vector", UR[:, 0, :], SM["c"], R, R)
        cp("vector", UI[:, 0, :], SM["s"], R, R)
        for k in range(8):
            tt("vector", SM["t1"], UR[:, k, :], UR[:, k, :], ALU.mult, R, R)
            tt("vector", SM["t2"], UI[:, k, :], UI[:, k, :], ALU.mult, R, R)
            tt("vector", SM["t3"], UR[:, k, :], UI[:, k, :], ALU.mult, R, R)
            tt("vector", UR[:, k + 1, :], SM["t1"], SM["t2"], ALU.subtract, R, R)
            ts("vector", UI[:, k + 1, :], SM["t3"], 2.0, None, ALU.mult, None, R, R)
        ts("vector", NUI.rearrange("p k g -> p (k g)"), UI.rearrange("p k g -> p (k g)"), -1.0, None, ALU.mult, None, R, R)
        tt("vector", AR[:, 0, :], SM["mag"], SM["c"], ALU.mult, R, R)
        tt("vector", AI[:, 0, :], SM["mag"], SM["s"], ALU.mult, R, R)
        for k in range(12):
            tt("vector", SM["t1"], AR[:, k, :], AR[:, k, :], ALU.mult, R, R)
            tt("vector", SM["t2"], AI[:, k, :], AI[:, k, :], ALU.mult, R, R)
            tt("vector", SM["t3"], AR[:, k, :], AI[:, k, :], ALU.mult, R, R)
            tt("vector", AR[:, k + 1, :], SM["t1"], SM["t2"], ALU.subtract, R, R)
            ts("vector", AI[:, k + 1, :], SM["t3"], 2.0, None, ALU.mult, None, R, R)
        ts("vector", NAI.rearrange("p k g -> p (k g)"), AI.rearrange("p k g -> p (k g)"), -1.0, None, ALU.mult, None, R, R)
        ts("vector", SM["nr"], AR[:, 0, :], -1.0, None, ALU.add, None, R, R)
        tt("vector", SM["t1"], SM["lr"], SM["lr"], ALU.mult, R, R)
        tt("vector", SM["t2"], SM["li"], SM["li"], ALU.mult, R, R)
        tt("vector", SM["den"], SM["t1"], SM["t2"], ALU.add, R, R)
        P.op("vector", lambda e: e.reciprocal(out=SM["den"], in_=SM["den"]), R, R)
        tt("vector", SM["t1"], SM["nr"], SM["lr"], ALU.mult, R, R)
        tt("vector", SM["t2"], AI[:, 0, :], SM["li"], ALU.mult, R, R)
        tt("vector", SM["t1"], SM["t1"], SM["t2"], ALU.add, R, R)
        tt("vector", SM["fre"], SM["t1"], SM["den"], ALU.mult, R, R)
        tt("vector", SM["t1"], AI[:, 0, :], SM["lr"], ALU.mult, R, R)
        tt("vector", SM["t2"], SM["nr"], SM["li"], ALU.mult, R, R)
        tt("vector", SM["t1"], SM["t1"], SM["t2"], ALU.subtract, R, R)
        tt("vector", SM["fim"], SM["t1"], SM["den"], ALU.mult, R, R)
        BTre = ar.bf16(16 * 128).rearrange("p (q j) -> p q j", q=16)
        BTim = ar.bf16(16 * 128).rearrange("p (q j) -> p q j", q=16)
        CBre = ar.bf16(16 * 32).rearrange("p (g c) -> p g c", g=16)
        CBren = ar.bf16(16 * 32).rearrange("p (g c) -> p g c", g=16)
        CBimn = ar.bf16(16 * 32).rearrange("p (g c) -> p g c", g=16)
        ssm_mark = ar.off
        CTre = ar.f32(16 * 32).rearrange("p (g c) -> p g c", g=16)
        CTimn = ar.f32(16 * 32).rearrange("p (g c) -> p g c", g=16)
        CTren = ar.f32(16 * 32).rearrange("p (g c) -> p g c", g=16)
        bre = ar.f32(16 * 32).rearrange("p (g c) -> p g c", g=16)
        bim = ar.f32(16 * 32).rearrange("p (g c) -> p g c", g=16)
        btmp = ar.f32(32)
        bbre = ar.bf16(16 * 32).rearrange("p (g c) -> p g c", g=16)
        bbim = ar.bf16(16 * 32).rearrange("p (g c) -> p g c", g=16)
        csr = ar.f32(16 * 128).rearrange("p (g j) -> p g j", g=16)
        csi = ar.f32(16 * 128).rearrange("p (g j) -> p g j", g=16)
        P.dma(bre, bsrc_re_d, [], ["bre"])
        P.dma(bim, bsrc_im_d, [], ["bim"])
        P.dma(csr[0:32], csrc_re_d, [], ["csr"])
        P.dma(csi[0:32], csrc_im_d, [], ["csi"])
        for g in range(16):
            ts("vector", btmp, bim[:, g, :], SM["fim"][:, g:g + 1], None, ALU.mult, None, ["bim"] + R, ["btmp"])
            stt(bbre[:, g, :], bre[:, g, :], SM["fre"][:, g:g + 1], btmp, ALU.mult, ALU.subtract, ["bre", "btmp"] + R, ["bbre"])
            ts("vector", btmp, bre[:, g, :], SM["fim"][:, g:g + 1], None, ALU.mult, None, ["bre"] + R, ["btmp"])
            stt(bbim[:, g, :], bim[:, g, :], SM["fre"][:, g:g + 1], btmp, ALU.mult, ALU.add, ["bim", "btmp"] + R, ["bbim"])
        psbf = [psb[i][:, :].bitcast(BF16) for i in range(8)]
        for g in range(16):
            tr(psbf[0][0:32, 0:128], bbre[:, g, :], ident_bf[:], ["bbre"], [PSN[0]])
            tr(psbf[0][0:32, 128:256], bbim[:, g, :], ident_bf[:], ["bbim"], [PSN[0]])
            cp("vector", BTre[0:32, g, :], psbf[0][0:32, 0:128], [PSN[0]], ["BT"])
            cp("vector", BTim[0:32, g, :], psbf[0][0:32, 128:256], [PSN[0]], ["BT"])
        for g in range(16):
            tr(psb[1][:, 0:32], csr[0:32, g, :], ident_f[0:32, 0:32], ["csr"], [PSN[1]])
            tr(psb[1][:, 32:64], csi[0:32, g, :], ident_f[0:32, 0:32], ["csi"], [PSN[1]])
            cp("vector", CTre[:, g, :], psb[1][:, 0:32], [PSN[1]], ["CT"])
            ts("vector", CTimn[:, g, :], psb[1][:, 32:64], -1.0, None, ALU.mult, None, [PSN[1]], ["CT"])
            ts("vector", CTren[:, g, :], psb[1][:, 0:32], -1.0, None, ALU.mult, None, [PSN[1]], ["CT"])
            cp("vector", CBre[:, g, :], CTre[:, g, :], ["CT"], ["CT"])
            cp("vector", CBren[:, g, :], CTren[:, g, :], ["CT"], ["CT"])
            cp("vector", CBimn[:, g, :], CTimn[:, g, :], ["CT"], ["CT"])
        P.barrier()
        ar.off = ssm_mark
        H = TO
        LC = 256
        NCH = H // LC
        ZTr, ZTi, XTr, XTi = ar.f32(H), ar.f32(H), ar.f32(H), ar.f32(H)
        up1 = ar.bf16(T)
        yst = [ar.f32(512) for _ in range(2)]
        Dc = [ar.f32(512) for _ in range(2)]
        Ds = [ar.f32(512) for _ in range(2)]
        dtmp = ar.f32(128)
        ptm = [ar.f32(512) for _ in range(2)]
        pbf = [ar.bf16(512) for _ in range(8)]
        ones_l = ar.f32(LC)
        rtile = ar.f32(LC)
        ini = ar.f32(4)
        Gs = ar.f32(2)
        P.op("gpsimd", lambda e: e.memset(ones_l, 1.0), [], ["ones_l"])
        ZB_R, ZB_I, YB = 5, 6, 7

        def bn(pref, lo, hi):
            return [f"{pref}{b}" for b in range(lo // 512, (hi + 511) // 512)]

        def build_D(g):
            d = g % 2
            dc, ds_ = Dc[d], Ds[d]
            rn = [f"D{d}"]
            P.op("vector", lambda e: e.memset(dc[:, 0:1], 1.0), rn, rn)
            P.op("vector", lambda e: e.memset(ds_[:, 0:1], 0.0), rn, rn)
            for k in range(8):
                n = 1 << k
                ur, ui, nui = UR[:, k, g:g + 1], UI[:, k, g:g + 1], NUI[:, k, g:g + 1]
                ts("vector", dtmp[:, 0:n], ds_[:, 0:n], nui, None, ALU.mult, None, rn + R, ["dtmp"])
                ts("vector", dc[:, n:2 * n], dc[:, 0:n], ur, None, ALU.mult, None, rn + R, rn)
                tt("vector", dc[:, n:2 * n], dc[:, n:2 * n], dtmp[:, 0:n], ALU.add, rn + ["dtmp"], rn)
                ts("vector", dtmp[:, 0:n], ds_[:, 0:n], ur, None, ALU.mult, None, rn + R, ["dtmp"])
                ts("vector", ds_[:, n:2 * n], dc[:, 0:n], ui, None, ALU.mult, None, rn + R, rn)
                tt("vector", ds_[:, n:2 * n], ds_[:, n:2 * n], dtmp[:, 0:n], ALU.add, rn + ["dtmp"], rn)
            cp("vector", dc[:, 256:512], dc[:, 0:256], rn, rn)
            cp("vector", ds_[:, 256:512], ds_[:, 0:256], rn, rn)

        def ssm_main():
            build_D(0)
            yield
            for g in range(DBG_NPAIR):
                d = g % 2
                dc, ds_ = Dc[d], Ds[d]
                DN = [f"D{d}"]
                P.dma(up1[0:32, :], uT_s[g * 32:(g + 1) * 32, :], [], ["up0"])
                if g + 1 < 16:
                    build_D(g + 1)
                ts("vector", rtile, ones_l, SM["mag"][:, g:g + 1], None, ALU.mult, None, ["ones_l"] + R, ["rtile"])
                yield
                for blk in range(8):
                    cs = slice(blk * 512, (blk + 1) * 512)
                    mm(psb[ZB_R][:, :], BTre[0:32, g, :], up1[0:32, cs], True, True, ["up0", "BT"], [PSN[ZB_R]])
                    mm(psb[ZB_I][:, :], BTim[0:32, g, :], up1[0:32, cs], True, True, ["up0", "BT"], [PSN[ZB_I]])
                    cp("scalar", XTr[:, cs], psb[ZB_R][:, :], [PSN[ZB_R]], [f"xtr{blk}"])
                    cp("scalar", XTi[:, cs], psb[ZB_I][:, :], [PSN[ZB_I]], [f"xti{blk}"])
                    if blk % 2 == 1:
                        yield
                tb = [(XTr, XTi, "xtr", "xti"), (ZTr, ZTi, "ztr", "zti")]
                n = H
                for k in range(12):
                    sr, si, pr_, pi2 = tb[k % 2]
                    dr, di, qr_, qi2 = tb[(k + 1) % 2]
                    m = n // 2
                    svr = sr[:, 0:n].rearrange("p (j two) -> p j two", two=2)
                    svi = si[:, 0:n].rearrange("p (j two) -> p j two", two=2)
                    srcn = bn(pr_, 0, n) + bn(pi2, 0, n)
                    stt(dr[:, 0:m], svi[:, :, 0], NAI[:, k, g:g + 1], svr[:, :, 1], ALU.mult, ALU.add, srcn + R, bn(qr_, 0, m))
                    if k == 0:
                        yield
                    stt(dr[:, 0:m], svr[:, :, 0], AR[:, k, g:g + 1], dr[:, 0:m], ALU.mult, ALU.add, srcn + bn(qr_, 0, m) + R, bn(qr_, 0, m))
                    if k <= 1:
                        yield
                    stt(di[:, 0:m], svr[:, :, 0], AI[:, k, g:g + 1], svi[:, :, 1], ALU.mult, ALU.add, srcn + R, bn(qi2, 0, m))
                    if k == 0:
                        yield
                    stt(di[:, 0:m], svi[:, :, 0], AR[:, k, g:g + 1], di[:, 0:m], ALU.mult, ALU.add, srcn + bn(qi2, 0, m) + R, bn(qi2, 0, m))
                    n = m
                    if k < 4:
                        yield
                cp("vector", Gs[:, 0:1], XTr[:, 0:1], ["xtr0"], ["Gs"])
                cp("vector", Gs[:, 1:2], XTi[:, 0:1], ["xti0"], ["Gs"])
                Gr, Gi = Gs[:, 0:1], Gs[:, 1:2]
                yield
                for ob in range(8):
                    blk = 8 + ob
                    cs = slice(blk * 512, (blk + 1) * 512)
                    co = slice(ob * 512, (ob + 1) * 512)
                    mm(psb[ZB_R][:, :], BTre[0:32, g, :], up1[0:32, cs], True, True, ["up0", "BT"], [PSN[ZB_R]])
                    mm(psb[ZB_I][:, :], BTim[0:32, g, :], up1[0:32, cs], True, True, ["up0", "BT"], [PSN[ZB_I]])
                    t1, t2 = ptm[0], ptm[1]
                    yield
                    tt("vector", ZTr[:, co], psb[ZB_R][:, :], dc, ALU.mult, [PSN[ZB_R]] + DN, [f"ztr{ob}"])
                    tt("vector", t1, psb[ZB_I][:, :], ds_, ALU.mult, [PSN[ZB_I]] + DN, ["ptm0"])
                    tt("vector", ZTi[:, co], psb[ZB_I][:, :], dc, ALU.mult, [PSN[ZB_I]] + DN, [f"zti{ob}"])
                    tt("vector", t2, psb[ZB_R][:, :], ds_, ALU.mult, [PSN[ZB_R]] + DN, ["ptm1"])
                    yield
                    tt("vector", ZTr[:, co], ZTr[:, co], t1, ALU.add, [f"ztr{ob}", "ptm0"], [f"ztr{ob}"])
                    tt("vector", ZTi[:, co], ZTi[:, co], t2, ALU.subtract, [f"zti{ob}", "ptm1"], [f"zti{ob}"])
                    yield
                stt(ZTr[:, 0:1], Gi, NAI[:, 0, g:g + 1], ZTr[:, 0:1], ALU.mult, ALU.add, ["Gs", "ztr0"] + R, ["ztr0"])
                stt(ZTr[:, 0:1], Gr, AR[:, 0, g:g + 1], ZTr[:, 0:1], ALU.mult, ALU.add, ["Gs", "ztr0"] + R, ["ztr0"])
                stt(ZTi[:, 0:1], Gr, AI[:, 0, g:g + 1], ZTi[:, 0:1], ALU.mult, ALU.add, ["Gs", "zti0"] + R, ["zti0"])
                stt(ZTi[:, 0:1], Gi, AR[:, 0, g:g + 1], ZTi[:, 0:1], ALU.mult, ALU.add, ["Gs", "zti0"] + R, ["zti0"])
                for c in range(NCH):
                    cc = slice(c * LC, (c + 1) * LC)
                    ob = c // 2
                    if c == 0:
                        i_r, i_i = 0.0, 0.0
                    else:
                        er, ei = XTr[:, c * LC - 1:c * LC], XTi[:, c * LC - 1:c * LC]
                        ul_r, ul_i, nul_i = UR[:, 8, g:g + 1], UI[:, 8, g:g + 1], NUI[:, 8, g:g + 1]
                        ts("vector", ini[:, 2:3], er, ul_r, None, ALU.mult, None, [f"xtr{(c - 1) // 2}"] + R, ["ini_t"])
                        stt(ini[:, 0:1], ei, nul_i, ini[:, 2:3], ALU.mult, ALU.add, [f"xti{(c - 1) // 2}", "ini_t"] + R, ["ini_r"])
                        ts("vector", ini[:, 3:4], ei, ul_r, None, ALU.mult, None, [f"xti{(c - 1) // 2}"] + R, ["ini_u"])
                        stt(ini[:, 1:2], er, ul_i, ini[:, 3:4], ALU.mult, ALU.add, [f"xtr{(c - 1) // 2}", "ini_u"] + R, ["ini_i"])
                        i_r, i_i = ini[:, 0:1], ini[:, 1:2]
                    P.op("vector", lambda e, cc=cc, i_r=i_r: e.tensor_tensor_scan(out=XTr[:, cc], data0=rtile, data1=ZTr[:, cc], initial=i_r,
                                                                              op0=ALU.mult, op1=ALU.add),
                         [f"ztr{ob}", "rtile", "ini_r"], [f"xtr{ob}"])
                    P.op("vector", lambda e, cc=cc, i_i=i_i: e.tensor_tensor_scan(out=XTi[:, cc], data0=rtile, data1=ZTi[:, cc], initial=i_i,
                                                                              op0=ALU.mult, op1=ALU.add),
                         [f"zti{ob}", "rtile", "ini_i"], [f"xti{ob}"])
                    yield
                pend = None
                for ob in range(8):
                    co = slice(ob * 512, (ob + 1) * 512)
                    q0 = 4 * (ob % 2)
                    m1, m2, m3, m4 = pbf[q0], pbf[q0 + 1], pbf[q0 + 2], pbf[q0 + 3]
                    nn = [f"pbf{q0 + j}" for j in range(4)]
                    tt("vector", m1, XTr[:, co], dc, ALU.mult, [f"xtr{ob}"] + DN, [nn[0]])
                    tt("vector", m2, XTi[:, co], ds_, ALU.mult, [f"xti{ob}"] + DN, [nn[1]])
                    yield
                    tt("vector", m3, XTr[:, co], ds_, ALU.mult, [f"xtr{ob}"] + DN, [nn[2]])
                    tt("vector", m4, XTi[:, co], dc, ALU.mult, [f"xti{ob}"] + DN, [nn[3]])

                    def ymm(ob=ob, m1=m1, m2=m2, m3=m3, m4=m4, nn=nn, g=g):
                        mm(psb[YB][0:32, :], CBre[:, g, :], m1, True, False, [nn[0], "CT"], [PSN[YB]])
                        mm(psb[YB][0:32, :], CBren[:, g, :], m2, False, False, [nn[1], "CT"], [PSN[YB]])
                        mm(psb[YB][0:32, :], CBimn[:, g, :], m3, False, False, [nn[2], "CT"], [PSN[YB]])
                        mm(psb[YB][0:32, :], CBimn[:, g, :], m4, False, True, [nn[3], "CT"], [PSN[YB]])
                        ys = nslot("yst", None, 2)
                        cp("scalar", yst[ys][0:32, :], psb[YB][0:32, :], [PSN[YB]], [f"yst{ys}"])
                        P.dma(ypre_s[g * 32:(g + 1) * 32, TO + ob * 512:TO + (ob + 1) * 512], yst[ys][0:32, :], [f"yst{ys}"], [], queue="gpsimd")
                    if pend is not None:
                        pend()
                    pend = ymm
                    yield
                pend()
                yield

        ssm_gen = ssm_main()
        if "att" not in stages:
            for _ in ssm_gen:
                pass
            P.barrier()

    if "att" in stages:
        if ssm_gen is None:
            ar.reset()
        Kaug = ar.bf16(T)
        Qaug = ar.bf16(TO)
        Vh = ar.bf16(64 * 66).rearrange("p (t d) -> p t d", t=64)
        kmf = ar.f32(32)
        kmb = ar.bf16(32)
        elig = ar.f32(32 * 32).rearrange("p (o n) -> p o n", o=32)
        tri = ar.bf16(128)
        gsc = [ar.f32(32) for _ in range(2)]
        top8 = [ar.f32(8) for _ in range(2)]
        thr = [ar.f32(2) for _ in range(2)]
        mb = [ar.bf16(32) for _ in range(2)]
        pT = [ar.bf16(512) for _ in range(3)]
        rs = [ar.f32(2) for _ in range(2)]
        yh = [ar.bf16(64) for _ in range(2)]
        yattT = ar.bf16(TO)
        psbf = [psb[i][:, :].bitcast(BF16) for i in range(8)]
        P.dma(Kaug[64:96, :], eind_d, [], ["Kind"])
        P.dma(tri, tri_d, [], ["tri"])
        P.dma(elig, elig_d, [], ["elig"])
        P.op("gpsimd", lambda e: e.memset(Vh[:, :, 64:66], 1.0), [], ["Vones"])
        SB = [1, 2]
        B0 = "psB0"
        tick_n = [0]

        def tick():
            tick_n[0] += 1
            if ssm_gen is not None and tick_n[0] % 2 == 0:
                next(ssm_gen, None)

        for h in range(DBG_NH):
            P.dma(Kaug[0:64, :], kT_s[h * 64:(h + 1) * 64, :], [], ["Kaug"])
            P.dma(Kaug[96:100, :], kaug_d[h], [], ["Kaug2"])
            P.dma(Qaug[0:64, :], qT_s[h * 64:(h + 1) * 64, TO:], [], ["Qaug"])
            P.dma(Qaug[96:100, :], qaug_d[h][:, TO:], [], ["Qaug2"])
            vsrc = v_s[:, h * 64:(h + 1) * 64].rearrange("(t p) d -> p t d", p=128)
            for v4 in range(4):
                P.dma(Vh[:, v4 * 16:(v4 + 1) * 16, 0:64], vsrc[:, v4 * 16:(v4 + 1) * 16, :], [], ["Vh"] if v4 == 0 else [f"Vh{v4}"])
            P.dma(kmf[0:64, :], kmean_s[h * 64:(h + 1) * 64, :], [], ["kmf"])
            cp("vector", kmb[0:64, :], kmf[0:64, :], ["kmf"], ["kmb"])
            KR = ["Kaug", "Kaug2", "Kind"]
            QR = ["Qaug", "Qaug2"]
            VR = ["Vh", "Vh1", "Vh2", "Vh3", "Vones"]

            def gate1(qt):
                own = qt // 2
                qc = slice((qt - 32) * 128, (qt - 31) * 128)
                p = qt % 2
                mm(psb[0][:, p * 32:(p + 1) * 32], Qaug[0:64, qc], kmb[0:64, :], True, True, ["Qaug", "kmb"], [B0])
                tt("vector", gsc[p], psb[0][:, p * 32:(p + 1) * 32], elig[:, own, :], ALU.add, [B0, "elig"], [f"g2{p}"])
                P.op("vector", lambda e: e.max(out=top8[p], in_=gsc[p]), [f"g2{p}"], [f"top8{p}"])
                ts("vector", thr[p][:, 0:1], top8[p][:, 2:3], -1e29, None, ALU.max, None, [f"top8{p}"], [f"thr{p}"])
                ts("vector", mb[p], gsc[p], thr[p][:, 0:1], NEGM, ALU.is_lt, ALU.mult, [f"g2{p}", f"thr{p}"], [f"mb{p}"])
                P.op("vector", lambda e: e.memset(mb[p][:, own:own + 1], 0.0), [f"mb{p}"], [f"mb{p}"])

            def gate2(qt):
                qc = slice((qt - 32) * 128, (qt - 31) * 128)
                p = qt % 2
                tr(psbf[0][64:96, 256 + p * 128:256 + (p + 1) * 128], mb[p], ident_bf[:], [f"mb{p}"], [B0])
                cp("scalar", Qaug[64:96, qc], psbf[0][64:96, 256 + p * 128:256 + (p + 1) * 128], [B0], [f"qm{qt}"])

            gate1(32)
            gate2(32)
            sctr = 0
            for qt in range(32, 64):
                own = qt // 2
                i = qt % 2
                qc = slice((qt - 32) * 128, (qt - 31) * 128)
                diag = 2 * own + i
                if qt + 1 < 64:
                    gate1(qt + 1)
                kts = list(range(diag + 1))
                groups = [kts[a:a + 4] for a in range(0, len(kts), 4)]
                po = 3 + ((qt + DBG_FLIP) % 2)
                psO = psb[po][:, 0:65]

                def emit_S(gi):
                    bank = SB[(sctr + gi) % 2]
                    psS = psb[bank][:, :].rearrange("p (s q) -> p s q", s=4)
                    for sl, kt in enumerate(groups[gi]):
                        kc = slice(kt * 128, (kt + 1) * 128)
                        mm(psS[:, sl, :], Kaug[0:100, kc], Qaug[0:100, qc], True, kt != diag, KR + QR + [f"qm{qt}"], [PSN[bank]])
                        if kt == diag:
                            mm(psS[:, sl, :], ident_bf[:], tri, False, True, ["tri"], [PSN[bank]])

                def emit_EP(gi):
                    bank = SB[(sctr + gi) % 2]
                    n = len(groups[gi])
                    pt = pT[(sctr + gi) % 3]
                    act(pt[:, 0:n * 128], psb[bank][:, 0:n * 128], AF.Exp, [PSN[bank]], [f"pT{(sctr + gi) % 3}"])
                    for sl, kt in enumerate(groups[gi]):
                        mm(psO, pt[:, sl * 128:(sl + 1) * 128], Vh[:, kt, 0:65], kt == 0, kt == diag,
                           [f"pT{(sctr + gi) % 3}"] + VR, [PSN[po]])

                emit_S(0)
                for gi in range(len(groups)):
                    if gi + 1 < len(groups):
                        emit_S(gi + 1)
                    emit_EP(gi)
                    tick()
                sctr += len(groups)
                p = qt % 2
                P.op("vector", lambda e, p=p, po=po: e.reciprocal(out=rs[p][:, 0:1], in_=psb[po][:, 64:65]), [PSN[po]], [f"rs{p}"])
                ts("vector", yh[p], psb[po][:, 0:64], rs[p][:, 0:1], None, ALU.mult, None, [PSN[po], f"rs{p}"], [f"yh{p}"])
                tr(psbf[0][0:64, 512 + p * 128:512 + (p + 1) * 128], yh[p], ident_bf[:], [f"yh{p}"], [B0])
                cp("scalar", yattT[0:64, qc], psbf[0][0:64, 512 + p * 128:512 + (p + 1) * 128], [B0], ["yattT"])
                if qt + 1 < 64:
                    gate2(qt + 1)
            P.dma(yattT_s[h * 64:(h + 1) * 64, TO:], yattT[0:64, :], ["yattT"], [], queue="gpsimd")
        if ssm_gen is not None:
            for _ in ssm_gen:
                pass
        P.barrier()

    def phase_C(mix):
        ar.reset()
        B = alloc_common(2 if mix else 3)
        outv = outT.rearrange("(k p) t -> p k t", p=128)
        if mix:
            yatt = ar.bf16(4 * NT).rearrange("p (k t) -> p k t", k=4)
            ypre = ar.f32(4 * NT).rearrange("p (k t) -> p k t", k=4)
            uTt = ar.bf16(4 * NT).rearrange("p (k t) -> p k t", k=4)
            sga = ar.bf16(8 * NT).rearrange("p (k t) -> p k t", k=8)
            sgs = ar.bf16(8 * NT).rearrange("p (k t) -> p k t", k=8)
            merged = ar.bf16(8 * NT).rearrange("p (k t) -> p k t", k=8)
            gT = ar.bf16(4 * NT).rearrange("p (k t) -> p k t", k=4)
            yssm = ar.bf16(4 * NT).rearrange("p (k t) -> p k t", k=4)
            yt = [ar.f32(NT) for _ in range(4)]
            wb = [ar.bf16(512).rearrange("p (k j) -> p k j", k=4) for _ in range(6)]
            wo = [ar.bf16(1024).rearrange("p (k j) -> p k j", k=8) for _ in range(3)]
            v4 = lambda d: d.rearrange("(k p) t -> p k t", p=128)
        P.dma(B["x"][0], x1v[:, :, OT0 * NT:(OT0 + 1) * NT], [], XR(0))
        for ti in range(OT0, NTL):
            xs = ti % 2
            t0 = ti * NT
            x = B["x"][xs]
            if ti + 1 < NTL:
                P.dma(B["x"][1 - xs], x1v[:, :, t0 + NT:t0 + 2 * NT], [], XR(1 - xs))
            if mix:
                P.dma(yatt, v4(yattT_s)[:, :, t0:t0 + NT], [], ["yatt"])
                P.dma(ypre, v4(ypre_s)[:, :, t0:t0 + NT], [], ["ypre"])
                P.dma(uTt, v4(uT_s)[:, :, t0:t0 + NT], [], ["uTt"])
                P.dma(sga, v4(sga_s)[:, :, t0:t0 + NT], [], ["sga"])
                P.dma(sgs, v4(sgs_s)[:, :, t0:t0 + NT], [], ["sgs"])
                for k in range(4):
                    ya = yt[k % 2]
                    yb = yt[2 + k % 2]
                    stt(ya, uTt[:, k, :], dsk[:, k:k + 1], ypre[:, k, :], ALU.mult, ALU.add, ["uTt", "ypre"], [f"yt{k % 2}"])
                    act(yb, ya, AF.Square, [f"yt{k % 2}"], [f"yt{2 + k % 2}"])
                    ts("vector", yb, yb, 0.044715, 1.0, ALU.mult, ALU.add, [f"yt{2 + k % 2}"], [f"yt{2 + k % 2}"])
                    tt("vector", yb, yb, ya, ALU.mult, [f"yt{2 + k % 2}", f"yt{k % 2}"], [f"yt{2 + k % 2}"])
                    act(yb, yb, AF.Sigmoid, [f"yt{2 + k % 2}"], [f"yt{2 + k % 2}"], scale=1.5957691216057308)
                    tt("vector", gT[:, k, :], ya, yb, ALU.mult, [f"yt{k % 2}", f"yt{2 + k % 2}"], [f"gT{k}"])
                steps = []
                for m in range(4):
                    def load(m=m, ti=ti):
                        s = nslot("wb", ("glu", ti, m), 6)
                        P.dma(wb[s], wglu_s[m], [], [f"wb{s}"])

                    def comp(m=m, ti=ti):
                        s = slot[("glu", ti, m)]
                        po = 5 + (m % 2)
                        for k in range(4):
                            mm(psb[po][:, :], wb[s][:, k, :], gT[:, k, :], k == 0, k == 3, [f"wb{s}", f"gT{k}"], [PSN[po]])
                        sgt = B["sg"][m % 2]
                        act(sgt, psb[po][:, :], AF.Sigmoid, [PSN[po]], [f"sg{m % 2}"], bias=bglu[:, m:m + 1])
                        tt("vector", yssm[:, m, :], gT[:, m, :], sgt, ALU.mult, [f"gT{m}", f"sg{m % 2}"], [f"yssm{m}"])
                    steps.append((load, comp))
                for m in range(8):
                    def load(m=m, ti=ti):
                        s = nslot("wb", ("ba", ti, m), 6)
                        P.dma(wb[s], wba_s[m], [], [f"wb{s}"])
                        s = nslot("wb", ("bs", ti, m), 6)
                        P.dma(wb[s], wbs_s[m], [], [f"wb{s}"])

                    def comp(m=m, ti=ti):
                        sa = slot[("ba", ti, m)]
                        ss = slot[("bs", ti, m)]
                        pa = 1 + (m % 2)
                        pss = 3 + (m % 2)
                        for k in range(4):
                            mm(psb[pa][:, :], wb[sa][:, k, :], yatt[:, k, :], k == 0, k == 3, [f"wb{sa}", "yatt"], [PSN[pa]])
                        for k in range(4):
                            mm(psb[pss][:, :], wb[ss][:, k, :], yssm[:, k, :], k == 0, k == 3, [f"wb{ss}", f"yssm{k}"], [PSN[pss]])
                        t1 = yt[m % 2]
                        t2 = yt[2 + m % 2]
                        tt("vector", t1, psb[pa][:, :], sga[:, m, :], ALU.mult, [PSN[pa], "sga"], [f"yt{m % 2}"])
                        tt("vector", t2, psb[pss][:, :], sgs[:, m, :], ALU.mult, [PSN[pss], "sgs"], [f"yt{2 + m % 2}"])
                        tt("gpsimd", merged[:, m, :], t1, t2, ALU.add, [f"yt{m % 2}", f"yt{2 + m % 2}"], [f"mg{m}"])
                    steps.append((load, comp))
                for m in range(8):
                    def load(m=m, ti=ti):
                        s = nslot("wo", ("wo", ti, m), 3)
                        P.dma(wo[s], wout_s[m], [], [f"wo{s}"])

                    def comp(m=m, ti=ti, x=x, xs=xs):
                        s = slot[("wo", ti, m)]
                        po = 5 + (m % 2)
                        for k in range(8):
                            mm(psb[po][:, :], wo[s][:, k, :], merged[:, k, :], k == 0, k == 7, [f"wo{s}", f"mg{k}"], [PSN[po]])
                        stt(x[:, m, :], psb[po][:, :], g2[:, m:m + 1], x[:, m, :], ALU.mult, ALU.add,
                            [PSN[po], f"x{xs}_{m}"], [f"x{xs}_{m}"])
                    steps.append((load, comp))
                pipeline(steps, 2)
            norm_mod(B, xs, a3, b3)
            pipeline(ffn_steps(B, xs, wgu2_s, wd2_s, g3h, ("C2", ti)), 2)
            norm_stats(B, xs)
            for k in range(8):
                stt(x[:, k, :], x[:, k, :], nfin[:, k:k + 1], B["rstd"], ALU.mult, ALU.mult,
                    [f"x{xs}_{k}", "rstd"], [f"x{xs}_{k}"])
            P.dma(outv[:, :, t0 - TO:t0 - TO + NT], x, XR(xs), [], queue="gpsimd", final=True)

    if "C" in stages:
        phase_C(("att" in stages) and ("ssm" in stages))

    P.finish()
    return nc


_CACHE = {}


def _vec8(v):
    return np.ascontiguousarray(np.asarray(v, np.float32).reshape(-1, 128).T)


def _host_inputs(inp, core):
    import ml_dtypes
    f = lambda a: np.ascontiguousarray(np.asarray(a, np.float32))
    m = {}
    b, half = core // 2, core % 2
    xb = np.asarray(inp["x"][b], np.float32)
    if half == 1:
        win = xb
    else:
        win = np.concatenate([np.zeros((TO, D), np.float32), xb[:TO]], axis=0)
    m["xT"] = np.ascontiguousarray(win.T)
    m["uflag"] = np.full((128, 1), float(half), np.float32)
    m["cT"] = _vec8(inp["c"][b])
    m["w_ada"] = f(inp["w_ada"][0])
    m["b_adaT"] = _vec8(inp["b_ada"][0])
    m["nf1"] = _vec8(inp["norm_ffn1"][0]); m["nmix"] = _vec8(inp["norm_mix"][0])
    m["nf2"] = _vec8(inp["norm_ffn2"][0]); m["nfin"] = _vec8(inp["norm_final"])
    for k in ("w_ffn1_in", "w_ffn1_out", "w_ffn2_in", "w_ffn2_out", "w_in", "w_glu", "w_br_att", "w_br_ssm", "w_out"):
        m[k] = f(inp[k][0])
    def gp(a):
        a = np.asarray(a, np.float32).reshape(16, 2, 64)
        return np.ascontiguousarray(a.transpose(1, 2, 0).reshape(128, 16))
    m["lamr"] = gp(inp["lam_re"][0]); m["lami"] = gp(inp["lam_im"][0])
    m["logdt"] = gp(np.repeat(np.asarray(inp["log_dt"][0], np.float32)[:, None], 64, axis=1))
    def bsrc(a):
        a = np.asarray(a, np.float32).reshape(16, 2, 64, 16)
        o = np.zeros((2, 64, 16, 2, 16), np.float32)
        for two in range(2):
            o[two, :, :, two, :] = a[:, two].transpose(1, 0, 2)
        return np.ascontiguousarray(o.reshape(128, 16, 32))
    m["bsrc_re"] = bsrc(inp["ssm_b_re"][0]); m["bsrc_im"] = bsrc(inp["ssm_b_im"][0])
    def csrc(a):
        a = np.asarray(a, np.float32).reshape(16, 2, 16, 64)
        o = np.zeros((2, 16, 16, 2, 64), np.float32)
        for two in range(2):
            o[two, :, :, two, :] = a[:, two].transpose(1, 0, 2)
        return np.ascontiguousarray(o.reshape(32, 16, 128))
    m["csrc_re"] = csrc(inp["ssm_c_re"][0]); m["csrc_im"] = csrc(inp["ssm_c_im"][0])
    m["dsk"] = _vec8(inp["ssm_d"][0]); m["bglu"] = _vec8(inp["b_glu"][0])
    t = np.arange(T)
    kaug = np.zeros((8, 4, T), np.float32); qaug = np.zeros((8, 4, T), np.float32)
    for h in range(8):
        sl = 2.0 ** (-(h + 1))
        kaug[h, 0] = 1.0; kaug[h, 1] = 1.0; kaug[h, 2] = sl * 256.0 * (t // 256); kaug[h, 3] = sl * (t % 256)
        qaug[h, 0] = -sl * 256.0 * (t // 256); qaug[h, 1] = -sl * (t % 256); qaug[h, 2] = 1.0; qaug[h, 3] = 1.0
    m["kaug"] = kaug.astype(ml_dtypes.bfloat16); m["qaug"] = qaug.astype(ml_dtypes.bfloat16)
    m["eind"] = (np.arange(32)[:, None] == (t // 256)[None, :]).astype(np.float32).astype(ml_dtypes.bfloat16)
    kk = np.arange(128)
    m["tri"] = np.where(kk[:, None] <= kk[None, :], 0.0, NEGM).astype(np.float32).astype(ml_dtypes.bfloat16)
    okn = np.arange(32)[None, :] < np.arange(32)[:, None]
    if half == 0:
        okn = okn & (np.arange(32)[None, :] >= 16)
    el = np.where(okn, 0.0, -1e30).astype(np.float32)
    m["elig"] = np.ascontiguousarray(np.broadcast_to(el[None], (128, 32, 32)))
    return m


STAGES = ("cast", "A", "ssm", "att", "C")


def kernel(**inputs):
    key = STAGES
    if key not in _CACHE:
        _CACHE[key] = build_program(STAGES)
    nc = _CACHE[key]
    in_maps = [_host_inputs(inputs, c) for c in range(8)]
    res = run_bass_kernel_spmd(nc, in_maps, core_ids=list(range(8)))
    out = np.empty((4, T, D), np.float32)
    for c in range(8):
        out[c // 2, (c % 2) * TO:(c % 2 + 1) * TO] = res.results[c]["outT"].T
    return out
```
